# Optimizing a Trainium2 kernel written in Bass

```python
import math
import jax, jax.numpy as jnp
from jax import lax
import numpy as np

D_MODEL = 1024
BATCH = 8
SEQ = 8192
DEPTH = 1

DA_HEADS = 8
DA_HEAD_DIM = 64
DA_WIDTH = DA_HEADS * 2 * DA_HEAD_DIM
Q_BLOCK = 128
SSD_EXPAND = 2
D_INNER = SSD_EXPAND * D_MODEL
SSD_HEAD_DIM = 64
SSD_HEADS = D_INNER // SSD_HEAD_DIM
SSD_GROUPS = 4
SSD_STATE = 128
CONV_W = 4
SSD_CHUNK = 128
CONV_DIM = D_INNER + 2 * SSD_GROUPS * SSD_STATE
N_BRANCH = 2
IN_COLS = 3 * DA_WIDTH + D_INNER + CONV_DIM + SSD_HEADS + N_BRANCH * D_MODEL
N_EXPERTS = 256
TOP_K = 8
N_EXPERT_GROUPS = 8
TOPK_GROUPS = 4
D_EXPERT = 256
ROUTED_SCALE = 2.5
MOE_BLOCK = 256
DN_ALPHA = (2.0 * DEPTH) ** 0.25
DN_BETA = (8.0 * DEPTH) ** -0.25
LN_EPS = 1e-5

kernel_name = "hybrid_diffattn_ssd_moe_deepnorm"

F32 = jnp.float32


def _layer_norm(x, g, b):
    xf = x.astype(F32)
    mu = jnp.mean(xf, -1, keepdims=True)
    var = jnp.mean(jnp.square(xf - mu), -1, keepdims=True)
    return ((xf - mu) * lax.rsqrt(var + LN_EPS) * g.astype(F32) + b.astype(F32)).astype(x.dtype)


def _rms_norm(x, w, out_dtype):
    xf = x.astype(F32)
    return (xf * lax.rsqrt(jnp.mean(xf * xf, -1, keepdims=True) + LN_EPS) * w.astype(F32)).astype(out_dtype)


def _diff_attention(q, k, v, lam):
    bsz, s = q.shape[:2]
    q = jnp.transpose(q, (3, 0, 2, 1, 4))
    k = jnp.transpose(k, (3, 0, 2, 1, 4))
    v = jnp.transpose(v, (0, 2, 1, 3))
    scale = DA_HEAD_DIM ** -0.5
    k_pos = jnp.arange(s)

    def block(i):
        start = i * Q_BLOCK
        qb = lax.dynamic_slice_in_dim(q, start, Q_BLOCK, axis=3)
        scores = jnp.einsum('mbhqd,mbhkd->mbhqk', qb, k).astype(F32) * scale
        q_pos = start + jnp.arange(Q_BLOCK)
        causal = k_pos[None, :] <= q_pos[:, None]
        p = jax.nn.softmax(jnp.where(causal, scores, -jnp.inf), axis=-1)
        attn = (p[0] - lam * p[1]).astype(v.dtype)
        return jnp.einsum('bhqk,bhkv->bhqv', attn, v)

    out = lax.map(block, jnp.arange(s // Q_BLOCK))
    return jnp.transpose(out, (1, 0, 3, 2, 4)).reshape(bsz, s, DA_HEADS, 2 * DA_HEAD_DIM)


def _causal_depthwise_conv(x, w, b):
    c = x.shape[-1]
    y = lax.conv_general_dilated(x, w[:, None, :].astype(x.dtype), window_strides=(1,),
                                 padding=[(CONV_W - 1, 0)],
                                 dimension_numbers=('NWC', 'WIO', 'NWC'), feature_group_count=c)
    return y + b.astype(x.dtype)


def _ssd_chunked(x, dt, a, b_mat, c_mat):
    bsz, s = x.shape[:2]
    nc = s // SSD_CHUNK
    r = SSD_HEADS // SSD_GROUPS
    xd = (x * dt[..., None]).reshape(bsz, nc, SSD_CHUNK, SSD_GROUPS, r, SSD_HEAD_DIM)
    da = (dt * a).reshape(bsz, nc, SSD_CHUNK, SSD_GROUPS, r)
    bc = b_mat.reshape(bsz, nc, SSD_CHUNK, SSD_GROUPS, SSD_STATE)
    cc = c_mat.reshape(bsz, nc, SSD_CHUNK, SSD_GROUPS, SSD_STATE)
    xs = tuple(jnp.moveaxis(t, 1, 0) for t in (xd, da, bc, cc))
    tri = jnp.tril(jnp.ones((SSD_CHUNK, SSD_CHUNK), dtype=bool))[None, :, :, None, None]

    def step(state, inp):
        xc, dac, bcc, ccc = inp
        a_cum = jnp.cumsum(dac, axis=1)
        seg = a_cum[:, :, None] - a_cum[:, None, :]
        decay = jnp.exp(jnp.where(tri, seg, -jnp.inf))
        cb = jnp.einsum('blgn,bsgn->blsg', ccc, bcc)
        y_diag = jnp.einsum('blsgr,bsgrp->blgrp', cb[..., None] * decay, xc)
        y_off = jnp.einsum('blgn,bgrpn->blgrp', ccc, state) * jnp.exp(a_cum)[..., None]
        a_last = a_cum[:, -1]
        w = jnp.exp(a_last[:, None] - a_cum)
        new_state = state * jnp.exp(a_last)[..., None, None] + \
            jnp.einsum('bsgn,bsgrp->bgrpn', bcc, xc * w[..., None])
        return new_state, y_diag + y_off

    state0 = jnp.zeros((bsz, SSD_GROUPS, r, SSD_HEAD_DIM, SSD_STATE), F32)
    _, y = lax.scan(step, state0, xs)
    return jnp.moveaxis(y, 0, 1).reshape(bsz, s, SSD_HEADS, SSD_HEAD_DIM)


def _hybrid_mixer(x, w_in, lq1, lk1, lq2, lk2, subln_w, conv_w, conv_b, dt_bias, a_log, d_skip,
                  ssd_norm_w, w_br_attn, w_br_ssd, w_out, lam_init):
    bsz, s, _ = x.shape
    proj = jnp.einsum('bsd,dc->bsc', x, w_in)
    splits = [int(i) for i in np.cumsum([DA_WIDTH, DA_WIDTH, DA_WIDTH, D_INNER, CONV_DIM, SSD_HEADS])]
    q, k, v, z, xbc, dt_raw, gates = jnp.split(proj, splits, axis=-1)

    q = q.reshape(bsz, s, DA_HEADS, 2, DA_HEAD_DIM)
    k = k.reshape(bsz, s, DA_HEADS, 2, DA_HEAD_DIM)
    v = v.reshape(bsz, s, DA_HEADS, 2 * DA_HEAD_DIM)
    lam = (jnp.exp(jnp.sum(lq1.astype(F32) * lk1.astype(F32)))
           - jnp.exp(jnp.sum(lq2.astype(F32) * lk2.astype(F32))) + lam_init)
    attn = _diff_attention(q, k, v, lam)
    attn = (_rms_norm(attn, subln_w, F32) * (1.0 - lam_init)).astype(x.dtype).reshape(bsz, s, DA_WIDTH)

    xbc = jax.nn.silu(_causal_depthwise_conv(xbc, conv_w, conv_b))
    xs, bm, cm = jnp.split(xbc, [D_INNER, D_INNER + SSD_GROUPS * SSD_STATE], axis=-1)
    dt = jax.nn.softplus(dt_raw.astype(F32) + dt_bias.astype(F32))
    a = -jnp.exp(a_log.astype(F32))
    xs_h = xs.reshape(bsz, s, SSD_HEADS, SSD_HEAD_DIM).astype(F32)
    y = _ssd_chunked(xs_h, dt, a,
                     bm.reshape(bsz, s, SSD_GROUPS, SSD_STATE).astype(F32),
                     cm.reshape(bsz, s, SSD_GROUPS, SSD_STATE).astype(F32))
    y = y + d_skip.astype(F32)[:, None] * xs_h
    y = y.reshape(bsz, s, D_INNER) * jax.nn.silu(z.astype(F32))
    y = _rms_norm(y.reshape(bsz, s, SSD_GROUPS, D_INNER // SSD_GROUPS),
                  ssd_norm_w.reshape(SSD_GROUPS, D_INNER // SSD_GROUPS), x.dtype).reshape(bsz, s, D_INNER)

    g_attn, g_ssd = jnp.split(jax.nn.sigmoid(gates.astype(F32)).astype(x.dtype), N_BRANCH, axis=-1)
    merged = g_attn * (attn @ w_br_attn) + g_ssd * (y @ w_br_ssd)
    return merged @ w_out


def _moe(x, w_router, router_bias, w_eg, w_eu, w_ed, w_sg, w_su, w_sd):
    bsz, s, d = x.shape
    t = bsz * s
    xf = x.reshape(t, d)
    scores = jax.nn.sigmoid(jnp.dot(xf.astype(F32), w_router.astype(F32)))
    choice = (scores + router_bias.astype(F32)).reshape(t, N_EXPERT_GROUPS, N_EXPERTS // N_EXPERT_GROUPS)
    grp_score = jnp.sum(lax.top_k(choice, 2)[0], axis=-1)
    _, grp_idx = lax.top_k(grp_score, TOPK_GROUPS)
    grp_mask = jnp.sum(jax.nn.one_hot(grp_idx, N_EXPERT_GROUPS, dtype=F32), axis=1) > 0
    masked = jnp.where(grp_mask[:, :, None], choice, -jnp.inf).reshape(t, N_EXPERTS)
    _, top_idx = lax.top_k(masked, TOP_K)
    top_w = jnp.take_along_axis(scores, top_idx, axis=1)
    top_w = top_w / jnp.sum(top_w, -1, keepdims=True) * ROUTED_SCALE

    n_assign = t * TOP_K
    flat_e = top_idx.reshape(-1)
    flat_tok = jnp.broadcast_to(jnp.arange(t)[:, None], (t, TOP_K)).reshape(-1)
    flat_w = top_w.reshape(-1)
    order = jnp.argsort(flat_e)
    se, stok, sw = flat_e[order], flat_tok[order], flat_w[order]
    counts = jnp.bincount(flat_e, length=N_EXPERTS)
    blocks_per_e = (counts + MOE_BLOCK - 1) // MOE_BLOCK
    blk_end = jnp.cumsum(blocks_per_e)
    blk_start = blk_end - blocks_per_e
    offs = jnp.cumsum(counts) - counts
    pos = blk_start[se] * MOE_BLOCK + (jnp.arange(n_assign) - offs[se])
    n_blocks = (n_assign + MOE_BLOCK - 1) // MOE_BLOCK + N_EXPERTS
    n_rows = n_blocks * MOE_BLOCK
    ptok = jnp.zeros((n_rows,), jnp.int32).at[pos].set(stok)
    pw = jnp.zeros((n_rows,), F32).at[pos].set(sw)
    blk_expert = jnp.minimum(jnp.searchsorted(blk_end, jnp.arange(n_blocks), side='right'), N_EXPERTS - 1)

    def expert_block(acc, inp):
        e, tok, w = inp
        xb = xf[tok]
        h = jax.nn.silu(xb @ w_eg[e]) * (xb @ w_eu[e])
        yb = ((h @ w_ed[e]) * w[:, None]).astype(acc.dtype)
        return acc.at[tok].add(yb), None

    routed, _ = lax.scan(expert_block, jnp.zeros_like(xf),
                         (blk_expert, ptok.reshape(n_blocks, MOE_BLOCK), pw.reshape(n_blocks, MOE_BLOCK)))
    shared = (jax.nn.silu(xf @ w_sg) * (xf @ w_su)) @ w_sd
    return (routed + shared).reshape(bsz, s, d)


def _normal(key, shape, std):
    return jax.random.normal(key, shape, F32) * std


def setup_inputs(seed: int = 0) -> dict:
    key = jax.random.key(seed)
    ks = jax.random.split(key, 32)
    L, d = DEPTH, D_MODEL
    s_in = d ** -0.5
    w_in = jnp.concatenate([
        _normal(ks[1], (L, d, 2 * DA_WIDTH), s_in),
        _normal(ks[2], (L, d, DA_WIDTH), DN_BETA * s_in),
        _normal(ks[3], (L, d, D_INNER), s_in),
        _normal(ks[4], (L, d, D_INNER), DN_BETA * s_in),
        _normal(ks[5], (L, d, 2 * SSD_GROUPS * SSD_STATE), s_in),
        _normal(ks[6], (L, d, SSD_HEADS), s_in),
        _normal(ks[7], (L, d, N_BRANCH * D_MODEL), s_in),
    ], axis=-1)
    dt0 = jnp.exp(jax.random.uniform(ks[12], (L, SSD_HEADS), F32, math.log(1e-3), math.log(1e-1)))
    return {
        "x": jax.random.normal(ks[0], (BATCH, SEQ, D_MODEL), F32),
        "w_in": w_in,
        "lambda_q1": _normal(ks[8], (L, DA_HEAD_DIM), 0.1),
        "lambda_k1": _normal(ks[9], (L, DA_HEAD_DIM), 0.1),
        "lambda_q2": _normal(ks[10], (L, DA_HEAD_DIM), 0.1),
        "lambda_k2": _normal(ks[11], (L, DA_HEAD_DIM), 0.1),
        "attn_subln_w": 1.0 + _normal(ks[13], (L, 2 * DA_HEAD_DIM), 0.02),
        "conv_w": _normal(ks[14], (L, CONV_W, CONV_DIM), CONV_W ** -0.5),
        "conv_b": _normal(ks[15], (L, CONV_DIM), 0.02),
        "dt_bias": dt0 + jnp.log(-jnp.expm1(-dt0)),
        "a_log": jnp.log(jax.random.uniform(ks[16], (L, SSD_HEADS), F32, 1.0, 16.0)),
        "d_skip": 1.0 + _normal(ks[17], (L, SSD_HEADS), 0.02),
        "ssd_norm_w": 1.0 + _normal(ks[18], (L, D_INNER), 0.02),
        "w_br_attn": _normal(ks[19], (L, DA_WIDTH, d), DN_BETA * DA_WIDTH ** -0.5),
        "w_br_ssd": _normal(ks[20], (L, D_INNER, d), DN_BETA * D_INNER ** -0.5),
        "w_out": _normal(ks[21], (L, d, d), DN_BETA * s_in),
        "ln1_g": 1.0 + _normal(ks[22], (L, d), 0.02),
        "ln1_b": _normal(ks[23], (L, d), 0.02),
        "w_router": _normal(ks[24], (L, d, N_EXPERTS), s_in),
        "router_bias": _normal(ks[25], (L, N_EXPERTS), 0.01),
        "w_exp_gate": _normal(ks[26], (L, N_EXPERTS, d, D_EXPERT), DN_BETA * s_in),
        "w_exp_up": _normal(ks[27], (L, N_EXPERTS, d, D_EXPERT), DN_BETA * s_in),
        "w_exp_down": _normal(ks[28], (L, N_EXPERTS, D_EXPERT, d), DN_BETA * D_EXPERT ** -0.5),
        "w_sh_gate": _normal(ks[29], (L, d, D_EXPERT), DN_BETA * s_in),
        "w_sh_up": _normal(ks[30], (L, d, D_EXPERT), DN_BETA * s_in),
        "w_sh_down": _normal(ks[31], (L, D_EXPERT, d), DN_BETA * D_EXPERT ** -0.5),
        "ln2_g": 1.0 + _normal(jax.random.fold_in(key, 101), (L, d), 0.02),
        "ln2_b": _normal(jax.random.fold_in(key, 102), (L, d), 0.02),
    }


def reference(x, w_in, lambda_q1, lambda_k1, lambda_q2, lambda_k2, attn_subln_w, conv_w, conv_b,
              dt_bias, a_log, d_skip, ssd_norm_w, w_br_attn, w_br_ssd, w_out, ln1_g, ln1_b,
              w_router, router_bias, w_exp_gate, w_exp_up, w_exp_down, w_sh_gate, w_sh_up, w_sh_down,
              ln2_g, ln2_b):
    h = x
    for layer in range(DEPTH):
        lam_init = 0.8 - 0.6 * math.exp(-0.3 * layer)
        y = _hybrid_mixer(h, w_in[layer], lambda_q1[layer], lambda_k1[layer], lambda_q2[layer],
                          lambda_k2[layer], attn_subln_w[layer], conv_w[layer], conv_b[layer],
                          dt_bias[layer], a_log[layer], d_skip[layer], ssd_norm_w[layer],
                          w_br_attn[layer], w_br_ssd[layer], w_out[layer], lam_init)
        h = _layer_norm(DN_ALPHA * h + y, ln1_g[layer], ln1_b[layer])
        y = _moe(h, w_router[layer], router_bias[layer], w_exp_gate[layer], w_exp_up[layer],
                 w_exp_down[layer], w_sh_gate[layer], w_sh_up[layer], w_sh_down[layer])
        h = _layer_norm(DN_ALPHA * h + y, ln2_g[layer], ln2_b[layer])
    return h
```

```python
import os
import numpy as np
import concourse.bass as bass
import concourse.mybir as mybir
from concourse.bass_utils import run_bass_kernel_spmd

F32 = mybir.dt.float32
BF16 = mybir.dt.bfloat16
I32 = mybir.dt.int32
U32 = mybir.dt.uint32
AF = mybir.ActivationFunctionType
ALU = mybir.AluOpType
AX = mybir.AxisListType

SEQ = 8192
DM = 1024
NE = 256
CAP = 512
ALPHA = 2.0 ** 0.25
LAM_INIT = 0.2
EPS = 1e-5
DTSZ = {F32: 4, BF16: 2, I32: 4, U32: 4}


class Buf:
    __slots__ = ("name", "writers", "readers", "dsem", "dcnt", "dkey", "excl", "last_dma")

    def __init__(self, name, excl=False):
        self.name = name
        self.writers = []
        self.readers = []
        self.dsem = None
        self.dcnt = 0
        self.dkey = None
        self.excl = excl
        self.last_dma = None


class Ins:
    __slots__ = ("eng", "order", "fn", "waits", "needed", "sem", "val", "is_dma", "key")


class Sched:
    ENGS = ("pe", "act", "dve", "pool", "sp")

    def __init__(self, nc):
        self.nc = nc
        self.prog = {e: [] for e in self.ENGS}
        self.seen = {e: {} for e in self.ENGS}
        self.esem = {e: nc.alloc_semaphore("e_" + e) for e in self.ENGS}
        self.slots = []
        self.free_dsems = []
        self.nds = 0

    def _deps(self, eng, reads, writes, own_key=None):
        deps = {}

        def add(d):
            if d.eng == eng and not d.is_dma and eng == "pe":
                return
            if own_key is not None and d.is_dma and d.key == own_key:
                return
            cur = deps.get(d.key)
            if cur is None or cur.order < d.order:
                deps[d.key] = d

        for b in reads:
            for d in b.writers:
                add(d)
            if b.excl:
                for d in b.readers:
                    add(d)
        for b in writes:
            for d in b.writers:
                add(d)
            for d in b.readers:
                add(d)
        waits = []
        seen = self.seen[eng]
        for key, d in deps.items():
            if seen.get(key, -1) >= d.order:
                continue
            seen[key] = d.order
            d.needed = True
            waits.append(d)
        return waits

    def _commit(self, ins, reads, writes, acc):
        for b in writes:
            if acc:
                b.writers.append(ins)
                if len(b.writers) > 48:
                    b.writers = self._compress(b.writers)
            else:
                b.writers = [ins]
                b.readers = []
        for b in reads:
            b.readers.append(ins)
            if len(b.readers) > 48:
                b.readers = self._compress(b.readers)

    @staticmethod
    def _compress(lst):
        last = {}
        for r in lst:
            c = last.get(r.key)
            if c is None or c.order < r.order:
                last[r.key] = r
        return list(last.values())

    def op(self, eng, fn, reads=(), writes=(), acc=False):
        ins = Ins()
        ins.eng = eng
        ins.is_dma = False
        ins.key = eng
        ins.order = len(self.prog[eng])
        ins.fn = fn
        ins.needed = False
        ins.sem = self.esem[eng]
        ins.val = None
        ins.waits = self._deps(eng, reads, writes)
        self.prog[eng].append(ins)
        self._commit(ins, reads, writes, acc)
        return ins

    def dma(self, eng, fn, slot, reads=(), writes=(), acc=False):
        if slot.dsem is None:
            if self.free_dsems:
                slot.dsem, slot.dcnt, slot.dkey = self.free_dsems.pop()
            else:
                self.nds += 1
                slot.dkey = "ds%d" % self.nds
                slot.dsem = self.nc.alloc_semaphore(slot.dkey)
                slot.dcnt = 0
            self.slots.append(slot)
        ins = Ins()
        ins.eng = eng
        ins.is_dma = True
        ins.key = slot.dkey
        slot.dcnt += 1
        ins.order = slot.dcnt
        ins.fn = fn
        ins.needed = True
        ins.sem = slot.dsem
        ins.val = 16 * slot.dcnt
        ins.waits = self._deps(eng, reads, writes, own_key=(slot.dkey if acc else None))
        self.prog[eng].append(ins)
        self._commit(ins, reads, writes, acc)
        slot.last_dma = ins
        return ins

    def barrier(self, release=True):
        lasts = []
        for e in self.ENGS:
            for ins in reversed(self.prog[e]):
                if ins.fn is not None and not ins.is_dma:
                    lasts.append(ins)
                    break
        for s in self.slots:
            if s.last_dma is not None:
                lasts.append(s.last_dma)
        for e in self.ENGS:
            waits = []
            seen = self.seen[e]
            for d in lasts:
                if d.eng == e and not d.is_dma:
                    continue
                if seen.get(d.key, -1) >= d.order:
                    continue
                seen[d.key] = d.order
                d.needed = True
                waits.append(d)
            ins = Ins()
            ins.eng = e
            ins.is_dma = False
            ins.key = e
            ins.order = len(self.prog[e])
            ins.fn = None
            ins.needed = False
            ins.sem = self.esem[e]
            ins.val = None
            ins.waits = waits
            self.prog[e].append(ins)
        if release:
            for s in self.slots:
                self.free_dsems.append((s.dsem, s.dcnt, s.dkey))
                s.dsem = None
            self.slots = []

    def emit(self):
        nc = self.nc
        for e in self.ENGS:
            c = 0
            for ins in self.prog[e]:
                if not ins.is_dma and ins.needed:
                    c += 1
                    ins.val = c
        engobj = {"pe": nc.tensor, "act": nc.scalar, "dve": nc.vector, "pool": nc.gpsimd, "sp": nc.sync}

        def run(e):
            eo = engobj[e]
            for ins in self.prog[e]:
                for w in ins.waits:
                    eo.wait_ge(w.sem, w.val)
                if ins.fn is None:
                    continue
                r = ins.fn()
                if ins.is_dma:
                    r.then_inc(ins.sem, 16)
                elif ins.needed:
                    r.then_inc(ins.sem, 1)

        with nc.Block() as block:
            @block.tensor
            def _(eng):
                run("pe")

            @block.scalar
            def _(eng):
                run("act")

            @block.vector
            def _(eng):
                run("dve")

            @block.gpsimd
            def _(eng):
                run("pool")

            @block.sync
            def _(eng):
                run("sp")


CST_COLS = dict(ident=(0, 128), triu=(128, 256), sl=(256, 384), slt=(384, 512), ones=(512, 640), iota=(640, 896))
PRM_LAYOUT = [("lam4", 256), ("subln", 1), ("convw", 96), ("convb", 24), ("dtb", 32), ("alog", 32), ("dsk", 32),
              ("rbias", 256), ("ssdw", 2048), ("ln1g", 1024), ("ln1b", 1024), ("ln2g", 1024), ("ln2b", 1024)]
PRM_OFF = {}
_o = 0
for _n, _w in PRM_LAYOUT:
    PRM_OFF[_n] = (_o, _o + _w)
    _o += _w
PRM_W = _o


def build_program(last_phase=9, debug=False):
    nc = bass.Bass("TRN2", target_bir_lowering=False)
    S = Sched(nc)
    eng = {"pe": nc.tensor, "act": nc.scalar, "dve": nc.vector, "pool": nc.gpsimd, "sp": nc.sync}

    def DIN(name, shape, dt):
        return nc.dram_tensor(name, list(shape), dt, kind="ExternalInput").ap()

    def DSCR(name, shape, dt, dbg=False):
        kind = "ExternalOutput" if (dbg and debug) else "Internal"
        return nc.dram_tensor(name, list(shape), dt, kind=kind).ap()

    xT_d = DIN("xT", [DM, SEQ], F32)
    x_d = DIN("x", [SEQ, DM], F32)
    w_in_d = DIN("w_in", [DM, 10272], F32)
    cst_d = DIN("cst", [128, 896], F32)
    prm_d = DIN("prm", [128, PRM_W], F32)
    tokid_d = DIN("tokid", [128, 64], I32)
    w_bra_d = DIN("w_br_attn", [1024, 1024], F32)
    w_brs_d = DIN("w_br_ssd", [2048, 1024], F32)
    w_out_d = DIN("w_out", [1024, 1024], F32)
    w_rt_d = DIN("w_router", [1024, 256], F32)
    w_sg_d = DIN("w_sh_gate", [1024, 256], F32)
    w_su_d = DIN("w_sh_up", [1024, 256], F32)
    w_sd_d = DIN("w_sh_down", [256, 1024], F32)
    w_eg_d = DIN("w_exp_gate", [NE, 1024, 256], F32)
    w_eu_d = DIN("w_exp_up", [NE, 1024, 256], F32)
    w_ed_d = DIN("w_exp_down", [NE, 256, 1024], F32)
    out_d = nc.dram_tensor("out", [SEQ, DM], F32, kind="ExternalOutput").ap()

    QT_d = DSCR("QT", [1024, SEQ], BF16, dbg=True)
    KT_d = DSCR("KT", [1024, SEQ], BF16)
    V_d = DSCR("V", [SEQ, 1024], BF16, dbg=True)
    ZS_d = DSCR("ZS", [SEQ, 2048], BF16, dbg=True)
    XBCT_d = DSCR("XBCT", [3072, SEQ], BF16)
    DT_d = DSCR("DT", [SEQ, 32], F32, dbg=True)
    GT_d = DSCR("GT", [2048, SEQ], BF16, dbg=True)
    XS_d = DSCR("XS", [SEQ, 2048], BF16, dbg=True)
    BTM_d = DSCR("BTM", [SEQ, 512], BF16, dbg=True)
    BCT_d = DSCR("BCT", [1024, SEQ], BF16, dbg=True)
    AT_d = DSCR("AT", [1024, SEQ], BF16, dbg=True)
    YT_d = DSCR("YT", [2048, SEQ], BF16, dbg=True)
    R2_d = DSCR("R2", [SEQ, DM], F32, dbg=True)
    H1_d = DSCR("H1", [SEQ, DM], F32, dbg=True)
    XB_d = DSCR("XB", [NE * CAP, DM], BF16)
    Y_d = DSCR("Y", [NE * CAP, DM], BF16)

    def A(name, shape, dt):
        return nc.alloc_sbuf_tensor("s_" + name, shape, dt)
    cst = A("cst", [128, 896], F32)
    prm = A("prm", [128, PRM_W], F32)
    cstb = A("cstb", [128, 640], BF16)
    tokid = A("tokid", [128, 64], I32)
    slot_all = A("slot_all", [128, 64, 8], I32)
    tw_all = A("tw_all", [128, 64, 8], F32)
    derived = A("derived", [128, 40], F32)
    B_cst, B_prm, B_cstb, B_tokid = Buf("cst"), Buf("prm"), Buf("cstb"), Buf("tokid")
    B_slot, B_tw, B_der = Buf("slot_all"), Buf("tw_all"), Buf("derived")
    bst = A("bst", [128, 12], F32)
    mv = A("mv", [128, 4], F32)
    B_bst, B_mv = Buf("bst"), Buf("mv")

    def C(name):
        a, b = CST_COLS[name]
        return cst[:, a:b]

    def CB(name):
        a, b = CST_COLS[name]
        return cstb[:, a:b]

    def P(name):
        a, b = PRM_OFF[name]
        return prm[:, a:b]

    PS = [nc.alloc_psum_tensor("psb%d" % i, [128, 512], F32) for i in range(8)]
    BPS = [Buf("psb%d" % i, excl=True) for i in range(8)]

    arena_base = (nc.sbuf_base + 63) // 64 * 64
    arena_top = nc.sbuf_top
    st = {"ptr": arena_base, "n": 0}

    def phase_begin():
        st["ptr"] = arena_base

    def SB(name, shape, dt, excl=False):
        sz = DTSZ[dt]
        for s_ in shape[1:]:
            sz *= s_
        sz = (sz + 63) // 64 * 64
        off = st["ptr"]
        assert off + sz <= arena_top, ("SBUF arena overflow", name, off + sz - arena_top)
        st["ptr"] = off + sz
        st["n"] += 1
        t = nc.alloc_sbuf_tensor_at("%s_%d" % (name, st["n"]), list(shape), dt, offset=off)
        return t, Buf(name)

    def MM(out, lhsT, rhs, start, stop, R, W, acc=False):
        S.op("pe", lambda: nc.tensor.matmul(out, lhsT=lhsT, rhs=rhs, start=start, stop=stop), R, W, acc)

    def TR(out, in_, ident, R, W, acc=False):
        S.op("pe", lambda: nc.tensor.transpose(out=out, in_=in_, identity=ident), R, W, acc)

    def ACT(out, in_, func, R, W, bias=None, scale=None, accum_out=None, acc=False):
        kw = {}
        if bias is not None:
            kw["bias"] = bias
        if scale is not None:
            kw["scale"] = scale
        if accum_out is not None:
            kw["accum_out"] = accum_out
        S.op("act", lambda: nc.scalar.activation(out=out, in_=in_, func=func, **kw), R, W, acc)

    def CP(e, out, in_, R, W, acc=False):
        if e == "act":
            S.op("act", lambda: nc.scalar.copy(out=out, in_=in_), R, W, acc)
        else:
            S.op(e, lambda: eng[e].tensor_copy(out=out, in_=in_), R, W, acc)

    def TT(e, out, in0, in1, op, R, W, acc=False):
        S.op(e, lambda: eng[e].tensor_tensor(out=out, in0=in0, in1=in1, op=op), R, W, acc)

    def TS(e, out, in0, s1, s2, op0, op1, R, W, acc=False):
        if s2 is None:
            S.op(e, lambda: eng[e].tensor_scalar(out=out, in0=in0, scalar1=s1, scalar2=None, op0=op0), R, W, acc)
        else:
            S.op(e, lambda: eng[e].tensor_scalar(out=out, in0=in0, scalar1=s1, scalar2=s2, op0=op0, op1=op1), R, W, acc)

    def STT(out, in0, scalar, in1, op0, op1, R, W, acc=False):
        S.op("dve", lambda: nc.vector.scalar_tensor_tensor(out=out, in0=in0, scalar=scalar, in1=in1, op0=op0, op1=op1), R, W, acc)

    def RED(out, in_, op, R, W, acc=False):
        S.op("dve", lambda: nc.vector.tensor_reduce(out=out, in_=in_, axis=AX.X, op=op), R, W, acc)

    def MSET(e, ap, val, W, acc=False):
        S.op(e, lambda: eng[e].memset(ap, val), (), W, acc)

    def DMA(q, out, in_, slot, R, W, acc=False):
        S.dma(q, lambda: eng[q].dma_start(out=out, in_=in_), slot, R, W, acc)

    NOBUF = ()
    regs = {}

    def _init_pool_regs():
        regs["bc"] = nc.gpsimd.alloc_register("bc_reg")
        return nc.gpsimd.reg_mov(regs["bc"], NE * CAP - 1)

    S.op("pool", _init_pool_regs)

    DMA("sp", cst[:], cst_d, B_cst, NOBUF, [B_cst])
    DMA("sp", prm[:], prm_d, B_prm, NOBUF, [B_prm])
    DMA("sp", tokid[:], tokid_d, B_tokid, NOBUF, [B_tokid])
    CP("act", cstb[:], cst[:, 0:640], [B_cst], [B_cstb])
    l4 = P("lam4")
    phase_begin()
    tmpA, B_tmpA = SB("tmpA", [128, 64], F32)
    tmpS, B_tmpS = SB("tmpS", [128, 4], F32)
    TT("dve", tmpA[:], l4[:, 0:64], l4[:, 64:128], ALU.mult, [B_prm], [B_tmpA])
    RED(tmpS[:, 0:1], tmpA[:], ALU.add, [B_tmpA], [B_tmpS])
    TT("dve", tmpA[:], l4[:, 128:192], l4[:, 192:256], ALU.mult, [B_prm, B_tmpS], [B_tmpA])
    RED(tmpS[:, 1:2], tmpA[:], ALU.add, [B_tmpA], [B_tmpS], acc=True)
    ACT(tmpS[:, 2:4], tmpS[:, 0:2], AF.Exp, [B_tmpS], [B_tmpS])
    TS("dve", derived[:, 0:1], tmpS[:, 3:4], tmpS[:, 2:3], -LAM_INIT, ALU.subtract, ALU.add, [B_tmpS], [B_der])
    TS("dve", derived[:, 1:2], P("subln"), 1.0 - LAM_INIT, None, ALU.mult, None, [B_prm], [B_der], acc=True)
    ACT(derived[:, 8:40], P("alog"), AF.Exp, [B_prm], [B_der], acc=True)
    TS("dve", derived[:, 8:40], derived[:, 8:40], -1.0, None, ALU.mult, None, [B_der], [B_der])
    S.barrier()

    phase_begin()
    xTb, B_xTb = SB("xTb", [128, 8, SEQ], BF16)
    zero_t, B_zero = SB("zero", [128, 8192], BF16)
    Wb = [SB("Wb%d" % i, [128, 8, 512], BF16) for i in range(2)]
    stg = [SB("stg%d" % i, [128, 512], BF16) for i in range(4)]
    stgf = [SB("stgf%d" % i, [128, 32], F32) for i in range(2)]
    MSET("dve", zero_t[:], 0.0, [B_zero])
    for k in range(8):
        for hh in range(2):
            DMA("pool", xTb[:, k, hh * 4096:(hh + 1) * 4096], xT_d[k * 128:(k + 1) * 128, hh * 4096:(hh + 1) * 4096],
                B_xTb, NOBUF, [B_xTb], acc=True)
    XBz = XB_d.rearrange("(n p r) d -> n p (r d)", p=128, r=8)
    for n in range(NE * CAP // 1024):
        DMA("sp", XBz[n], zero_t[:], B_zero, [B_zero], NOBUF)

    w_in_v = w_in_d.rearrange("(k p) c -> p k c", p=128)
    blocks = []
    for i in range(2):
        blocks.append((i * 512, 512, "FM", QT_d, i * 512, None))
    for i in range(2):
        blocks.append((1024 + i * 512, 512, "FM", KT_d, i * 512, None))
    for i in range(2):
        blocks.append((2048 + i * 512, 512, "TM", V_d, i * 512, None))
    for i in range(4):
        blocks.append((3072 + i * 512, 512, "TM", ZS_d, i * 512, AF.Silu))
    for i in range(6):
        blocks.append((5120 + i * 512, 512, "FM", XBCT_d, i * 512, None))
    blocks.append((8192, 32, "TMF", DT_d, 0, None))
    for i in range(4):
        blocks.append((8224 + i * 512, 512, "FM", GT_d, i * 512, AF.Sigmoid))

    def load_w(bi):
        c0, ncol = blocks[bi][0], blocks[bi][1]
        wt, wbuf = Wb[bi % 2]
        DMA("pool", wt[:, :, 0:ncol], w_in_v[:, :, c0:c0 + ncol], wbuf, NOBUF, [wbuf])

    load_w(0)
    ev = 0
    for bi, (c0, ncol, kind, dst, dc0, func) in enumerate(blocks):
        if bi + 1 < len(blocks):
            load_w(bi + 1)
        wt, wbuf = Wb[bi % 2]
        if kind == "FM":
            for ct in range(ncol // 128):
                for tc in range(16):
                    pb = ev % 4
                    for k in range(8):
                        MM(PS[pb][:, :], wt[:, k, ct * 128:(ct + 1) * 128], xTb[:, k, tc * 512:(tc + 1) * 512],
                           k == 0, k == 7, [wbuf, B_xTb], [BPS[pb]], acc=(k > 0))
                    sg, bsg = stg[ev % 4]
                    if func is not None:
                        ACT(sg[:], PS[pb][:, :], func, [BPS[pb]], [bsg])
                    elif ev % 2 == 0:
                        CP("act", sg[:], PS[pb][:, :], [BPS[pb]], [bsg])
                    else:
                        CP("dve", sg[:], PS[pb][:, :], [BPS[pb]], [bsg])
                    DMA("sp", dst[dc0 + ct * 128: dc0 + (ct + 1) * 128, tc * 512:(tc + 1) * 512], sg[:], bsg, [bsg], NOBUF)
                    ev += 1
        else:
            for tt in range(64):
                pb = ev % 4
                for k in range(8):
                    MM(PS[pb][:, 0:ncol], xTb[:, k, tt * 128:(tt + 1) * 128], wt[:, k, 0:ncol],
                       k == 0, k == 7, [wbuf, B_xTb], [BPS[pb]], acc=(k > 0))
                if kind == "TMF":
                    sg, bsg = stgf[ev % 2]
                    CP("dve", sg[:], PS[pb][:, 0:ncol], [BPS[pb]], [bsg])
                    DMA("sp", dst[tt * 128:(tt + 1) * 128, :], sg[:], bsg, [bsg], NOBUF)
                else:
                    sg, bsg = stg[ev % 4]
                    if func is not None:
                        ACT(sg[:], PS[pb][:, :], func, [BPS[pb]], [bsg])
                    elif ev % 2 == 0:
                        CP("act", sg[:], PS[pb][:, :], [BPS[pb]], [bsg])
                    else:
                        CP("dve", sg[:], PS[pb][:, :], [BPS[pb]], [bsg])
                    DMA("sp", dst[tt * 128:(tt + 1) * 128, dc0:dc0 + 512], sg[:], bsg, [bsg], NOBUF)
                ev += 1
    S.barrier()

    phase_begin()
    xin = [SB("xin%d" % i, [128, 24, 515], BF16) for i in range(2)]
    cacc = [SB("cacc%d" % i, [128, 512], F32) for i in range(2)]
    xo = [SB("xo%d" % i, [128, 24, 512], BF16) for i in range(2)]
    xsst = [SB("xsst%d" % i, [128, 4, 2048], BF16) for i in range(2)]
    btst = [SB("btst%d" % i, [128, 4, 512], BF16) for i in range(2)]
    XBCT_v = XBCT_d.rearrange("(c p) t -> p c t", p=128)
    convw = P("convw")
    convb = P("convb")

    def load_xin(blk):
        t, b = xin[blk % 2]
        if blk == 0:
            MSET("dve", t[:, :, 0:3], 0.0, [b])
            DMA("sp", t[:, :, 3:515], XBCT_v[:, :, 0:512], b, NOBUF, [b], acc=True)
        else:
            DMA("sp", t[:, :, :], XBCT_v[:, :, blk * 512 - 3: blk * 512 + 512], b, NOBUF, [b])

    load_xin(0)
    ev = 0
    for blk in range(16):
        if blk + 1 < 16:
            load_xin(blk + 1)
        xi, bxi = xin[blk % 2]
        xot, bxo = xo[blk % 2]
        for ct in range(24):
            ca, bca = cacc[ct % 2]
            TS("dve", ca[:], xi[:, ct, 0:512], convw[:, ct * 4:ct * 4 + 1], None, ALU.mult, None, [bxi, B_prm], [bca])
            for j in range(1, 4):
                STT(ca[:], xi[:, ct, j:j + 512], convw[:, ct * 4 + j:ct * 4 + j + 1], ca[:], ALU.mult, ALU.add, [bxi, bca, B_prm], [bca])
            ACT(xot[:, ct, :], ca[:], AF.Silu, [bca, B_prm], [bxo], bias=convb[:, ct:ct + 1], scale=1.0, acc=(ct > 0))
        DMA("sp", BCT_d.rearrange("(c p) t -> p c t", p=128)[:, :, blk * 512:(blk + 1) * 512], xot[:, 16:24, :], bxo, [bxo], NOBUF)
        xs_t, bxs = xsst[blk % 2]
        bt_t, bbt = btst[blk % 2]
        for tt in range(4):
            for grp in range(3):
                nct = 8 if grp < 2 else 4
                pb = ev % 4
                psv = PS[pb][:].bitcast(BF16)
                for i in range(nct):
                    ct = grp * 8 + i
                    TR(psv[:, i * 128:(i + 1) * 128], xot[:, ct, tt * 128:(tt + 1) * 128], CB("ident"), [bxo, B_cstb], [BPS[pb]], acc=(i > 0))
                if grp < 2:
                    dst_ap, bdst = xs_t[:, tt, grp * 1024:(grp + 1) * 1024], bxs
                else:
                    dst_ap, bdst = bt_t[:, tt, :], bbt
                CP("act" if ev % 2 == 0 else "dve", dst_ap, psv[:, 0:nct * 128], [BPS[pb]], [bdst], acc=True)
                ev += 1
        DMA("sp", XS_d.rearrange("(t p) c -> p t c", p=128)[:, blk * 4:(blk + 1) * 4, :], xs_t[:], bxs, [bxs], NOBUF)
        DMA("sp", BTM_d.rearrange("(t p) c -> p t c", p=128)[:, blk * 4:(blk + 1) * 4, :], bt_t[:], bbt, [bbt], NOBUF)
    S.barrier()
    if last_phase < 2:
        return finish(nc, S, out_d)

    phase_begin()
    KTh = [SB("KTh%d" % i, [128, SEQ], BF16) for i in range(2)]
    Qz = [[SB("Qz%d_%d" % (i, m), [128, SEQ], BF16) for m in range(2)] for i in range(2)]
    Vh = [SB("Vh%d" % i, [128, 64, 128], BF16) for i in range(2)]
    pT = [SB("pT%d" % i, [128, 512], BF16) for i in range(6)]
    Osb = [SB("Osb%d" % i, [128, 512], F32) for i in range(4)]
    Lsb = [SB("Lsb%d" % i, [128, 512], F32) for i in range(4)]
    rL = [SB("rL%d" % i, [128, 512], F32) for i in range(2)]
    Aa = [SB("Aa%d" % i, [128, 512], F32) for i in range(2)]
    at32, B_at32 = SB("at32", [128, 512], F32)
    sq32, B_sq32 = SB("sq32", [128, 512], F32)
    rstd, B_rstd = SB("rstd", [128, 512], F32)
    ato = [SB("ato%d" % i, [128, 512], BF16) for i in range(2)]
    V_v = V_d.rearrange("(t p) c -> p t c", p=128)
    for i in range(2):
        MSET("dve", Qz[i][0][0][64:128, :], 0.0, [Qz[i][0][1]])
        MSET("pool", Qz[i][1][0][0:64, :], 0.0, [Qz[i][1][1]])

    def load_head(h):
        kt, bk = KTh[h % 2]
        vt, bv = Vh[h % 2]
        DMA("sp", kt[:], KT_d[h * 128:(h + 1) * 128, :], bk, NOBUF, [bk])
        q0, bq0 = Qz[h % 2][0]
        q1, bq1 = Qz[h % 2][1]
        DMA("sp", q0[0:64, :], QT_d[h * 128:h * 128 + 64, :], bq0, NOBUF, [bq0], acc=True)
        DMA("sp", q1[64:128, :], QT_d[h * 128 + 64:(h + 1) * 128, :], bq1, NOBUF, [bq1], acc=True)
        DMA("sp", vt[:], V_v[:, :, h * 128:(h + 1) * 128], bv, NOBUF, [bv])

    def attn_epilogue_a(h, j, par, epi):
        for m in range(2):
            ls_, bls = Lsb[par * 2 + m]
            os_, bos = Osb[par * 2 + m]
            rl, brl = rL[m]
            aa, baa = Aa[m]
            S.op("dve", (lambda o=rl, i_=ls_: nc.vector.reciprocal(out=o[:], in_=i_[:])), [bls], [brl])
            TT("dve", aa[:], os_[:], rl[:], ALU.mult, [bos, brl], [baa])
        STT(at32[:], Aa[1][0][:], derived[:, 0:1], Aa[0][0][:], ALU.mult, ALU.add, [Aa[1][1], Aa[0][1], B_der], [B_at32])
        TT("dve", sq32[:], at32[:], at32[:], ALU.mult, [B_at32], [B_sq32])

    def attn_epilogue_b(h, j, par, epi):
        MM(PS[7][:, :], C("ones"), sq32[:], True, True, [B_cst, B_sq32], [BPS[7]])
        TS("dve", rstd[:], PS[7][:, :], 1.0 / 128.0, EPS, ALU.mult, ALU.add, [BPS[7]], [B_rstd])
        ACT(rstd[:], rstd[:], AF.Ln, [B_rstd], [B_rstd])
        ACT(rstd[:], rstd[:], AF.Exp, [B_rstd], [B_rstd], scale=-0.5)
        ao, bao = ato[epi % 2]
        STT(ao[:], at32[:], derived[:, 1:2], rstd[:], ALU.mult, ALU.mult, [B_at32, B_rstd, B_der], [bao])
        DMA("sp", AT_d[h * 128:(h + 1) * 128, j * 512:(j + 1) * 512], ao[:], bao, [bao], NOBUF)

    load_head(0)
    pti = 0
    unit = 0
    pending = None
    LOOK = 2
    MASK_ENG = ("dve", "pool")
    for h in range(8):
        if h + 1 < 8:
            load_head(h + 1)
        kt, bk = KTh[h % 2]
        vt, bv = Vh[h % 2]
        for j in range(16):
            par = unit % 2
            tiles = []
            nk = 4 * j + 4
            for kti in range(nk):
                for m in range(2):
                    r = kti - 4 * j
                    tiles.append((m, kti, r if r > 0 else 0, r >= 0, kti == 0, kti == nk - 1))
            nt = len(tiles)

            def qk(i):
                m, kti, r, diag, first, last = tiles[i]
                c0 = 128 * r
                sb_ = i % 3
                qz, bqz = Qz[h % 2][m]
                MM(PS[sb_][:, c0:512], kt[:, kti * 128:(kti + 1) * 128], qz[:, j * 512 + c0:(j + 1) * 512], True, True, [bk, bqz], [BPS[sb_]])

            def pv(i, pslot):
                m, kti, r, diag, first, last = tiles[i]
                c0 = 128 * r
                sb_ = i % 3
                pt, bpt = pT[pslot]
                ACT(pt[:, c0:512], PS[sb_][:, c0:512], AF.Exp, [BPS[sb_]], [bpt], scale=0.125)
                if diag:
                    TT(MASK_ENG[m], pt[:, c0:c0 + 128], pt[:, c0:c0 + 128], CB("triu"), ALU.mult, [bpt, B_cstb], [bpt])
                MM(PS[3 + m][:, c0:512], vt[:, kti, :], pt[:, c0:512], first, last, [bv, bpt], [BPS[3 + m]], acc=(not first))
                MM(PS[5 + m][:, c0:512], CB("ones"), pt[:, c0:512], first, last, [B_cstb, bpt], [BPS[5 + m]], acc=(not first))

            for i in range(min(LOOK, nt)):
                qk(i)
            for i in range(nt):
                if i + LOOK < nt:
                    qk(i + LOOK)
                pv(i, pti % 6)
                pti += 1
                if i == 0 and pending is not None:
                    attn_epilogue_a(*pending)
                if i == nt - 1 and pending is not None:
                    attn_epilogue_b(*pending)
                    pending = None
            for m in range(2):
                os_, bos = Osb[par * 2 + m]
                ls_, bls = Lsb[par * 2 + m]
                CP("dve", os_[:], PS[3 + m][:, :], [BPS[3 + m]], [bos])
                CP("dve", ls_[:], PS[5 + m][:, :], [BPS[5 + m]], [bls])
            pending = (h, j, par, unit)
            unit += 1
    attn_epilogue_a(*pending)
    attn_epilogue_b(*pending)
    S.barrier()
    if last_phase < 3:
        return finish(nc, S, out_d)

    phase_begin()
    xs_b = [SB("xs%d" % i, [128, 2048], BF16) for i in range(2)]
    bt_b = [SB("bt%d" % i, [128, 512], BF16) for i in range(2)]
    bc_b = [SB("bc%d" % i, [128, 8, 128], BF16) for i in range(2)]
    dt_b = [SB("dtr%d" % i, [128, 32], F32) for i in range(2)]
    zs_b = [SB("zs%d" % i, [128, 2048], BF16) for i in range(2)]
    sm, B_sm = SB("sm", [128, 8, 32], F32)
    xd, B_xd = SB("xd", [128, 2048], BF16)
    xw, B_xw = SB("xw", [128, 2048], BF16)
    xD, B_xD = SB("xD", [128, 2048], F32)
    cbm = [SB("cbm%d" % i, [128, 128], F32) for i in range(4)]
    Lh = [SB("Lh%d" % i, [128, 4, 128], F32) for i in range(3)]
    dec = [SB("dec%d" % i, [128, 4, 128], F32) for i in range(3)]
    MT = [SB("MT%d" % i, [128, 4, 128], BF16) for i in range(3)]
    stf, B_stf = SB("stf", [128, 2048], F32)
    stb, B_stb = SB("stb", [128, 2048], BF16)
    t1, B_t1 = SB("t1", [128, 512], F32)
    t2, B_t2 = SB("t2", [128, 512], F32)
    junk, B_junk = SB("junk", [128, 512], F32)
    ssq, B_ssq = SB("ssq", [128, 4], F32)
    yn, B_yn = SB("yn", [128, 512], BF16)
    yTs = [SB("yTs%d" % i, [128, 16, 512], BF16) for i in range(2)]
    MSET("dve", stf[:], 0.0, [B_stf])
    MSET("pool", stb[:], 0.0, [B_stb])
    BCT_v = BCT_d.rearrange("(c p) t -> p c t", p=128)
    YT_v = YT_d.rearrange("(c p) t -> p c t", p=128)

    def load_chunk(c):
        i = c % 2
        DMA("sp", xs_b[i][0][:], XS_d[c * 128:(c + 1) * 128, :], xs_b[i][1], NOBUF, [xs_b[i][1]])
        DMA("sp", bt_b[i][0][:], BTM_d[c * 128:(c + 1) * 128, :], bt_b[i][1], NOBUF, [bt_b[i][1]])
        DMA("sp", bc_b[i][0][:], BCT_v[:, :, c * 128:(c + 1) * 128], bc_b[i][1], NOBUF, [bc_b[i][1]])
        DMA("sp", dt_b[i][0][:], DT_d[c * 128:(c + 1) * 128, :], dt_b[i][1], NOBUF, [dt_b[i][1]])
        DMA("sp", zs_b[i][0][:], ZS_d[c * 128:(c + 1) * 128, :], zs_b[i][1], NOBUF, [zs_b[i][1]])

    def b3(ap32):
        return ap32.unsqueeze(2).to_broadcast([128, 32, 64])

    load_chunk(0)
    hc = 0
    for c in range(64):
        if c + 1 < 64:
            load_chunk(c + 1)
        i = c % 2
        xs, bxs = xs_b[i]
        bt, bbt = bt_b[i]
        bc, bbc = bc_b[i]
        dtr, bdt = dt_b[i]
        zs, bzs = zs_b[i]
        TT("dve", sm[:, 6, :], dtr[:], P("dtb"), ALU.add, [bdt, B_prm], [B_sm])
        ACT(sm[:, 6, :], sm[:, 6, :], AF.Exp, [B_sm], [B_sm])
        ACT(sm[:, 0, :], sm[:, 6, :], AF.Ln, [B_sm], [B_sm], bias=1.0, scale=1.0)
        TT("dve", sm[:, 1, :], sm[:, 0, :], derived[:, 8:40], ALU.mult, [B_sm, B_der], [B_sm])
        MM(PS[7][:, 0:32], C("triu"), sm[:, 1, :], True, True, [B_cst, B_sm], [BPS[7]])
        MM(PS[7][:, 32:64], C("ones"), sm[:, 1, :], True, True, [B_cst, B_sm], [BPS[7]], acc=True)
        CP("dve", sm[:, 2, :], PS[7][:, 0:32], [BPS[7]], [B_sm])
        ACT(sm[:, 3, :], PS[7][:, 0:32], AF.Exp, [BPS[7]], [B_sm])
        TT("dve", sm[:, 6, :], PS[7][:, 32:64], sm[:, 2, :], ALU.subtract, [BPS[7], B_sm], [B_sm])
        ACT(sm[:, 4, :], sm[:, 6, :], AF.Exp, [B_sm], [B_sm])
        ACT(sm[:, 5, :], PS[7][:, 32:64], AF.Exp, [BPS[7]], [B_sm])
        xs3 = xs[:].rearrange("p (h q) -> p h q", h=32)
        TT("dve", xd[:].rearrange("p (h q) -> p h q", h=32), xs3, b3(sm[:, 0, :]), ALU.mult, [bxs, B_sm], [B_xd])
        if c % 4 == 0:
            ys, bys = yTs[(c // 4) % 2]
        for g in range(4):
            MM(PS[6][:, g * 128:(g + 1) * 128], bc[:, g, :], bc[:, 4 + g, :], True, True, [bbc], [BPS[6]], acc=(g > 0))
        for g in range(4):
            TT("dve", cbm[g][0][:], PS[6][:, g * 128:(g + 1) * 128], C("triu"), ALU.mult, [BPS[6], B_cst], [cbm[g][1]])
        def quads(g, hcbox):
            ypb = 4 + (g % 2)
            for qd in range(2):
                h0 = g * 8 + qd * 4
                k3 = hcbox[0] % 3
                lh, blh = Lh[k3]
                de, bde = dec[k3]
                mt, bmt = MT[k3]
                TT("pool", lh[:], C("sl").unsqueeze(1).to_broadcast([128, 4, 128]),
                   sm[:, 1, h0:h0 + 4].unsqueeze(2).to_broadcast([128, 4, 128]), ALU.mult, [B_cst, B_sm], [blh])
                sb_ = hcbox[0] % 2
                for q in range(4):
                    MM(PS[sb_][:, q * 128:(q + 1) * 128], lh[:, q, :], C("triu"), True, True, [blh, B_cst], [BPS[sb_]], acc=(q > 0))
                ACT(de[:], PS[sb_][:, :].rearrange("p (q l) -> p q l", q=4), AF.Exp, [BPS[sb_]], [bde])
                TT("dve", mt[:], de[:], cbm[g][0][:].unsqueeze(1).to_broadcast([128, 4, 128]), ALU.mult, [bde, cbm[g][1]], [bmt])
                for q in range(4):
                    hh = qd * 4 + q
                    hd = h0 + q
                    MM(PS[ypb][:, hh * 64:(hh + 1) * 64], mt[:, q, :], xd[:, hd * 64:(hd + 1) * 64], True, True, [bmt, B_xd], [BPS[ypb]], acc=(hh > 0))
                hcbox[0] += 1
        hcbox = [hc]
        quads(0, hcbox)
        TT("pool", xD[:].rearrange("p (h q) -> p h q", h=32), xs3, b3(P("dsk")), ALU.mult, [bxs, B_prm], [B_xD])
        TT("pool", xw[:].rearrange("p (h q) -> p h q", h=32), xd[:].rearrange("p (h q) -> p h q", h=32), b3(sm[:, 4, :]), ALU.mult, [B_xd, B_sm], [B_xw])
        for g in range(4):
            ypb = 4 + (g % 2)
            if g + 1 < 4:
                quads(g + 1, hcbox)
            sb_ = 2 + hc % 2
            hc += 1
            MM(PS[sb_][:, :], bc[:, 4 + g, :], stb[:, g * 512:(g + 1) * 512], True, True, [bbc, B_stb], [BPS[sb_]])
            TT("dve", t1[:].rearrange("p (h q) -> p h q", h=8), PS[sb_][:].rearrange("p (h q) -> p h q", h=8),
               sm[:, 3, g * 8:(g + 1) * 8].unsqueeze(2).to_broadcast([128, 8, 64]), ALU.mult, [BPS[sb_], B_sm], [B_t1])
            TT("dve", t2[:], PS[ypb][:, :], t1[:], ALU.add, [BPS[ypb], B_t1], [B_t2])
            TT("dve", t2[:], t2[:], xD[:, g * 512:(g + 1) * 512], ALU.add, [B_t2, B_xD], [B_t2])
            TT("dve", t2[:], t2[:], zs[:, g * 512:(g + 1) * 512], ALU.mult, [B_t2, bzs], [B_t2])
            ACT(junk[:], t2[:], AF.Square, [B_t2], [B_junk, B_ssq], accum_out=ssq[:, 0:1])
            TS("dve", ssq[:, 1:2], ssq[:, 0:1], 1.0 / 512.0, EPS, ALU.mult, ALU.add, [B_ssq], [B_ssq])
            ACT(ssq[:, 2:3], ssq[:, 1:2], AF.Ln, [B_ssq], [B_ssq])
            ACT(ssq[:, 3:4], ssq[:, 2:3], AF.Exp, [B_ssq], [B_ssq], scale=-0.5)
            a0, a1 = PRM_OFF["ssdw"]
            STT(yn[:], t2[:], ssq[:, 3:4], prm[:, a0 + g * 512:a0 + (g + 1) * 512], ALU.mult, ALU.mult, [B_t2, B_ssq, B_prm], [B_yn])
            tb = 6 + (g % 2) if False else 7
            psv = PS[7][:].bitcast(BF16)
            for q in range(4):
                TR(psv[:, q * 128:(q + 1) * 128], yn[:, q * 128:(q + 1) * 128], CB("ident"), [B_yn, B_cstb], [BPS[7]], acc=(q > 0))
            CP("act", ys[:, g * 4:(g + 1) * 4, (c % 4) * 128:(c % 4 + 1) * 128], psv[:, 0:512].rearrange("p (q t) -> p q t", q=4),
               [BPS[7]], [bys], acc=True)
            sb_ = 2 + hc % 2
            hc += 1
            MM(PS[sb_][:, :], bt[:, g * 128:(g + 1) * 128], xw[:, g * 512:(g + 1) * 512], True, True, [bbt, B_xw], [BPS[sb_]])
            TT("pool", stf[:, g * 512:(g + 1) * 512].rearrange("p (h q) -> p h q", h=8), stf[:, g * 512:(g + 1) * 512].rearrange("p (h q) -> p h q", h=8),
               sm[:, 5, g * 8:(g + 1) * 8].unsqueeze(2).to_broadcast([128, 8, 64]), ALU.mult, [B_stf, B_sm], [B_stf])
            TT("dve", stf[:, g * 512:(g + 1) * 512], stf[:, g * 512:(g + 1) * 512], PS[sb_][:, :], ALU.add, [B_stf, BPS[sb_]], [B_stf])
            CP("act", stb[:, g * 512:(g + 1) * 512], stf[:, g * 512:(g + 1) * 512], [B_stf], [B_stb])
        if c % 4 == 3:
            DMA("sp", YT_v[:, :, (c // 4) * 512:(c // 4 + 1) * 512], ys[:], bys, [bys], NOBUF)
    S.barrier()
    if last_phase < 4:
        return finish(nc, S, out_d)

    phase_begin()
    wba, B_wba = SB("wba", [128, 8, 1024], BF16)
    wbs, B_wbs = SB("wbs", [128, 16, 1024], BF16)
    wo, B_wo = SB("wo", [128, 8, 1024], BF16)
    DMA("pool", wba[:], w_bra_d.rearrange("(k p) c -> p k c", p=128), B_wba, NOBUF, [B_wba])
    DMA("pool", wbs[:], w_brs_d.rearrange("(k p) c -> p k c", p=128), B_wbs, NOBUF, [B_wbs])
    DMA("pool", wo[:], w_out_d.rearrange("(k p) c -> p k c", p=128), B_wo, NOBUF, [B_wo])
    at_b = [SB("at%d" % i, [128, 8, 256], BF16) for i in range(2)]
    yt_b = [SB("yt%d" % i, [128, 16, 256], BF16) for i in range(2)]
    gt_b = [SB("gt%d" % i, [128, 16, 256], BF16) for i in range(2)]
    xr_b = [SB("xr%d" % i, [128, 2, 1024], F32) for i in range(2)]
    m1, B_m1 = SB("m1", [128, 256], F32)
    m2, B_m2 = SB("m2", [128, 256], F32)
    mT, B_mT = SB("mT", [128, 8, 256], BF16)
    h1s = [SB("h1s%d" % i, [128, 1024], F32) for i in range(2)]
    AT_v = AT_d.rearrange("(c p) t -> p c t", p=128)
    GT_v = GT_d.rearrange("(c p) t -> p c t", p=128)
    YT_v2 = YT_d.rearrange("(c p) t -> p c t", p=128)
    x_v = x_d.rearrange("(t p) c -> p t c", p=128)

    def load_p4(ch):
        i = ch % 2
        DMA("sp", at_b[i][0][:], AT_v[:, :, ch * 256:(ch + 1) * 256], at_b[i][1], NOBUF, [at_b[i][1]])
        DMA("sp", yt_b[i][0][:], YT_v2[:, :, ch * 256:(ch + 1) * 256], yt_b[i][1], NOBUF, [yt_b[i][1]])
        DMA("sp", gt_b[i][0][:], GT_v[:, :, ch * 256:(ch + 1) * 256], gt_b[i][1], NOBUF, [gt_b[i][1]])
        DMA("sp", xr_b[i][0][:], x_v[:, ch * 2:(ch + 1) * 2, :], xr_b[i][1], NOBUF, [xr_b[i][1]])

    def layer_norm(dst, src, bsrc, bdst, gname, bname):
        S.op("dve", lambda: nc.vector.bn_stats(out=bst[:, 0:6], in_=src[:, 0:512]), [bsrc], [B_bst])
        S.op("dve", lambda: nc.vector.bn_stats(out=bst[:, 6:12], in_=src[:, 512:1024]), [bsrc], [B_bst], acc=True)
        S.op("dve", lambda: nc.vector.bn_aggr(out=mv[:, 0:2], in_=bst[:]), [B_bst], [B_mv])
        TS("dve", mv[:, 2:3], mv[:, 1:2], EPS, None, ALU.add, None, [B_mv], [B_mv])
        ACT(mv[:, 2:3], mv[:, 2:3], AF.Ln, [B_mv], [B_mv])
        ACT(mv[:, 3:4], mv[:, 2:3], AF.Exp, [B_mv], [B_mv], scale=-0.5)
        TS("dve", src, src, mv[:, 0:1], mv[:, 3:4], ALU.subtract, ALU.mult, [bsrc, B_mv], [bsrc])
        TT("dve", src, src, P(gname), ALU.mult, [bsrc, B_prm], [bsrc])
        TT("dve", dst, src, P(bname), ALU.add, [bsrc, B_prm], [bdst])

    load_p4(0)
    ev = 0
    for ch in range(32):
        if ch + 1 < 32:
            load_p4(ch + 1)
        i = ch % 2
        at, bat = at_b[i]
        yt, byt = yt_b[i]
        gt, bgt = gt_b[i]
        xr, bxr = xr_b[i]
        for dmi in range(8):
            pa = ev % 2
            pbk = 2 + ev % 2
            ev += 1
            for k in range(8):
                MM(PS[pa][:, 0:256], wba[:, k, dmi * 128:(dmi + 1) * 128], at[:, k, :], k == 0, k == 7, [B_wba, bat], [BPS[pa]], acc=(k > 0))
            for k in range(16):
                MM(PS[pbk][:, 0:256], wbs[:, k, dmi * 128:(dmi + 1) * 128], yt[:, k, :], k == 0, k == 15, [B_wbs, byt], [BPS[pbk]], acc=(k > 0))
            TT("dve", m1[:], PS[pa][:, 0:256], gt[:, dmi, :], ALU.mult, [BPS[pa], bgt], [B_m1])
            TT("dve", m2[:], PS[pbk][:, 0:256], gt[:, 8 + dmi, :], ALU.mult, [BPS[pbk], bgt], [B_m2])
            TT("pool", mT[:, dmi, :], m1[:], m2[:], ALU.add, [B_m1, B_m2], [B_mT], acc=(dmi > 0))
        for tt in range(2):
            T = ch * 2 + tt
            for half in range(2):
                pb = 4 + half
                for k in range(8):
                    MM(PS[pb][:, :], mT[:, k, tt * 128:(tt + 1) * 128], wo[:, k, half * 512:(half + 1) * 512], k == 0, k == 7,
                       [B_mT, B_wo], [BPS[pb]], acc=(k > 0))
                STT(xr[:, tt, half * 512:(half + 1) * 512], xr[:, tt, half * 512:(half + 1) * 512], ALPHA, PS[pb][:, :], ALU.mult, ALU.add,
                    [bxr, BPS[pb]], [bxr])
            hs, bhs = h1s[T % 2]
            layer_norm(hs[:], xr[:, tt, :], bxr, bhs, "ln1g", "ln1b")
            DMA("sp", H1_d[T * 128:(T + 1) * 128, :], hs[:], bhs, [bhs], NOBUF)
    S.barrier()
    if last_phase < 5:
        return finish(nc, S, out_d)

    phase_begin()
    wr, B_wr = SB("wr", [128, 8, 256], F32)
    wsg, B_wsg = SB("wsg", [128, 8, 256], BF16)
    wsu, B_wsu = SB("wsu", [128, 8, 256], BF16)
    wsd, B_wsd = SB("wsd", [128, 2, 1024], BF16)
    DMA("sp", wr[:], w_rt_d.rearrange("(k p) c -> p k c", p=128), B_wr, NOBUF, [B_wr])
    DMA("pool", wsg[:], w_sg_d.rearrange("(k p) c -> p k c", p=128), B_wsg, NOBUF, [B_wsg])
    DMA("pool", wsu[:], w_su_d.rearrange("(k p) c -> p k c", p=128), B_wsu, NOBUF, [B_wsu])
    DMA("pool", wsd[:], w_sd_d.rearrange("(k p) c -> p k c", p=128), B_wsd, NOBUF, [B_wsd])
    h1c_b = [SB("h1c%d" % i, [128, 4, 1024], F32) for i in range(2)]
    h1b = [SB("h1b%d" % i, [128, 1024], BF16) for i in range(2)]
    h1T, B_h1T = SB("h1T", [128, 8, 128], F32)
    h1Tb, B_h1Tb = SB("h1Tb", [128, 8, 512], BF16)
    rt, B_rt = SB("rt", [128, 6, 256], F32)
    selcum, B_selcum = SB("selcum", [128, 256], F32)
    rs_, B_rs = SB("rs", [128, 8, 8], F32)
    i8u, B_i8u = SB("i8u", [128, 8], U32)
    rsc, B_rsc = SB("rsc", [128, 4], F32)
    tmpr, B_tmpr = SB("tmpr", [128, 256], F32)
    sgs, B_sgs = SB("sgs", [128, 512], F32)
    hsT, B_hsT = SB("hsT", [128, 2, 512], BF16)
    r2s = [SB("r2s%d" % i, [128, 1024], F32) for i in range(2)]
    MSET("dve", selcum[:], 0.0, [B_selcum])
    H1_v = H1_d.rearrange("(t p) c -> p t c", p=128)

    def load_h1(ch):
        t_, b_ = h1c_b[ch % 2]
        DMA("sp", t_[:], H1_v[:, ch * 4:(ch + 1) * 4, :], b_, NOBUF, [b_])

    load_h1(0)
    for ch in range(16):
        if ch + 1 < 16:
            load_h1(ch + 1)
        h1c, B_h1c = h1c_b[ch % 2]
        for tt in range(4):
            T = ch * 4 + tt
            hb, bhb = h1b[T % 2]
            CP("act", hb[:], h1c[:, tt, :], [B_h1c], [bhb])
            for half in range(2):
                pb = 4 + half
                for q in range(4):
                    k = half * 4 + q
                    TR(PS[pb][:, q * 128:(q + 1) * 128], h1c[:, tt, k * 128:(k + 1) * 128], C("ident"), [B_h1c, B_cst], [BPS[pb]], acc=(q > 0))
                CP("act", h1T[:, half * 4:(half + 1) * 4, :], PS[pb][:, :].rearrange("p (q t) -> p q t", q=4), [BPS[pb]], [B_h1T], acc=(half > 0))
            CP("pool", h1Tb[:, :, tt * 128:(tt + 1) * 128], h1T[:], [B_h1T], [B_h1Tb], acc=True)
            for k in range(8):
                MM(PS[6][:, 0:256], h1T[:, k, :], wr[:, k, :], k == 0, k == 7, [B_h1T, B_wr], [BPS[6]], acc=(k > 0))
            sc = rt[:, 0, :]
            chh = rt[:, 1, :]
            wk2 = rt[:, 2, :]
            sel = rt[:, 3, :]
            wsel = rt[:, 4, :]
            pos = rt[:, 5, :]
            ACT(sc, PS[6][:, 0:256], AF.Sigmoid, [BPS[6]], [B_rt])
            TT("dve", chh, sc, P("rbias"), ALU.add, [B_rt, B_prm], [B_rt])
            ch3 = chh.rearrange("p (g e) -> p g e", g=8)
            wk3 = wk2.rearrange("p (g e) -> p g e", g=8)
            RED(rs_[:, 0, :], ch3, ALU.max, [B_rt], [B_rs])
            TT("dve", wk3, ch3, rs_[:, 0, :].unsqueeze(2).to_broadcast([128, 8, 32]), ALU.is_equal, [B_rt, B_rs], [B_rt])
            STT(wk2, wk2, -1e9, chh, ALU.mult, ALU.add, [B_rt], [B_rt])
            RED(rs_[:, 1, :], wk3, ALU.max, [B_rt], [B_rs])
            TT("dve", rs_[:, 1, :], rs_[:, 1, :], rs_[:, 0, :], ALU.add, [B_rs], [B_rs])
            S.op("dve", lambda: nc.vector.max(out=rs_[:, 2, :], in_=rs_[:, 1, :]), [B_rs], [B_rs])
            TS("dve", rs_[:, 3, :], rs_[:, 1, :], rs_[:, 2, 3:4], None, ALU.is_ge, None, [B_rs], [B_rs])
            TS("dve", rs_[:, 3, :], rs_[:, 3, :], 1e9, -1e9, ALU.mult, ALU.add, [B_rs], [B_rs])
            TT("dve", wk3, ch3, rs_[:, 3, :].unsqueeze(2).to_broadcast([128, 8, 32]), ALU.add, [B_rt, B_rs], [B_rt])
            S.op("dve", lambda: nc.vector.max(out=rs_[:, 7, :], in_=rt[:, 2, :]), [B_rt, B_rs], [B_rs])
            TS("dve", sel, wk2, rs_[:, 7, 7:8], None, ALU.is_ge, None, [B_rt, B_rs], [B_rt])
            TT("dve", wsel, sc, sel, ALU.mult, [B_rt], [B_rt])
            S.op("dve", lambda: nc.vector.max(out=rs_[:, 4, :], in_=rt[:, 4, :]), [B_rt, B_rs], [B_rs])
            S.op("dve", lambda: nc.vector.max_index(out=i8u[:], in_max=rs_[:, 4, :], in_values=rt[:, 4, :]), [B_rt, B_rs], [B_i8u])
            CP("dve", rs_[:, 5, :], i8u[:], [B_i8u], [B_rs])
            RED(rsc[:, 0:1], rs_[:, 4, :], ALU.add, [B_rs], [B_rsc])
            S.op("dve", lambda: nc.vector.reciprocal(out=rsc[:, 1:2], in_=rsc[:, 0:1]), [B_rsc], [B_rsc])
            MM(PS[7][:, 0:256], C("slt"), sel, True, False, [B_cst, B_rt], [BPS[7]])
            MM(PS[7][:, 0:256], C("ones"), selcum[:], False, True, [B_cst, B_selcum], [BPS[7]], acc=True)
            CP("act", pos, PS[7][:, 0:256], [BPS[7]], [B_rt])
            TT("pool", selcum[:], selcum[:], sel, ALU.add, [B_selcum, B_rt], [B_selcum])
            for k in range(8):
                STT(tmpr[:], cst[:, 640:896], rs_[:, 5, k:k + 1], pos, ALU.is_equal, ALU.mult, [B_cst, B_rs, B_rt, B_tmpr], [B_tmpr])
                RED(rs_[:, 6, k:k + 1], tmpr[:], ALU.add, [B_tmpr], [B_rs])
            STT(rs_[:, 7, :], rs_[:, 5, :], float(CAP), rs_[:, 6, :], ALU.mult, ALU.add, [B_rs], [B_rs])
            TS("dve", rs_[:, 3, :], rs_[:, 6, :], CAP - 0.5, 1e6, ALU.is_gt, ALU.mult, [B_rs], [B_rs])
            TT("dve", rs_[:, 7, :], rs_[:, 7, :], rs_[:, 3, :], ALU.max, [B_rs], [B_rs])
            CP("dve", slot_all[:, T, :], rs_[:, 7, :], [B_rs], [B_slot], acc=True)
            TS("dve", rs_[:, 3, :], rs_[:, 6, :], CAP - 0.5, None, ALU.is_lt, None, [B_rs], [B_rs])
            TS("dve", rs_[:, 4, :], rs_[:, 4, :], rsc[:, 1:2], 2.5, ALU.mult, ALU.mult, [B_rs, B_rsc], [B_rs])
            TT("dve", tw_all[:, T, :], rs_[:, 4, :], rs_[:, 3, :], ALU.mult, [B_rs], [B_tw], acc=True)
            for k in range(8):
                S.dma("pool", (lambda T=T, k=k, hb=hb: nc.gpsimd.indirect_dma_start(
                    out=XB_d[:, :], out_offset=bass.IndirectOffsetOnAxis(ap=slot_all[:, T, k:k + 1], axis=0),
                    in_=hb[:, :], in_offset=None, bounds_check=regs["bc"], oob_is_err=False)),
                    bhb, [bhb, B_slot], NOBUF)
        for fh in range(2):
            for k in range(8):
                MM(PS[0][:, :], wsg[:, k, fh * 128:(fh + 1) * 128], h1Tb[:, k, :], k == 0, k == 7, [B_wsg, B_h1Tb], [BPS[0]], acc=(k > 0))
            for k in range(8):
                MM(PS[1][:, :], wsu[:, k, fh * 128:(fh + 1) * 128], h1Tb[:, k, :], k == 0, k == 7, [B_wsu, B_h1Tb], [BPS[1]], acc=(k > 0))
            ACT(sgs[:], PS[0][:, :], AF.Silu, [BPS[0]], [B_sgs])
            TT("dve", hsT[:, fh, :], PS[1][:, :], sgs[:], ALU.mult, [BPS[1], B_sgs], [B_hsT], acc=(fh > 0))
        for tt in range(4):
            T = ch * 4 + tt
            r2, br2 = r2s[T % 2]
            for half in range(2):
                pb = 2 + half
                for fk in range(2):
                    MM(PS[pb][:, :], hsT[:, fk, tt * 128:(tt + 1) * 128], wsd[:, fk, half * 512:(half + 1) * 512], fk == 0, fk == 1,
                       [B_hsT, B_wsd], [BPS[pb]], acc=(fk > 0))
                STT(r2[:, half * 512:(half + 1) * 512], h1c[:, tt, half * 512:(half + 1) * 512], ALPHA, PS[pb][:, :], ALU.mult, ALU.add,
                    [B_h1c, BPS[pb]], [br2], acc=(half > 0))
            DMA("sp", R2_d[T * 128:(T + 1) * 128, :], r2[:], br2, [br2], NOBUF)
    S.barrier()
    if last_phase < 6:
        return finish(nc, S, out_d)

    phase_begin()
    xb_b = [SB("xb%d" % i, [128, 4, 1024], BF16) for i in range(3)]
    wf_b = [SB("wf%d" % i, [128, 6144], F32) for i in range(3)]
    wbf_b = [SB("wbf%d" % i, [128, 6144], BF16) for i in range(3)]
    xbT_b = [SB("xbT%d" % i, [128, 8, 512], BF16) for i in range(2)]
    sg_b = [SB("sg%d" % i, [128, 512], BF16) for i in range(2)]
    hT_b = [SB("hT%d" % i, [128, 2, 512], BF16) for i in range(2)]
    yb_b = [SB("yb%d" % i, [128, 4, 1024], BF16) for i in range(2)]
    XB_v = XB_d.rearrange("(e p t) d -> e p t d", p=128, t=4)
    Y_v = Y_d.rearrange("(e p t) d -> e p t d", p=128, t=4)

    def load_e(e):
        DMA("sp", xb_b[e % 3][0][:], XB_v[e], xb_b[e % 3][1], NOBUF, [xb_b[e % 3][1]])
        wf, bwf = wf_b[e % 3]
        DMA("sp", wf[:, 0:2048], w_eg_d[e].rearrange("(p k) f -> p (k f)", p=128), bwf, NOBUF, [bwf])
        DMA("sp", wf[:, 2048:4096], w_eu_d[e].rearrange("(p k) f -> p (k f)", p=128), bwf, NOBUF, [bwf], acc=True)
        DMA("sp", wf[:, 4096:6144].rearrange("p (k f) -> p k f", k=2), w_ed_d[e].rearrange("(k p) f -> p k f", p=128), bwf, NOBUF, [bwf], acc=True)

    def cast_e(e):
        wf, bwf = wf_b[e % 3]
        wb, bwb = wbf_b[e % 3]
        CP("act", wb[:, 0:2048], wf[:, 0:2048], [bwf], [bwb])
        CP("dve", wb[:, 2048:4096], wf[:, 2048:4096], [bwf], [bwb], acc=True)
        CP("pool", wb[:, 4096:6144], wf[:, 4096:6144], [bwf], [bwb], acc=True)

    def stage_T(e):
        xb, bxb = xb_b[e % 3]
        xbT, bxbT = xbT_b[e % 2]
        for t in range(4):
            pb = t % 2
            psv = PS[pb][:].bitcast(BF16)
            for k in range(8):
                TR(psv[:, k * 128:(k + 1) * 128], xb[:, t, :].rearrange("p (d k) -> p k d", k=8)[:, k, :], CB("ident"), [bxb, B_cstb], [BPS[pb]], acc=(k > 0))
            CP("act" if t % 2 == 0 else "dve", xbT[:, :, t * 128:(t + 1) * 128], psv[:, :].rearrange("p (k s) -> p k s", k=8), [BPS[pb]], [bxbT], acc=(t > 0))

    def steps_GU(e):
        wb, bwb = wbf_b[e % 3]
        xbT, bxbT = xbT_b[e % 2]
        sg, bsg = sg_b[e % 2]
        hT, bhT = hT_b[e % 2]
        weg = wb[:, 0:2048].rearrange("p (k f) -> p k f", k=8)
        weu = wb[:, 2048:4096].rearrange("p (k f) -> p k f", k=8)

        def g_step(fh):
            for k in range(8):
                MM(PS[2 + fh][:, :], weg[:, k, fh * 128:(fh + 1) * 128], xbT[:, k, :], k == 0, k == 7, [bwb, bxbT], [BPS[2 + fh]], acc=(k > 0))
            ACT(sg[:], PS[2 + fh][:, :], AF.Silu, [BPS[2 + fh]], [bsg])

        def u_step(fh):
            for k in range(8):
                MM(PS[4 + fh][:, :], weu[:, k, fh * 128:(fh + 1) * 128], xbT[:, k, :], k == 0, k == 7, [bwb, bxbT], [BPS[4 + fh]], acc=(k > 0))
            TT("dve", hT[:, fh, :], PS[4 + fh][:, :], sg[:], ALU.mult, [BPS[4 + fh], bsg], [bhT], acc=(fh > 0))

        return [lambda: g_step(0), lambda: u_step(0), lambda: g_step(1), lambda: u_step(1)]

    def steps_D(e):
        wb, bwb = wbf_b[e % 3]
        hT, bhT = hT_b[e % 2]
        yb, byb = yb_b[e % 2]
        wed = wb[:, 4096:6144].rearrange("p (k f) -> p k f", k=2)

        def d_step(n6):
            t, half = n6 // 2, n6 % 2
            pb = 6 + n6 % 2
            for fk in range(2):
                MM(PS[pb][:, :], hT[:, fk, t * 128:(t + 1) * 128], wed[:, fk, half * 512:(half + 1) * 512], fk == 0, fk == 1,
                   [bhT, bwb], [BPS[pb]], acc=(fk > 0))
            CP("act" if n6 % 2 == 0 else "dve", yb[:, t, half * 512:(half + 1) * 512], PS[pb][:, :], [BPS[pb]], [byb], acc=(n6 > 0))

        return [(lambda n6=n6: d_step(n6)) for n6 in range(8)], (lambda: DMA("sp", Y_v[e], yb[:], byb, [byb], NOBUF))

    for e in range(3):
        load_e(e)
    for i in range(-2, NE):
        if 3 <= i + 4 < NE:
            load_e(i + 4)
        if 0 <= i + 2 < NE:
            stage_T(i + 2)
        gsteps = steps_GU(i + 1) if 0 <= i + 1 < NE else None
        dsteps, dstore = steps_D(i) if i >= 0 else (None, None)
        for st_ in range(4):
            if gsteps is not None:
                gsteps[st_]()
            if dsteps is not None:
                dsteps[2 * st_]()
                dsteps[2 * st_ + 1]()
        if dstore is not None:
            dstore()
        if 0 <= i + 2 < NE:
            cast_e(i + 2)
    S.barrier()
    if last_phase < 7:
        return finish(nc, S, out_d)

    phase_begin()
    r2_b = [SB("r2l%d" % i, [128, 1024], F32) for i in range(2)]
    gk_b = [SB("gk%d" % i, [128, 8, 1024], BF16) for i in range(2)]
    ot_b = [SB("ot%d" % i, [128, 1024], F32) for i in range(2)]
    B_out = Buf("out")

    def load_t(T):
        i = T % 2
        DMA("sp", r2_b[i][0][:], R2_d[T * 128:(T + 1) * 128, :], r2_b[i][1], NOBUF, [r2_b[i][1]])
        gk, bgk = gk_b[i]
        for k in range(8):
            S.dma("pool", (lambda T=T, k=k, gk=gk: nc.gpsimd.indirect_dma_start(
                out=gk[:, k, :], out_offset=None, in_=Y_d[:, :],
                in_offset=bass.IndirectOffsetOnAxis(ap=slot_all[:, T, k:k + 1], axis=0),
                bounds_check=regs["bc"], oob_is_err=False)),
                bgk, [B_slot], [bgk], acc=(k > 0))

    for i in range(2):
        MSET("dve", gk_b[i][0][:], 0.0, [gk_b[i][1]])
    load_t(0)
    for T in range(64):
        if T + 1 < 64:
            load_t(T + 1)
        i = T % 2
        r2, br2 = r2_b[i]
        gk, bgk = gk_b[i]
        ot, bot = ot_b[i]
        for k in range(8):
            STT(r2[:], gk[:, k, :], tw_all[:, T, k:k + 1], r2[:], ALU.mult, ALU.add, [bgk, B_tw, br2], [br2])
        layer_norm(ot[:], r2[:], br2, bot, "ln2g", "ln2b")
        DMA("sp", out_d[T * 128:(T + 1) * 128, :], ot[:], bot, [bot], [B_out], acc=True)
    S.barrier(release=False)
    return finish(nc, S, out_d)


def finish(nc, S, out_d):
    S.barrier(release=False)
    S.emit()
    return nc


def make_consts():
    p = np.arange(128)[:, None]
    f = np.arange(128)[None, :]
    cst = np.zeros((128, 896), np.float32)
    cst[:, 0:128] = (p == f)
    cst[:, 128:256] = (p <= f)
    cst[:, 256:384] = (p > f)
    cst[:, 384:512] = (p < f)
    cst[:, 512:640] = 1.0
    cst[:, 640:896] = np.arange(256, dtype=np.float32)[None, :]
    tokid = (np.arange(64)[None, :] * 128 + np.arange(128)[:, None]).astype(np.int32)
    return cst, tokid


def make_prm(inp):
    prm = np.zeros((128, PRM_W), np.float32)

    def put(name, arr):
        a, b = PRM_OFF[name]
        prm[:, a:b] = arr

    put("lam4", np.concatenate([inp["lambda_q1"][0], inp["lambda_k1"][0], inp["lambda_q2"][0], inp["lambda_k2"][0]])[None, :])
    put("subln", inp["attn_subln_w"][0][:, None])
    cw = inp["conv_w"][0]
    put("convw", cw.reshape(4, 24, 128).transpose(2, 1, 0).reshape(128, 96))
    put("convb", inp["conv_b"][0].reshape(24, 128).T)
    put("dtb", inp["dt_bias"][0][None, :])
    put("alog", inp["a_log"][0][None, :])
    put("dsk", inp["d_skip"][0][None, :])
    put("rbias", inp["router_bias"][0][None, :])
    put("ssdw", inp["ssd_norm_w"][0][None, :])
    put("ln1g", inp["ln1_g"][0][None, :])
    put("ln1b", inp["ln1_b"][0][None, :])
    put("ln2g", inp["ln2_g"][0][None, :])
    put("ln2b", inp["ln2_b"][0][None, :])
    return prm


def make_in_maps(inp, batches):
    cst, tokid = make_consts()
    prm = make_prm(inp)
    shared = {
        "w_in": np.ascontiguousarray(inp["w_in"][0]), "cst": cst, "prm": prm, "tokid": tokid,
        "w_br_attn": np.ascontiguousarray(inp["w_br_attn"][0]), "w_br_ssd": np.ascontiguousarray(inp["w_br_ssd"][0]),
        "w_out": np.ascontiguousarray(inp["w_out"][0]), "w_router": np.ascontiguousarray(inp["w_router"][0]),
        "w_sh_gate": np.ascontiguousarray(inp["w_sh_gate"][0]), "w_sh_up": np.ascontiguousarray(inp["w_sh_up"][0]),
        "w_sh_down": np.ascontiguousarray(inp["w_sh_down"][0]),
        "w_exp_gate": np.ascontiguousarray(inp["w_exp_gate"][0]), "w_exp_up": np.ascontiguousarray(inp["w_exp_up"][0]),
        "w_exp_down": np.ascontiguousarray(inp["w_exp_down"][0]),
    }
    maps = []
    for b in batches:
        m = dict(shared)
        xb = np.ascontiguousarray(inp["x"][b])
        m["x"] = xb
        m["xT"] = np.ascontiguousarray(xb.T)
        maps.append(m)
    return maps


def kernel(**inputs):
    inp = {k: np.asarray(v) for k, v in inputs.items()}
    nc = build_program()
    maps = make_in_maps(inp, list(range(8)))
    res = run_bass_kernel_spmd(nc, maps, core_ids=list(range(8)))
    out = np.stack([np.asarray(r["out"]) for r in res.results], axis=0)
    return out.astype(np.float32)
```

```python
import os
import numpy as np
import concourse.bass as bass
import concourse.mybir as mybir
from concourse.bass_utils import run_bass_kernel_spmd

F32 = mybir.dt.float32
BF16 = mybir.dt.bfloat16
I32 = mybir.dt.int32
U32 = mybir.dt.uint32
AF = mybir.ActivationFunctionType
ALU = mybir.AluOpType
AX = mybir.AxisListType

SEQ = 8192
DM = 1024
NE = 256
CAP = 512
ALPHA = 2.0 ** 0.25
LAM_INIT = 0.2
EPS = 1e-5
DTSZ = {F32: 4, BF16: 2, I32: 4, U32: 4}


class Buf:
    __slots__ = ("name", "writers", "readers", "dsem", "dcnt", "dkey", "excl", "last_dma")

    def __init__(self, name, excl=False):
        self.name = name
        self.writers = []
        self.readers = []
        self.dsem = None
        self.dcnt = 0
        self.dkey = None
        self.excl = excl
        self.last_dma = None


class Ins:
    __slots__ = ("eng", "order", "fn", "waits", "needed", "sem", "val", "is_dma", "key")


class Sched:
    ENGS = ("pe", "act", "dve", "pool", "sp")

    def __init__(self, nc):
        self.nc = nc
        self.prog = {e: [] for e in self.ENGS}
        self.seen = {e: {} for e in self.ENGS}
        self.esem = {e: nc.alloc_semaphore("e_" + e) for e in self.ENGS}
        self.slots = []
        self.free_dsems = []
        self.nds = 0

    def _deps(self, eng, reads, writes, own_key=None):
        deps = {}

        def add(d):
            if d.eng == eng and not d.is_dma and eng == "pe":
                return
            if own_key is not None and d.is_dma and d.key == own_key:
                return
            cur = deps.get(d.key)
            if cur is None or cur.order < d.order:
                deps[d.key] = d

        for b in reads:
            for d in b.writers:
                add(d)
            if b.excl:
                for d in b.readers:
                    add(d)
        for b in writes:
            for d in b.writers:
                add(d)
            for d in b.readers:
                add(d)
        waits = []
        seen = self.seen[eng]
        for key, d in deps.items():
            if seen.get(key, -1) >= d.order:
                continue
            seen[key] = d.order
            d.needed = True
            waits.append(d)
        return waits

    def _commit(self, ins, reads, writes, acc):
        for b in writes:
            if acc:
                b.writers.append(ins)
                if len(b.writers) > 48:
                    b.writers = self._compress(b.writers)
            else:
                b.writers = [ins]
                b.readers = []
        for b in reads:
            b.readers.append(ins)
            if len(b.readers) > 48:
                b.readers = self._compress(b.readers)

    @staticmethod
    def _compress(lst):
        last = {}
        for r in lst:
            c = last.get(r.key)
            if c is None or c.order < r.order:
                last[r.key] = r
        return list(last.values())

    def op(self, eng, fn, reads=(), writes=(), acc=False):
        ins = Ins()
        ins.eng = eng
        ins.is_dma = False
        ins.key = eng
        ins.order = len(self.prog[eng])
        ins.fn = fn
        ins.needed = False
        ins.sem = self.esem[eng]
        ins.val = None
        ins.waits = self._deps(eng, reads, writes)
        self.prog[eng].append(ins)
        self._commit(ins, reads, writes, acc)
        return ins

    def dma(self, eng, fn, slot, reads=(), writes=(), acc=False):
        if slot.dsem is None:
            if self.free_dsems:
                slot.dsem, slot.dcnt, slot.dkey = self.free_dsems.pop()
            else:
                self.nds += 1
                slot.dkey = "ds%d" % self.nds
                slot.dsem = self.nc.alloc_semaphore(slot.dkey)
                slot.dcnt = 0
            self.slots.append(slot)
        ins = Ins()
        ins.eng = eng
        ins.is_dma = True
        ins.key = slot.dkey
        slot.dcnt += 1
        ins.order = slot.dcnt
        ins.fn = fn
        ins.needed = True
        ins.sem = slot.dsem
        ins.val = 16 * slot.dcnt
        ins.waits = self._deps(eng, reads, writes, own_key=(slot.dkey if acc else None))
        self.prog[eng].append(ins)
        self._commit(ins, reads, writes, acc)
        slot.last_dma = ins
        return ins

    def barrier(self, release=True):
        lasts = []
        for e in self.ENGS:
            for ins in reversed(self.prog[e]):
                if ins.fn is not None and not ins.is_dma:
                    lasts.append(ins)
                    break
        for s in self.slots:
            if s.last_dma is not None:
                lasts.append(s.last_dma)
        for e in self.ENGS:
            waits = []
            seen = self.seen[e]
            for d in lasts:
                if d.eng == e and not d.is_dma:
                    continue
                if seen.get(d.key, -1) >= d.order:
                    continue
                seen[d.key] = d.order
                d.needed = True
                waits.append(d)
            ins = Ins()
            ins.eng = e
            ins.is_dma = False
            ins.key = e
            ins.order = len(self.prog[e])
            ins.fn = None
            ins.needed = False
            ins.sem = self.esem[e]
            ins.val = None
            ins.waits = waits
            self.prog[e].append(ins)
        if release:
            for s in self.slots:
                self.free_dsems.append((s.dsem, s.dcnt, s.dkey))
                s.dsem = None
            self.slots = []

    def emit(self):
        nc = self.nc
        for e in self.ENGS:
            c = 0
            for ins in self.prog[e]:
                if not ins.is_dma and ins.needed:
                    c += 1
                    ins.val = c
        engobj = {"pe": nc.tensor, "act": nc.scalar, "dve": nc.vector, "pool": nc.gpsimd, "sp": nc.sync}

        def run(e):
            eo = engobj[e]
            for ins in self.prog[e]:
                for w in ins.waits:
                    eo.wait_ge(w.sem, w.val)
                if ins.fn is None:
                    continue
                r = ins.fn()
                if ins.is_dma:
                    r.then_inc(ins.sem, 16)
                elif ins.needed:
                    r.then_inc(ins.sem, 1)

        with nc.Block() as block:
            @block.tensor
            def _(eng):
                run("pe")

            @block.scalar
            def _(eng):
                run("act")

            @block.vector
            def _(eng):
                run("dve")

            @block.gpsimd
            def _(eng):
                run("pool")

            @block.sync
            def _(eng):
                run("sp")


CST_COLS = dict(ident=(0, 128), triu=(128, 256), sl=(256, 384), slt=(384, 512), ones=(512, 640), iota=(640, 896))
PRM_LAYOUT = [("lam4", 256), ("subln", 1), ("convw", 96), ("convb", 24), ("dtb", 32), ("alog", 32), ("dsk", 32),
              ("rbias", 256), ("ssdw", 2048), ("ln1g", 1024), ("ln1b", 1024), ("ln2g", 1024), ("ln2b", 1024)]
PRM_OFF = {}
_o = 0
for _n, _w in PRM_LAYOUT:
    PRM_OFF[_n] = (_o, _o + _w)
    _o += _w
PRM_W = _o


def build_program(last_phase=9, debug=False):
    nc = bass.Bass("TRN2", target_bir_lowering=False)
    S = Sched(nc)
    eng = {"pe": nc.tensor, "act": nc.scalar, "dve": nc.vector, "pool": nc.gpsimd, "sp": nc.sync}

    def DIN(name, shape, dt):
        return nc.dram_tensor(name, list(shape), dt, kind="ExternalInput").ap()

    def DSCR(name, shape, dt, dbg=False):
        kind = "ExternalOutput" if (dbg and debug) else "Internal"
        return nc.dram_tensor(name, list(shape), dt, kind=kind).ap()

    xT_d = DIN("xT", [DM, SEQ], F32)
    x_d = DIN("x", [SEQ, DM], F32)
    w_in_d = DIN("w_in", [DM, 10272], F32)
    cst_d = DIN("cst", [128, 896], F32)
    prm_d = DIN("prm", [128, PRM_W], F32)
    tokid_d = DIN("tokid", [128, 64], I32)
    w_bra_d = DIN("w_br_attn", [1024, 1024], F32)
    w_brs_d = DIN("w_br_ssd", [2048, 1024], F32)
    w_out_d = DIN("w_out", [1024, 1024], F32)
    w_rt_d = DIN("w_router", [1024, 256], F32)
    w_sg_d = DIN("w_sh_gate", [1024, 256], F32)
    w_su_d = DIN("w_sh_up", [1024, 256], F32)
    w_sd_d = DIN("w_sh_down", [256, 1024], F32)
    w_eg_d = DIN("w_exp_gate", [NE, 1024, 256], F32)
    w_eu_d = DIN("w_exp_up", [NE, 1024, 256], F32)
    w_ed_d = DIN("w_exp_down", [NE, 256, 1024], F32)
    out_d = nc.dram_tensor("out", [SEQ, DM], F32, kind="ExternalOutput").ap()

    QT_d = DSCR("QT", [1024, SEQ], BF16, dbg=True)
    KT_d = DSCR("KT", [1024, SEQ], BF16)
    V_d = DSCR("V", [SEQ, 1024], BF16, dbg=True)
    ZS_d = DSCR("ZS", [SEQ, 2048], BF16, dbg=True)
    XBCT_d = DSCR("XBCT", [3072, SEQ], BF16)
    DT_d = DSCR("DT", [SEQ, 32], F32, dbg=True)
    GT_d = DSCR("GT", [2048, SEQ], BF16, dbg=True)
    XS_d = DSCR("XS", [SEQ, 2048], BF16, dbg=True)
    BTM_d = DSCR("BTM", [SEQ, 512], BF16, dbg=True)
    BCT_d = DSCR("BCT", [1024, SEQ], BF16, dbg=True)
    AT_d = DSCR("AT", [1024, SEQ], BF16, dbg=True)
    YT_d = DSCR("YT", [2048, SEQ], BF16, dbg=True)
    R2_d = DSCR("R2", [SEQ, DM], F32, dbg=True)
    H1_d = DSCR("H1", [SEQ, DM], F32, dbg=True)
    XB_d = DSCR("XB", [NE * CAP, DM], BF16)
    Y_d = DSCR("Y", [NE * CAP, DM], BF16)

    def A(name, shape, dt):
        return nc.alloc_sbuf_tensor("s_" + name, shape, dt)
    cst = A("cst", [128, 896], F32)
    prm = A("prm", [128, PRM_W], F32)
    cstb = A("cstb", [128, 640], BF16)
    tokid = A("tokid", [128, 64], I32)
    slot_all = A("slot_all", [128, 64, 8], I32)
    tw_all = A("tw_all", [128, 64, 8], F32)
    derived = A("derived", [128, 40], F32)
    B_cst, B_prm, B_cstb, B_tokid = Buf("cst"), Buf("prm"), Buf("cstb"), Buf("tokid")
    B_slot, B_tw, B_der = Buf("slot_all"), Buf("tw_all"), Buf("derived")
    bst = A("bst", [128, 12], F32)
    mv = A("mv", [128, 4], F32)
    B_bst, B_mv = Buf("bst"), Buf("mv")

    def C(name):
        a, b = CST_COLS[name]
        return cst[:, a:b]

    def CB(name):
        a, b = CST_COLS[name]
        return cstb[:, a:b]

    def P(name):
        a, b = PRM_OFF[name]
        return prm[:, a:b]

    PS = [nc.alloc_psum_tensor("psb%d" % i, [128, 512], F32) for i in range(8)]
    BPS = [Buf("psb%d" % i, excl=True) for i in range(8)]

    arena_base = (nc.sbuf_base + 63) // 64 * 64
    arena_top = nc.sbuf_top
    st = {"ptr": arena_base, "n": 0}

    def phase_begin():
        st["ptr"] = arena_base

    def SB(name, shape, dt, excl=False):
        sz = DTSZ[dt]
        for s_ in shape[1:]:
            sz *= s_
        sz = (sz + 63) // 64 * 64
        off = st["ptr"]
        assert off + sz <= arena_top, ("SBUF arena overflow", name, off + sz - arena_top)
        st["ptr"] = off + sz
        st["n"] += 1
        t = nc.alloc_sbuf_tensor_at("%s_%d" % (name, st["n"]), list(shape), dt, offset=off)
        return t, Buf(name)

    def MM(out, lhsT, rhs, start, stop, R, W, acc=False):
        S.op("pe", lambda: nc.tensor.matmul(out, lhsT=lhsT, rhs=rhs, start=start, stop=stop), R, W, acc)

    def TR(out, in_, ident, R, W, acc=False):
        S.op("pe", lambda: nc.tensor.transpose(out=out, in_=in_, identity=ident), R, W, acc)

    def ACT(out, in_, func, R, W, bias=None, scale=None, accum_out=None, acc=False):
        kw = {}
        if bias is not None:
            kw["bias"] = bias
        if scale is not None:
            kw["scale"] = scale
        if accum_out is not None:
            kw["accum_out"] = accum_out
        S.op("act", lambda: nc.scalar.activation(out=out, in_=in_, func=func, **kw), R, W, acc)

    def CP(e, out, in_, R, W, acc=False):
        if e == "act":
            S.op("act", lambda: nc.scalar.copy(out=out, in_=in_), R, W, acc)
        else:
            S.op(e, lambda: eng[e].tensor_copy(out=out, in_=in_), R, W, acc)

    def TT(e, out, in0, in1, op, R, W, acc=False):
        S.op(e, lambda: eng[e].tensor_tensor(out=out, in0=in0, in1=in1, op=op), R, W, acc)

    def TS(e, out, in0, s1, s2, op0, op1, R, W, acc=False):
        if s2 is None:
            S.op(e, lambda: eng[e].tensor_scalar(out=out, in0=in0, scalar1=s1, scalar2=None, op0=op0), R, W, acc)
        else:
            S.op(e, lambda: eng[e].tensor_scalar(out=out, in0=in0, scalar1=s1, scalar2=s2, op0=op0, op1=op1), R, W, acc)

    def STT(out, in0, scalar, in1, op0, op1, R, W, acc=False):
        S.op("dve", lambda: nc.vector.scalar_tensor_tensor(out=out, in0=in0, scalar=scalar, in1=in1, op0=op0, op1=op1), R, W, acc)

    def RED(out, in_, op, R, W, acc=False):
        S.op("dve", lambda: nc.vector.tensor_reduce(out=out, in_=in_, axis=AX.X, op=op), R, W, acc)

    def MSET(e, ap, val, W, acc=False):
        S.op(e, lambda: eng[e].memset(ap, val), (), W, acc)

    def DMA(q, out, in_, slot, R, W, acc=False):
        S.dma(q, lambda: eng[q].dma_start(out=out, in_=in_), slot, R, W, acc)

    NOBUF = ()
    regs = {}

    def _init_pool_regs():
        regs["bc"] = nc.gpsimd.alloc_register("bc_reg")
        return nc.gpsimd.reg_mov(regs["bc"], NE * CAP - 1)

    S.op("pool", _init_pool_regs)

    DMA("sp", cst[:], cst_d, B_cst, NOBUF, [B_cst])
    DMA("sp", prm[:], prm_d, B_prm, NOBUF, [B_prm])
    DMA("sp", tokid[:], tokid_d, B_tokid, NOBUF, [B_tokid])
    CP("act", cstb[:], cst[:, 0:640], [B_cst], [B_cstb])
    l4 = P("lam4")
    phase_begin()
    tmpA, B_tmpA = SB("tmpA", [128, 64], F32)
    tmpS, B_tmpS = SB("tmpS", [128, 4], F32)
    TT("dve", tmpA[:], l4[:, 0:64], l4[:, 64:128], ALU.mult, [B_prm], [B_tmpA])
    RED(tmpS[:, 0:1], tmpA[:], ALU.add, [B_tmpA], [B_tmpS])
    TT("dve", tmpA[:], l4[:, 128:192], l4[:, 192:256], ALU.mult, [B_prm, B_tmpS], [B_tmpA])
    RED(tmpS[:, 1:2], tmpA[:], ALU.add, [B_tmpA], [B_tmpS], acc=True)
    ACT(tmpS[:, 2:4], tmpS[:, 0:2], AF.Exp, [B_tmpS], [B_tmpS])
    TS("dve", derived[:, 0:1], tmpS[:, 3:4], tmpS[:, 2:3], -LAM_INIT, ALU.subtract, ALU.add, [B_tmpS], [B_der])
    TS("dve", derived[:, 1:2], P("subln"), 1.0 - LAM_INIT, None, ALU.mult, None, [B_prm], [B_der], acc=True)
    ACT(derived[:, 8:40], P("alog"), AF.Exp, [B_prm], [B_der], acc=True)
    TS("dve", derived[:, 8:40], derived[:, 8:40], -1.0, None, ALU.mult, None, [B_der], [B_der])
    S.barrier()

    phase_begin()
    xTb, B_xTb = SB("xTb", [128, 8, SEQ], BF16)
    zero_t, B_zero = SB("zero", [128, 8192], BF16)
    Wb = [SB("Wb%d" % i, [128, 8, 512], BF16) for i in range(2)]
    stg = [SB("stg%d" % i, [128, 512], BF16) for i in range(4)]
    stgf = [SB("stgf%d" % i, [128, 32], F32) for i in range(2)]
    MSET("dve", zero_t[:], 0.0, [B_zero])
    for k in range(8):
        for hh in range(2):
            DMA("pool", xTb[:, k, hh * 4096:(hh + 1) * 4096], xT_d[k * 128:(k + 1) * 128, hh * 4096:(hh + 1) * 4096],
                B_xTb, NOBUF, [B_xTb], acc=True)
    XBz = XB_d.rearrange("(n p r) d -> n p (r d)", p=128, r=8)
    for n in range(NE * CAP // 1024):
        DMA("sp", XBz[n], zero_t[:], B_zero, [B_zero], NOBUF)

    w_in_v = w_in_d.rearrange("(k p) c -> p k c", p=128)
    blocks = []
    for i in range(2):
        blocks.append((i * 512, 512, "FM", QT_d, i * 512, None))
    for i in range(2):
        blocks.append((1024 + i * 512, 512, "FM", KT_d, i * 512, None))
    for i in range(2):
        blocks.append((2048 + i * 512, 512, "TM", V_d, i * 512, None))
    for i in range(4):
        blocks.append((3072 + i * 512, 512, "TM", ZS_d, i * 512, AF.Silu))
    for i in range(6):
        blocks.append((5120 + i * 512, 512, "FM", XBCT_d, i * 512, None))
    blocks.append((8192, 32, "TMF", DT_d, 0, None))
    for i in range(4):
        blocks.append((8224 + i * 512, 512, "FM", GT_d, i * 512, AF.Sigmoid))

    def load_w(bi):
        c0, ncol = blocks[bi][0], blocks[bi][1]
        wt, wbuf = Wb[bi % 2]
        DMA("pool", wt[:, :, 0:ncol], w_in_v[:, :, c0:c0 + ncol], wbuf, NOBUF, [wbuf])

    load_w(0)
    ev = 0
    for bi, (c0, ncol, kind, dst, dc0, func) in enumerate(blocks):
        if bi + 1 < len(blocks):
            load_w(bi + 1)
        wt, wbuf = Wb[bi % 2]
        if kind == "FM":
            for ct in range(ncol // 128):
                for tc in range(16):
                    pb = ev % 4
                    for k in range(8):
                        MM(PS[pb][:, :], wt[:, k, ct * 128:(ct + 1) * 128], xTb[:, k, tc * 512:(tc + 1) * 512],
                           k == 0, k == 7, [wbuf, B_xTb], [BPS[pb]], acc=(k > 0))
                    sg, bsg = stg[ev % 4]
                    if func is not None:
                        ACT(sg[:], PS[pb][:, :], func, [BPS[pb]], [bsg])
                    elif ev % 2 == 0:
                        CP("act", sg[:], PS[pb][:, :], [BPS[pb]], [bsg])
                    else:
                        CP("dve", sg[:], PS[pb][:, :], [BPS[pb]], [bsg])
                    DMA("sp", dst[dc0 + ct * 128: dc0 + (ct + 1) * 128, tc * 512:(tc + 1) * 512], sg[:], bsg, [bsg], NOBUF)
                    ev += 1
        else:
            for tt in range(64):
                pb = ev % 4
                for k in range(8):
                    MM(PS[pb][:, 0:ncol], xTb[:, k, tt * 128:(tt + 1) * 128], wt[:, k, 0:ncol],
                       k == 0, k == 7, [wbuf, B_xTb], [BPS[pb]], acc=(k > 0))
                if kind == "TMF":
                    sg, bsg = stgf[ev % 2]
                    CP("dve", sg[:], PS[pb][:, 0:ncol], [BPS[pb]], [bsg])
                    DMA("sp", dst[tt * 128:(tt + 1) * 128, :], sg[:], bsg, [bsg], NOBUF)
                else:
                    sg, bsg = stg[ev % 4]
                    if func is not None:
                        ACT(sg[:], PS[pb][:, :], func, [BPS[pb]], [bsg])
                    elif ev % 2 == 0:
                        CP("act", sg[:], PS[pb][:, :], [BPS[pb]], [bsg])
                    else:
                        CP("dve", sg[:], PS[pb][:, :], [BPS[pb]], [bsg])
                    DMA("sp", dst[tt * 128:(tt + 1) * 128, dc0:dc0 + 512], sg[:], bsg, [bsg], NOBUF)
                ev += 1
    S.barrier()

    phase_begin()
    xin = [SB("xin%d" % i, [128, 24, 515], BF16) for i in range(2)]
    cacc = [SB("cacc%d" % i, [128, 512], F32) for i in range(2)]
    xo = [SB("xo%d" % i, [128, 24, 512], BF16) for i in range(2)]
    xsst = [SB("xsst%d" % i, [128, 4, 2048], BF16) for i in range(2)]
    btst = [SB("btst%d" % i, [128, 4, 512], BF16) for i in range(2)]
    XBCT_v = XBCT_d.rearrange("(c p) t -> p c t", p=128)
    convw = P("convw")
    convb = P("convb")

    def load_xin(blk):
        t, b = xin[blk % 2]
        if blk == 0:
            MSET("dve", t[:, :, 0:3], 0.0, [b])
            DMA("sp", t[:, :, 3:515], XBCT_v[:, :, 0:512], b, NOBUF, [b], acc=True)
        else:
            DMA("sp", t[:, :, :], XBCT_v[:, :, blk * 512 - 3: blk * 512 + 512], b, NOBUF, [b])

    load_xin(0)
    ev = 0
    for blk in range(16):
        if blk + 1 < 16:
            load_xin(blk + 1)
        xi, bxi = xin[blk % 2]
        xot, bxo = xo[blk % 2]
        for ct in range(24):
            ca, bca = cacc[ct % 2]
            TS("dve", ca[:], xi[:, ct, 0:512], convw[:, ct * 4:ct * 4 + 1], None, ALU.mult, None, [bxi, B_prm], [bca])
            for j in range(1, 4):
                STT(ca[:], xi[:, ct, j:j + 512], convw[:, ct * 4 + j:ct * 4 + j + 1], ca[:], ALU.mult, ALU.add, [bxi, bca, B_prm], [bca])
            ACT(xot[:, ct, :], ca[:], AF.Silu, [bca, B_prm], [bxo], bias=convb[:, ct:ct + 1], scale=1.0, acc=(ct > 0))
        DMA("sp", BCT_d.rearrange("(c p) t -> p c t", p=128)[:, :, blk * 512:(blk + 1) * 512], xot[:, 16:24, :], bxo, [bxo], NOBUF)
        xs_t, bxs = xsst[blk % 2]
        bt_t, bbt = btst[blk % 2]
        for tt in range(4):
            for grp in range(3):
                nct = 8 if grp < 2 else 4
                pb = ev % 4
                psv = PS[pb][:].bitcast(BF16)
                for i in range(nct):
                    ct = grp * 8 + i
                    TR(psv[:, i * 128:(i + 1) * 128], xot[:, ct, tt * 128:(tt + 1) * 128], CB("ident"), [bxo, B_cstb], [BPS[pb]], acc=(i > 0))
                if grp < 2:
                    dst_ap, bdst = xs_t[:, tt, grp * 1024:(grp + 1) * 1024], bxs
                else:
                    dst_ap, bdst = bt_t[:, tt, :], bbt
                CP("act" if ev % 2 == 0 else "dve", dst_ap, psv[:, 0:nct * 128], [BPS[pb]], [bdst], acc=True)
                ev += 1
        DMA("sp", XS_d.rearrange("(t p) c -> p t c", p=128)[:, blk * 4:(blk + 1) * 4, :], xs_t[:], bxs, [bxs], NOBUF)
        DMA("sp", BTM_d.rearrange("(t p) c -> p t c", p=128)[:, blk * 4:(blk + 1) * 4, :], bt_t[:], bbt, [bbt], NOBUF)
    S.barrier()
    if last_phase < 2:
        return finish(nc, S, out_d)

    phase_begin()
    KTh = [SB("KTh%d" % i, [128, SEQ], BF16) for i in range(2)]
    Qz = [[SB("Qz%d_%d" % (i, m), [128, SEQ], BF16) for m in range(2)] for i in range(2)]
    Vh = [SB("Vh%d" % i, [128, 64, 128], BF16) for i in range(2)]
    pT = [SB("pT%d" % i, [128, 512], BF16) for i in range(6)]
    Osb = [SB("Osb%d" % i, [128, 512], F32) for i in range(4)]
    Lsb = [SB("Lsb%d" % i, [128, 512], F32) for i in range(4)]
    rL = [SB("rL%d" % i, [128, 512], F32) for i in range(2)]
    Aa = [SB("Aa%d" % i, [128, 512], F32) for i in range(2)]
    at32, B_at32 = SB("at32", [128, 512], F32)
    sq32, B_sq32 = SB("sq32", [128, 512], F32)
    rstd, B_rstd = SB("rstd", [128, 512], F32)
    ato = [SB("ato%d" % i, [128, 512], BF16) for i in range(2)]
    V_v = V_d.rearrange("(t p) c -> p t c", p=128)
    for i in range(2):
        MSET("dve", Qz[i][0][0][64:128, :], 0.0, [Qz[i][0][1]])
        MSET("pool", Qz[i][1][0][0:64, :], 0.0, [Qz[i][1][1]])

    def load_head(h):
        kt, bk = KTh[h % 2]
        vt, bv = Vh[h % 2]
        DMA("sp", kt[:], KT_d[h * 128:(h + 1) * 128, :], bk, NOBUF, [bk])
        q0, bq0 = Qz[h % 2][0]
        q1, bq1 = Qz[h % 2][1]
        DMA("sp", q0[0:64, :], QT_d[h * 128:h * 128 + 64, :], bq0, NOBUF, [bq0], acc=True)
        DMA("sp", q1[64:128, :], QT_d[h * 128 + 64:(h + 1) * 128, :], bq1, NOBUF, [bq1], acc=True)
        DMA("sp", vt[:], V_v[:, :, h * 128:(h + 1) * 128], bv, NOBUF, [bv])

    def attn_epilogue_a(h, j, par, epi):
        for m in range(2):
            ls_, bls = Lsb[par * 2 + m]
            os_, bos = Osb[par * 2 + m]
            rl, brl = rL[m]
            aa, baa = Aa[m]
            S.op("dve", (lambda o=rl, i_=ls_: nc.vector.reciprocal(out=o[:], in_=i_[:])), [bls], [brl])
            TT("dve", aa[:], os_[:], rl[:], ALU.mult, [bos, brl], [baa])
        STT(at32[:], Aa[1][0][:], derived[:, 0:1], Aa[0][0][:], ALU.mult, ALU.add, [Aa[1][1], Aa[0][1], B_der], [B_at32])
        TT("dve", sq32[:], at32[:], at32[:], ALU.mult, [B_at32], [B_sq32])

    def attn_epilogue_b(h, j, par, epi):
        MM(PS[7][:, :], C("ones"), sq32[:], True, True, [B_cst, B_sq32], [BPS[7]])
        TS("dve", rstd[:], PS[7][:, :], 1.0 / 128.0, EPS, ALU.mult, ALU.add, [BPS[7]], [B_rstd])
        ACT(rstd[:], rstd[:], AF.Ln, [B_rstd], [B_rstd])
        ACT(rstd[:], rstd[:], AF.Exp, [B_rstd], [B_rstd], scale=-0.5)
        ao, bao = ato[epi % 2]
        STT(ao[:], at32[:], derived[:, 1:2], rstd[:], ALU.mult, ALU.mult, [B_at32, B_rstd, B_der], [bao])
        DMA("sp", AT_d[h * 128:(h + 1) * 128, j * 512:(j + 1) * 512], ao[:], bao, [bao], NOBUF)

    load_head(0)
    pti = 0
    unit = 0
    pending = None
    LOOK = 2
    MASK_ENG = ("dve", "pool")
    for h in range(8):
        if h + 1 < 8:
            load_head(h + 1)
        kt, bk = KTh[h % 2]
        vt, bv = Vh[h % 2]
        for j in range(16):
            par = unit % 2
            tiles = []
            nk = 4 * j + 4
            for kti in range(nk):
                for m in range(2):
                    r = kti - 4 * j
                    tiles.append((m, kti, r if r > 0 else 0, r >= 0, kti == 0, kti == nk - 1))
            nt = len(tiles)

            def qk(i):
                m, kti, r, diag, first, last = tiles[i]
                c0 = 128 * r
                sb_ = i % 3
                qz, bqz = Qz[h % 2][m]
                MM(PS[sb_][:, c0:512], kt[:, kti * 128:(kti + 1) * 128], qz[:, j * 512 + c0:(j + 1) * 512], True, True, [bk, bqz], [BPS[sb_]])

            def pv(i, pslot):
                m, kti, r, diag, first, last = tiles[i]
                c0 = 128 * r
                sb_ = i % 3
                pt, bpt = pT[pslot]
                ACT(pt[:, c0:512], PS[sb_][:, c0:512], AF.Exp, [BPS[sb_]], [bpt], scale=0.125)
                if diag:
                    TT(MASK_ENG[m], pt[:, c0:c0 + 128], pt[:, c0:c0 + 128], CB("triu"), ALU.mult, [bpt, B_cstb], [bpt])
                MM(PS[3 + m][:, c0:512], vt[:, kti, :], pt[:, c0:512], first, last, [bv, bpt], [BPS[3 + m]], acc=(not first))
                MM(PS[5 + m][:, c0:512], CB("ones"), pt[:, c0:512], first, last, [B_cstb, bpt], [BPS[5 + m]], acc=(not first))

            for i in range(min(LOOK, nt)):
                qk(i)
            for i in range(nt):
                if i + LOOK < nt:
                    qk(i + LOOK)
                pv(i, pti % 6)
                pti += 1
                if i == 0 and pending is not None:
                    attn_epilogue_a(*pending)
                if i == nt - 1 and pending is not None:
                    attn_epilogue_b(*pending)
                    pending = None
            for m in range(2):
                os_, bos = Osb[par * 2 + m]
                ls_, bls = Lsb[par * 2 + m]
                CP("dve", os_[:], PS[3 + m][:, :], [BPS[3 + m]], [bos])
                CP("dve", ls_[:], PS[5 + m][:, :], [BPS[5 + m]], [bls])
            pending = (h, j, par, unit)
            unit += 1
    attn_epilogue_a(*pending)
    attn_epilogue_b(*pending)
    S.barrier()
    if last_phase < 3:
        return finish(nc, S, out_d)

    phase_begin()
    xs_b = [SB("xs%d" % i, [128, 2048], BF16) for i in range(2)]
    bt_b = [SB("bt%d" % i, [128, 512], BF16) for i in range(2)]
    bc_b = [SB("bc%d" % i, [128, 8, 128], BF16) for i in range(2)]
    dt_b = [SB("dtr%d" % i, [128, 32], F32) for i in range(2)]
    zs_b = [SB("zs%d" % i, [128, 2048], BF16) for i in range(2)]
    sm_b = [SB("sm%d" % i, [128, 8, 32], F32) for i in range(2)]
    xd_b = [SB("xd%d" % i, [128, 2048], BF16) for i in range(2)]
    xw, B_xw = SB("xw", [128, 2048], BF16)
    xD, B_xD = SB("xD", [128, 2048], F32)
    cbm_b = [[SB("cbm%d_%d" % (j, i), [128, 128], F32) for i in range(4)] for j in range(2)]
    Lh = [SB("Lh%d" % i, [128, 4, 128], F32) for i in range(3)]
    dec = [SB("dec%d" % i, [128, 4, 128], F32) for i in range(3)]
    MT = [SB("MT%d" % i, [128, 4, 128], BF16) for i in range(3)]
    stf, B_stf = SB("stf", [128, 2048], F32)
    stb, B_stb = SB("stb", [128, 2048], BF16)
    t1, B_t1 = SB("t1", [128, 512], F32)
    t2, B_t2 = SB("t2", [128, 512], F32)
    junk, B_junk = SB("junk", [128, 512], F32)
    ssq, B_ssq = SB("ssq", [128, 4], F32)
    yn, B_yn = SB("yn", [128, 512], BF16)
    yTs = [SB("yTs%d" % i, [128, 16, 512], BF16) for i in range(2)]
    MSET("dve", stf[:], 0.0, [B_stf])
    MSET("pool", stb[:], 0.0, [B_stb])
    BCT_v = BCT_d.rearrange("(c p) t -> p c t", p=128)
    YT_v = YT_d.rearrange("(c p) t -> p c t", p=128)

    def load_chunk(c):
        i = c % 2
        DMA("sp", xs_b[i][0][:], XS_d[c * 128:(c + 1) * 128, :], xs_b[i][1], NOBUF, [xs_b[i][1]])
        DMA("sp", bt_b[i][0][:], BTM_d[c * 128:(c + 1) * 128, :], bt_b[i][1], NOBUF, [bt_b[i][1]])
        DMA("sp", bc_b[i][0][:], BCT_v[:, :, c * 128:(c + 1) * 128], bc_b[i][1], NOBUF, [bc_b[i][1]])
        DMA("sp", dt_b[i][0][:], DT_d[c * 128:(c + 1) * 128, :], dt_b[i][1], NOBUF, [dt_b[i][1]])
        DMA("sp", zs_b[i][0][:], ZS_d[c * 128:(c + 1) * 128, :], zs_b[i][1], NOBUF, [zs_b[i][1]])

    def b3(ap32):
        return ap32.unsqueeze(2).to_broadcast([128, 32, 64])

    def prologue(c):
        i_ = c % 2
        xs_, bxs_ = xs_b[i_]
        bc_, bbc_ = bc_b[i_]
        dtr_, bdt_ = dt_b[i_]
        sm_, B_sm_ = sm_b[i_]
        xd_, B_xd_ = xd_b[i_]
        TT("dve", sm_[:, 6, :], dtr_[:], P("dtb"), ALU.add, [bdt_, B_prm], [B_sm_])
        ACT(sm_[:, 6, :], sm_[:, 6, :], AF.Exp, [B_sm_], [B_sm_])
        ACT(sm_[:, 0, :], sm_[:, 6, :], AF.Ln, [B_sm_], [B_sm_], bias=1.0, scale=1.0)
        TT("dve", sm_[:, 1, :], sm_[:, 0, :], derived[:, 8:40], ALU.mult, [B_sm_, B_der], [B_sm_])
        MM(PS[7][:, 0:32], C("triu"), sm_[:, 1, :], True, True, [B_cst, B_sm_], [BPS[7]])
        MM(PS[7][:, 32:64], C("ones"), sm_[:, 1, :], True, True, [B_cst, B_sm_], [BPS[7]], acc=True)
        CP("dve", sm_[:, 2, :], PS[7][:, 0:32], [BPS[7]], [B_sm_])
        ACT(sm_[:, 3, :], PS[7][:, 0:32], AF.Exp, [BPS[7]], [B_sm_])
        TT("dve", sm_[:, 6, :], PS[7][:, 32:64], sm_[:, 2, :], ALU.subtract, [BPS[7], B_sm_], [B_sm_])
        ACT(sm_[:, 4, :], sm_[:, 6, :], AF.Exp, [B_sm_], [B_sm_])
        ACT(sm_[:, 5, :], PS[7][:, 32:64], AF.Exp, [BPS[7]], [B_sm_])
        TT("dve", xd_[:].rearrange("p (h q) -> p h q", h=32), xs_[:].rearrange("p (h q) -> p h q", h=32), b3(sm_[:, 0, :]), ALU.mult, [bxs_, B_sm_], [B_xd_])
        for g_ in range(4):
            MM(PS[6][:, g_ * 128:(g_ + 1) * 128], bc_[:, g_, :], bc_[:, 4 + g_, :], True, True, [bbc_], [BPS[6]], acc=(g_ > 0))
        for g_ in range(4):
            TT("dve", cbm_b[i_][g_][0][:], PS[6][:, g_ * 128:(g_ + 1) * 128], C("triu"), ALU.mult, [BPS[6], B_cst], [cbm_b[i_][g_][1]])

    load_chunk(0)
    prologue(0)
    hc = 0
    for c in range(64):
        if c + 1 < 64:
            load_chunk(c + 1)
        i = c % 2
        xs, bxs = xs_b[i]
        bt, bbt = bt_b[i]
        bc, bbc = bc_b[i]
        dtr, bdt = dt_b[i]
        zs, bzs = zs_b[i]
        sm, B_sm = sm_b[i]
        xd, B_xd = xd_b[i]
        cbm = cbm_b[i]
        xs3 = xs[:].rearrange("p (h q) -> p h q", h=32)
        if c % 4 == 0:
            ys, bys = yTs[(c // 4) % 2]
        def quads(g, hcbox):
            ypb = 4 + (g % 2)
            for qd in range(2):
                h0 = g * 8 + qd * 4
                k3 = hcbox[0] % 3
                lh, blh = Lh[k3]
                de, bde = dec[k3]
                mt, bmt = MT[k3]
                TT("pool", lh[:], C("sl").unsqueeze(1).to_broadcast([128, 4, 128]),
                   sm[:, 1, h0:h0 + 4].unsqueeze(2).to_broadcast([128, 4, 128]), ALU.mult, [B_cst, B_sm], [blh])
                sb_ = hcbox[0] % 2
                for q in range(4):
                    MM(PS[sb_][:, q * 128:(q + 1) * 128], lh[:, q, :], C("triu"), True, True, [blh, B_cst], [BPS[sb_]], acc=(q > 0))
                ACT(de[:], PS[sb_][:, :].rearrange("p (q l) -> p q l", q=4), AF.Exp, [BPS[sb_]], [bde])
                TT("dve", mt[:], de[:], cbm[g][0][:].unsqueeze(1).to_broadcast([128, 4, 128]), ALU.mult, [bde, cbm[g][1]], [bmt])
                for q in range(4):
                    hh = qd * 4 + q
                    hd = h0 + q
                    MM(PS[ypb][:, hh * 64:(hh + 1) * 64], mt[:, q, :], xd[:, hd * 64:(hd + 1) * 64], True, True, [bmt, B_xd], [BPS[ypb]], acc=(hh > 0))
                hcbox[0] += 1
        hcbox = [hc]
        quads(0, hcbox)
        TT("pool", xD[:].rearrange("p (h q) -> p h q", h=32), xs3, b3(P("dsk")), ALU.mult, [bxs, B_prm], [B_xD])
        TT("pool", xw[:].rearrange("p (h q) -> p h q", h=32), xd[:].rearrange("p (h q) -> p h q", h=32), b3(sm[:, 4, :]), ALU.mult, [B_xd, B_sm], [B_xw])
        for g in range(4):
            ypb = 4 + (g % 2)
            if g + 1 < 4:
                quads(g + 1, hcbox)
            if g == 1 and c + 1 < 64:
                prologue(c + 1)
            sb_ = 2 + hc % 2
            hc += 1
            MM(PS[sb_][:, :], bc[:, 4 + g, :], stb[:, g * 512:(g + 1) * 512], True, True, [bbc, B_stb], [BPS[sb_]])
            TT("dve", t1[:].rearrange("p (h q) -> p h q", h=8), PS[sb_][:].rearrange("p (h q) -> p h q", h=8),
               sm[:, 3, g * 8:(g + 1) * 8].unsqueeze(2).to_broadcast([128, 8, 64]), ALU.mult, [BPS[sb_], B_sm], [B_t1])
            TT("dve", t2[:], PS[ypb][:, :], t1[:], ALU.add, [BPS[ypb], B_t1], [B_t2])
            TT("dve", t2[:], t2[:], xD[:, g * 512:(g + 1) * 512], ALU.add, [B_t2, B_xD], [B_t2])
            TT("dve", t2[:], t2[:], zs[:, g * 512:(g + 1) * 512], ALU.mult, [B_t2, bzs], [B_t2])
            ACT(junk[:], t2[:], AF.Square, [B_t2], [B_junk, B_ssq], accum_out=ssq[:, 0:1])
            TS("dve", ssq[:, 1:2], ssq[:, 0:1], 1.0 / 512.0, EPS, ALU.mult, ALU.add, [B_ssq], [B_ssq])
            ACT(ssq[:, 2:3], ssq[:, 1:2], AF.Ln, [B_ssq], [B_ssq])
            ACT(ssq[:, 3:4], ssq[:, 2:3], AF.Exp, [B_ssq], [B_ssq], scale=-0.5)
            a0, a1 = PRM_OFF["ssdw"]
            STT(yn[:], t2[:], ssq[:, 3:4], prm[:, a0 + g * 512:a0 + (g + 1) * 512], ALU.mult, ALU.mult, [B_t2, B_ssq, B_prm], [B_yn])
            tb = 6 + (g % 2) if False else 7
            psv = PS[7][:].bitcast(BF16)
            for q in range(4):
                TR(psv[:, q * 128:(q + 1) * 128], yn[:, q * 128:(q + 1) * 128], CB("ident"), [B_yn, B_cstb], [BPS[7]], acc=(q > 0))
            CP("act", ys[:, g * 4:(g + 1) * 4, (c % 4) * 128:(c % 4 + 1) * 128], psv[:, 0:512].rearrange("p (q t) -> p q t", q=4),
               [BPS[7]], [bys], acc=True)
            sb_ = 2 + hc % 2
            hc += 1
            MM(PS[sb_][:, :], bt[:, g * 128:(g + 1) * 128], xw[:, g * 512:(g + 1) * 512], True, True, [bbt, B_xw], [BPS[sb_]])
            TT("pool", stf[:, g * 512:(g + 1) * 512].rearrange("p (h q) -> p h q", h=8), stf[:, g * 512:(g + 1) * 512].rearrange("p (h q) -> p h q", h=8),
               sm[:, 5, g * 8:(g + 1) * 8].unsqueeze(2).to_broadcast([128, 8, 64]), ALU.mult, [B_stf, B_sm], [B_stf])
            TT("dve", stf[:, g * 512:(g + 1) * 512], stf[:, g * 512:(g + 1) * 512], PS[sb_][:, :], ALU.add, [B_stf, BPS[sb_]], [B_stf])
            CP("act", stb[:, g * 512:(g + 1) * 512], stf[:, g * 512:(g + 1) * 512], [B_stf], [B_stb])
        if c % 4 == 3:
            DMA("sp", YT_v[:, :, (c // 4) * 512:(c // 4 + 1) * 512], ys[:], bys, [bys], NOBUF)
    S.barrier()
    if last_phase < 4:
        return finish(nc, S, out_d)

    phase_begin()
    wba, B_wba = SB("wba", [128, 8, 1024], BF16)
    wbs, B_wbs = SB("wbs", [128, 16, 1024], BF16)
    wo, B_wo = SB("wo", [128, 8, 1024], BF16)
    DMA("pool", wba[:], w_bra_d.rearrange("(k p) c -> p k c", p=128), B_wba, NOBUF, [B_wba])
    DMA("pool", wbs[:], w_brs_d.rearrange("(k p) c -> p k c", p=128), B_wbs, NOBUF, [B_wbs])
    DMA("pool", wo[:], w_out_d.rearrange("(k p) c -> p k c", p=128), B_wo, NOBUF, [B_wo])
    at_b = [SB("at%d" % i, [128, 8, 256], BF16) for i in range(2)]
    yt_b = [SB("yt%d" % i, [128, 16, 256], BF16) for i in range(2)]
    gt_b = [SB("gt%d" % i, [128, 16, 256], BF16) for i in range(2)]
    xr_b = [SB("xr%d" % i, [128, 2, 1024], F32) for i in range(2)]
    m1, B_m1 = SB("m1", [128, 256], F32)
    m2, B_m2 = SB("m2", [128, 256], F32)
    mT, B_mT = SB("mT", [128, 8, 256], BF16)
    h1s = [SB("h1s%d" % i, [128, 1024], F32) for i in range(2)]
    AT_v = AT_d.rearrange("(c p) t -> p c t", p=128)
    GT_v = GT_d.rearrange("(c p) t -> p c t", p=128)
    YT_v2 = YT_d.rearrange("(c p) t -> p c t", p=128)
    x_v = x_d.rearrange("(t p) c -> p t c", p=128)

    def load_p4(ch):
        i = ch % 2
        DMA("sp", at_b[i][0][:], AT_v[:, :, ch * 256:(ch + 1) * 256], at_b[i][1], NOBUF, [at_b[i][1]])
        DMA("sp", yt_b[i][0][:], YT_v2[:, :, ch * 256:(ch + 1) * 256], yt_b[i][1], NOBUF, [yt_b[i][1]])
        DMA("sp", gt_b[i][0][:], GT_v[:, :, ch * 256:(ch + 1) * 256], gt_b[i][1], NOBUF, [gt_b[i][1]])
        DMA("sp", xr_b[i][0][:], x_v[:, ch * 2:(ch + 1) * 2, :], xr_b[i][1], NOBUF, [xr_b[i][1]])

    def layer_norm(dst, src, bsrc, bdst, gname, bname):
        S.op("dve", lambda: nc.vector.bn_stats(out=bst[:, 0:6], in_=src[:, 0:512]), [bsrc], [B_bst])
        S.op("dve", lambda: nc.vector.bn_stats(out=bst[:, 6:12], in_=src[:, 512:1024]), [bsrc], [B_bst], acc=True)
        S.op("dve", lambda: nc.vector.bn_aggr(out=mv[:, 0:2], in_=bst[:]), [B_bst], [B_mv])
        TS("dve", mv[:, 2:3], mv[:, 1:2], EPS, None, ALU.add, None, [B_mv], [B_mv])
        ACT(mv[:, 2:3], mv[:, 2:3], AF.Ln, [B_mv], [B_mv])
        ACT(mv[:, 3:4], mv[:, 2:3], AF.Exp, [B_mv], [B_mv], scale=-0.5)
        TS("dve", src, src, mv[:, 0:1], mv[:, 3:4], ALU.subtract, ALU.mult, [bsrc, B_mv], [bsrc])
        TT("dve", src, src, P(gname), ALU.mult, [bsrc, B_prm], [bsrc])
        TT("dve", dst, src, P(bname), ALU.add, [bsrc, B_prm], [bdst])

    load_p4(0)
    ev = 0
    for ch in range(32):
        if ch + 1 < 32:
            load_p4(ch + 1)
        i = ch % 2
        at, bat = at_b[i]
        yt, byt = yt_b[i]
        gt, bgt = gt_b[i]
        xr, bxr = xr_b[i]
        for dmi in range(8):
            pa = ev % 2
            pbk = 2 + ev % 2
            ev += 1
            for k in range(8):
                MM(PS[pa][:, 0:256], wba[:, k, dmi * 128:(dmi + 1) * 128], at[:, k, :], k == 0, k == 7, [B_wba, bat], [BPS[pa]], acc=(k > 0))
            for k in range(16):
                MM(PS[pbk][:, 0:256], wbs[:, k, dmi * 128:(dmi + 1) * 128], yt[:, k, :], k == 0, k == 15, [B_wbs, byt], [BPS[pbk]], acc=(k > 0))
            TT("dve", m1[:], PS[pa][:, 0:256], gt[:, dmi, :], ALU.mult, [BPS[pa], bgt], [B_m1])
            TT("dve", m2[:], PS[pbk][:, 0:256], gt[:, 8 + dmi, :], ALU.mult, [BPS[pbk], bgt], [B_m2])
            TT("pool", mT[:, dmi, :], m1[:], m2[:], ALU.add, [B_m1, B_m2], [B_mT], acc=(dmi > 0))
        for tt in range(2):
            T = ch * 2 + tt
            for half in range(2):
                pb = 4 + half
                for k in range(8):
                    MM(PS[pb][:, :], mT[:, k, tt * 128:(tt + 1) * 128], wo[:, k, half * 512:(half + 1) * 512], k == 0, k == 7,
                       [B_mT, B_wo], [BPS[pb]], acc=(k > 0))
                STT(xr[:, tt, half * 512:(half + 1) * 512], xr[:, tt, half * 512:(half + 1) * 512], ALPHA, PS[pb][:, :], ALU.mult, ALU.add,
                    [bxr, BPS[pb]], [bxr])
            hs, bhs = h1s[T % 2]
            layer_norm(hs[:], xr[:, tt, :], bxr, bhs, "ln1g", "ln1b")
            DMA("sp", H1_d[T * 128:(T + 1) * 128, :], hs[:], bhs, [bhs], NOBUF)
    S.barrier()
    if last_phase < 5:
        return finish(nc, S, out_d)

    phase_begin()
    wr, B_wr = SB("wr", [128, 8, 256], F32)
    wsg, B_wsg = SB("wsg", [128, 8, 256], BF16)
    wsu, B_wsu = SB("wsu", [128, 8, 256], BF16)
    wsd, B_wsd = SB("wsd", [128, 2, 1024], BF16)
    DMA("sp", wr[:], w_rt_d.rearrange("(k p) c -> p k c", p=128), B_wr, NOBUF, [B_wr])
    DMA("pool", wsg[:], w_sg_d.rearrange("(k p) c -> p k c", p=128), B_wsg, NOBUF, [B_wsg])
    DMA("pool", wsu[:], w_su_d.rearrange("(k p) c -> p k c", p=128), B_wsu, NOBUF, [B_wsu])
    DMA("pool", wsd[:], w_sd_d.rearrange("(k p) c -> p k c", p=128), B_wsd, NOBUF, [B_wsd])
    h1c_b = [SB("h1c%d" % i, [128, 4, 1024], F32) for i in range(2)]
    h1b = [SB("h1b%d" % i, [128, 1024], BF16) for i in range(2)]
    h1T, B_h1T = SB("h1T", [128, 8, 128], F32)
    h1Tb, B_h1Tb = SB("h1Tb", [128, 8, 512], BF16)
    rt, B_rt = SB("rt", [128, 6, 256], F32)
    selcum, B_selcum = SB("selcum", [128, 256], F32)
    rs_, B_rs = SB("rs", [128, 8, 8], F32)
    i8u, B_i8u = SB("i8u", [128, 8], U32)
    rsc, B_rsc = SB("rsc", [128, 4], F32)
    tmpr, B_tmpr = SB("tmpr", [128, 256], F32)
    sgs, B_sgs = SB("sgs", [128, 512], F32)
    hsT, B_hsT = SB("hsT", [128, 2, 512], BF16)
    r2s = [SB("r2s%d" % i, [128, 1024], F32) for i in range(2)]
    MSET("dve", selcum[:], 0.0, [B_selcum])
    H1_v = H1_d.rearrange("(t p) c -> p t c", p=128)

    def load_h1(ch):
        t_, b_ = h1c_b[ch % 2]
        DMA("sp", t_[:], H1_v[:, ch * 4:(ch + 1) * 4, :], b_, NOBUF, [b_])

    load_h1(0)
    for ch in range(16):
        if ch + 1 < 16:
            load_h1(ch + 1)
        h1c, B_h1c = h1c_b[ch % 2]
        for tt in range(4):
            T = ch * 4 + tt
            hb, bhb = h1b[T % 2]
            CP("act", hb[:], h1c[:, tt, :], [B_h1c], [bhb])
            for half in range(2):
                pb = 4 + half
                for q in range(4):
                    k = half * 4 + q
                    TR(PS[pb][:, q * 128:(q + 1) * 128], h1c[:, tt, k * 128:(k + 1) * 128], C("ident"), [B_h1c, B_cst], [BPS[pb]], acc=(q > 0))
                CP("act", h1T[:, half * 4:(half + 1) * 4, :], PS[pb][:, :].rearrange("p (q t) -> p q t", q=4), [BPS[pb]], [B_h1T], acc=(half > 0))
            CP("pool", h1Tb[:, :, tt * 128:(tt + 1) * 128], h1T[:], [B_h1T], [B_h1Tb], acc=True)
            for k in range(8):
                MM(PS[6][:, 0:256], h1T[:, k, :], wr[:, k, :], k == 0, k == 7, [B_h1T, B_wr], [BPS[6]], acc=(k > 0))
            sc = rt[:, 0, :]
            chh = rt[:, 1, :]
            wk2 = rt[:, 2, :]
            sel = rt[:, 3, :]
            wsel = rt[:, 4, :]
            pos = rt[:, 5, :]
            ACT(sc, PS[6][:, 0:256], AF.Sigmoid, [BPS[6]], [B_rt])
            TT("dve", chh, sc, P("rbias"), ALU.add, [B_rt, B_prm], [B_rt])
            ch3 = chh.rearrange("p (g e) -> p g e", g=8)
            wk3 = wk2.rearrange("p (g e) -> p g e", g=8)
            RED(rs_[:, 0, :], ch3, ALU.max, [B_rt], [B_rs])
            TT("dve", wk3, ch3, rs_[:, 0, :].unsqueeze(2).to_broadcast([128, 8, 32]), ALU.is_equal, [B_rt, B_rs], [B_rt])
            STT(wk2, wk2, -1e9, chh, ALU.mult, ALU.add, [B_rt], [B_rt])
            RED(rs_[:, 1, :], wk3, ALU.max, [B_rt], [B_rs])
            TT("dve", rs_[:, 1, :], rs_[:, 1, :], rs_[:, 0, :], ALU.add, [B_rs], [B_rs])
            S.op("dve", lambda: nc.vector.max(out=rs_[:, 2, :], in_=rs_[:, 1, :]), [B_rs], [B_rs])
            TS("dve", rs_[:, 3, :], rs_[:, 1, :], rs_[:, 2, 3:4], None, ALU.is_ge, None, [B_rs], [B_rs])
            TS("dve", rs_[:, 3, :], rs_[:, 3, :], 1e9, -1e9, ALU.mult, ALU.add, [B_rs], [B_rs])
            TT("dve", wk3, ch3, rs_[:, 3, :].unsqueeze(2).to_broadcast([128, 8, 32]), ALU.add, [B_rt, B_rs], [B_rt])
            S.op("dve", lambda: nc.vector.max(out=rs_[:, 7, :], in_=rt[:, 2, :]), [B_rt, B_rs], [B_rs])
            TS("dve", sel, wk2, rs_[:, 7, 7:8], None, ALU.is_ge, None, [B_rt, B_rs], [B_rt])
            TT("dve", wsel, sc, sel, ALU.mult, [B_rt], [B_rt])
            S.op("dve", lambda: nc.vector.max(out=rs_[:, 4, :], in_=rt[:, 4, :]), [B_rt, B_rs], [B_rs])
            S.op("dve", lambda: nc.vector.max_index(out=i8u[:], in_max=rs_[:, 4, :], in_values=rt[:, 4, :]), [B_rt, B_rs], [B_i8u])
            CP("dve", rs_[:, 5, :], i8u[:], [B_i8u], [B_rs])
            RED(rsc[:, 0:1], rs_[:, 4, :], ALU.add, [B_rs], [B_rsc])
            S.op("dve", lambda: nc.vector.reciprocal(out=rsc[:, 1:2], in_=rsc[:, 0:1]), [B_rsc], [B_rsc])
            MM(PS[7][:, 0:256], C("slt"), sel, True, False, [B_cst, B_rt], [BPS[7]])
            MM(PS[7][:, 0:256], C("ones"), selcum[:], False, True, [B_cst, B_selcum], [BPS[7]], acc=True)
            CP("act", pos, PS[7][:, 0:256], [BPS[7]], [B_rt])
            TT("pool", selcum[:], selcum[:], sel, ALU.add, [B_selcum, B_rt], [B_selcum])
            for k in range(8):
                STT(tmpr[:], cst[:, 640:896], rs_[:, 5, k:k + 1], pos, ALU.is_equal, ALU.mult, [B_cst, B_rs, B_rt, B_tmpr], [B_tmpr])
                RED(rs_[:, 6, k:k + 1], tmpr[:], ALU.add, [B_tmpr], [B_rs])
            STT(rs_[:, 7, :], rs_[:, 5, :], float(CAP), rs_[:, 6, :], ALU.mult, ALU.add, [B_rs], [B_rs])
            TS("dve", rs_[:, 3, :], rs_[:, 6, :], CAP - 0.5, 1e6, ALU.is_gt, ALU.mult, [B_rs], [B_rs])
            TT("dve", rs_[:, 7, :], rs_[:, 7, :], rs_[:, 3, :], ALU.max, [B_rs], [B_rs])
            CP("dve", slot_all[:, T, :], rs_[:, 7, :], [B_rs], [B_slot], acc=True)
            TS("dve", rs_[:, 3, :], rs_[:, 6, :], CAP - 0.5, None, ALU.is_lt, None, [B_rs], [B_rs])
            TS("dve", rs_[:, 4, :], rs_[:, 4, :], rsc[:, 1:2], 2.5, ALU.mult, ALU.mult, [B_rs, B_rsc], [B_rs])
            TT("dve", tw_all[:, T, :], rs_[:, 4, :], rs_[:, 3, :], ALU.mult, [B_rs], [B_tw], acc=True)
            for k in range(8):
                S.dma("pool", (lambda T=T, k=k, hb=hb: nc.gpsimd.indirect_dma_start(
                    out=XB_d[:, :], out_offset=bass.IndirectOffsetOnAxis(ap=slot_all[:, T, k:k + 1], axis=0),
                    in_=hb[:, :], in_offset=None, bounds_check=regs["bc"], oob_is_err=False)),
                    bhb, [bhb, B_slot], NOBUF)
        for fh in range(2):
            for k in range(8):
                MM(PS[0][:, :], wsg[:, k, fh * 128:(fh + 1) * 128], h1Tb[:, k, :], k == 0, k == 7, [B_wsg, B_h1Tb], [BPS[0]], acc=(k > 0))
            for k in range(8):
                MM(PS[1][:, :], wsu[:, k, fh * 128:(fh + 1) * 128], h1Tb[:, k, :], k == 0, k == 7, [B_wsu, B_h1Tb], [BPS[1]], acc=(k > 0))
            ACT(sgs[:], PS[0][:, :], AF.Silu, [BPS[0]], [B_sgs])
            TT("dve", hsT[:, fh, :], PS[1][:, :], sgs[:], ALU.mult, [BPS[1], B_sgs], [B_hsT], acc=(fh > 0))
        for tt in range(4):
            T = ch * 4 + tt
            r2, br2 = r2s[T % 2]
            for half in range(2):
                pb = 2 + half
                for fk in range(2):
                    MM(PS[pb][:, :], hsT[:, fk, tt * 128:(tt + 1) * 128], wsd[:, fk, half * 512:(half + 1) * 512], fk == 0, fk == 1,
                       [B_hsT, B_wsd], [BPS[pb]], acc=(fk > 0))
                STT(r2[:, half * 512:(half + 1) * 512], h1c[:, tt, half * 512:(half + 1) * 512], ALPHA, PS[pb][:, :], ALU.mult, ALU.add,
                    [B_h1c, BPS[pb]], [br2], acc=(half > 0))
            DMA("sp", R2_d[T * 128:(T + 1) * 128, :], r2[:], br2, [br2], NOBUF)
    S.barrier()
    if last_phase < 6:
        return finish(nc, S, out_d)

    phase_begin()
    xb_b = [SB("xb%d" % i, [128, 4, 1024], BF16) for i in range(3)]
    wf_b = [SB("wf%d" % i, [128, 6144], F32) for i in range(3)]
    wbf_b = [SB("wbf%d" % i, [128, 6144], BF16) for i in range(3)]
    xbT_b = [SB("xbT%d" % i, [128, 8, 512], BF16) for i in range(2)]
    sg_b = [SB("sg%d" % i, [128, 512], BF16) for i in range(2)]
    hT_b = [SB("hT%d" % i, [128, 2, 512], BF16) for i in range(2)]
    yb_b = [SB("yb%d" % i, [128, 4, 1024], BF16) for i in range(2)]
    XB_v = XB_d.rearrange("(e p t) d -> e p t d", p=128, t=4)
    Y_v = Y_d.rearrange("(e p t) d -> e p t d", p=128, t=4)

    def load_e(e):
        DMA("sp", xb_b[e % 3][0][:], XB_v[e], xb_b[e % 3][1], NOBUF, [xb_b[e % 3][1]])
        wf, bwf = wf_b[e % 3]
        DMA("sp", wf[:, 0:2048], w_eg_d[e].rearrange("(p k) f -> p (k f)", p=128), bwf, NOBUF, [bwf])
        DMA("sp", wf[:, 2048:4096], w_eu_d[e].rearrange("(p k) f -> p (k f)", p=128), bwf, NOBUF, [bwf], acc=True)
        DMA("sp", wf[:, 4096:6144].rearrange("p (k f) -> p k f", k=2), w_ed_d[e].rearrange("(k p) f -> p k f", p=128), bwf, NOBUF, [bwf], acc=True)

    def cast_e(e):
        wf, bwf = wf_b[e % 3]
        wb, bwb = wbf_b[e % 3]
        CP("act", wb[:, 0:2048], wf[:, 0:2048], [bwf], [bwb])
        CP("dve", wb[:, 2048:4096], wf[:, 2048:4096], [bwf], [bwb], acc=True)
        CP("pool", wb[:, 4096:6144], wf[:, 4096:6144], [bwf], [bwb], acc=True)

    def stage_T(e):
        xb, bxb = xb_b[e % 3]
        xbT, bxbT = xbT_b[e % 2]
        for t in range(4):
            pb = t % 2
            psv = PS[pb][:].bitcast(BF16)
            for k in range(8):
                TR(psv[:, k * 128:(k + 1) * 128], xb[:, t, :].rearrange("p (d k) -> p k d", k=8)[:, k, :], CB("ident"), [bxb, B_cstb], [BPS[pb]], acc=(k > 0))
            CP("act" if t % 2 == 0 else "dve", xbT[:, :, t * 128:(t + 1) * 128], psv[:, :].rearrange("p (k s) -> p k s", k=8), [BPS[pb]], [bxbT], acc=(t > 0))

    def stage_GU(e):
        wb, bwb = wbf_b[e % 3]
        xbT, bxbT = xbT_b[e % 2]
        sg, bsg = sg_b[e % 2]
        hT, bhT = hT_b[e % 2]
        weg = wb[:, 0:2048].rearrange("p (k f) -> p k f", k=8)
        weu = wb[:, 2048:4096].rearrange("p (k f) -> p k f", k=8)
        for fh in range(2):
            for k in range(8):
                MM(PS[2 + fh][:, :], weg[:, k, fh * 128:(fh + 1) * 128], xbT[:, k, :], k == 0, k == 7, [bwb, bxbT], [BPS[2 + fh]], acc=(k > 0))
            for k in range(8):
                MM(PS[4 + fh][:, :], weu[:, k, fh * 128:(fh + 1) * 128], xbT[:, k, :], k == 0, k == 7, [bwb, bxbT], [BPS[4 + fh]], acc=(k > 0))
            ACT(sg[:], PS[2 + fh][:, :], AF.Silu, [BPS[2 + fh]], [bsg])
            TT("dve", hT[:, fh, :], PS[4 + fh][:, :], sg[:], ALU.mult, [BPS[4 + fh], bsg], [bhT], acc=(fh > 0))

    def stage_D(e):
        wb, bwb = wbf_b[e % 3]
        hT, bhT = hT_b[e % 2]
        yb, byb = yb_b[e % 2]
        wed = wb[:, 4096:6144].rearrange("p (k f) -> p k f", k=2)
        n6 = 0
        for t in range(4):
            for half in range(2):
                pb = 6 + n6 % 2
                for fk in range(2):
                    MM(PS[pb][:, :], hT[:, fk, t * 128:(t + 1) * 128], wed[:, fk, half * 512:(half + 1) * 512], fk == 0, fk == 1,
                       [bhT, bwb], [BPS[pb]], acc=(fk > 0))
                CP("act" if n6 % 2 == 0 else "dve", yb[:, t, half * 512:(half + 1) * 512], PS[pb][:, :], [BPS[pb]], [byb], acc=(n6 > 0))
                n6 += 1
        DMA("act", Y_v[e], yb[:], byb, [byb], NOBUF)

    for e in range(3):
        load_e(e)
    for i in range(-2, NE):
        if 3 <= i + 4 < NE:
            load_e(i + 4)
        if 0 <= i + 2 < NE:
            cast_e(i + 2)
            stage_T(i + 2)
        if 0 <= i + 1 < NE:
            stage_GU(i + 1)
        if i >= 0:
            stage_D(i)
    S.barrier()
    if last_phase < 7:
        return finish(nc, S, out_d)

    phase_begin()
    r2_b = [SB("r2l%d" % i, [128, 1024], F32) for i in range(2)]
    gk_b = [SB("gk%d" % i, [128, 8, 1024], BF16) for i in range(2)]
    ot_b = [SB("ot%d" % i, [128, 1024], F32) for i in range(2)]
    B_out = Buf("out")

    def load_t(T):
        i = T % 2
        DMA("sp", r2_b[i][0][:], R2_d[T * 128:(T + 1) * 128, :], r2_b[i][1], NOBUF, [r2_b[i][1]])
        gk, bgk = gk_b[i]
        for k in range(8):
            S.dma("pool", (lambda T=T, k=k, gk=gk: nc.gpsimd.indirect_dma_start(
                out=gk[:, k, :], out_offset=None, in_=Y_d[:, :],
                in_offset=bass.IndirectOffsetOnAxis(ap=slot_all[:, T, k:k + 1], axis=0),
                bounds_check=regs["bc"], oob_is_err=False)),
                bgk, [B_slot], [bgk], acc=(k > 0))

    for i in range(2):
        MSET("dve", gk_b[i][0][:], 0.0, [gk_b[i][1]])
    load_t(0)
    for T in range(64):
        if T + 1 < 64:
            load_t(T + 1)
        i = T % 2
        r2, br2 = r2_b[i]
        gk, bgk = gk_b[i]
        ot, bot = ot_b[i]
        for k in range(8):
            STT(r2[:], gk[:, k, :], tw_all[:, T, k:k + 1], r2[:], ALU.mult, ALU.add, [bgk, B_tw, br2], [br2])
        layer_norm(ot[:], r2[:], br2, bot, "ln2g", "ln2b")
        DMA("sp", out_d[T * 128:(T + 1) * 128, :], ot[:], bot, [bot], [B_out], acc=True)
    S.barrier(release=False)
    return finish(nc, S, out_d)


def finish(nc, S, out_d):
    S.barrier(release=False)
    S.emit()
    return nc


def make_consts():
    p = np.arange(128)[:, None]
    f = np.arange(128)[None, :]
    cst = np.zeros((128, 896), np.float32)
    cst[:, 0:128] = (p == f)
    cst[:, 128:256] = (p <= f)
    cst[:, 256:384] = (p > f)
    cst[:, 384:512] = (p < f)
    cst[:, 512:640] = 1.0
    cst[:, 640:896] = np.arange(256, dtype=np.float32)[None, :]
    tokid = (np.arange(64)[None, :] * 128 + np.arange(128)[:, None]).astype(np.int32)
    return cst, tokid


def make_prm(inp):
    prm = np.zeros((128, PRM_W), np.float32)

    def put(name, arr):
        a, b = PRM_OFF[name]
        prm[:, a:b] = arr

    put("lam4", np.concatenate([inp["lambda_q1"][0], inp["lambda_k1"][0], inp["lambda_q2"][0], inp["lambda_k2"][0]])[None, :])
    put("subln", inp["attn_subln_w"][0][:, None])
    cw = inp["conv_w"][0]
    put("convw", cw.reshape(4, 24, 128).transpose(2, 1, 0).reshape(128, 96))
    put("convb", inp["conv_b"][0].reshape(24, 128).T)
    put("dtb", inp["dt_bias"][0][None, :])
    put("alog", inp["a_log"][0][None, :])
    put("dsk", inp["d_skip"][0][None, :])
    put("rbias", inp["router_bias"][0][None, :])
    put("ssdw", inp["ssd_norm_w"][0][None, :])
    put("ln1g", inp["ln1_g"][0][None, :])
    put("ln1b", inp["ln1_b"][0][None, :])
    put("ln2g", inp["ln2_g"][0][None, :])
    put("ln2b", inp["ln2_b"][0][None, :])
    return prm


def make_in_maps(inp, batches):
    cst, tokid = make_consts()
    prm = make_prm(inp)
    shared = {
        "w_in": np.ascontiguousarray(inp["w_in"][0]), "cst": cst, "prm": prm, "tokid": tokid,
        "w_br_attn": np.ascontiguousarray(inp["w_br_attn"][0]), "w_br_ssd": np.ascontiguousarray(inp["w_br_ssd"][0]),
        "w_out": np.ascontiguousarray(inp["w_out"][0]), "w_router": np.ascontiguousarray(inp["w_router"][0]),
        "w_sh_gate": np.ascontiguousarray(inp["w_sh_gate"][0]), "w_sh_up": np.ascontiguousarray(inp["w_sh_up"][0]),
        "w_sh_down": np.ascontiguousarray(inp["w_sh_down"][0]),
        "w_exp_gate": np.ascontiguousarray(inp["w_exp_gate"][0]), "w_exp_up": np.ascontiguousarray(inp["w_exp_up"][0]),
        "w_exp_down": np.ascontiguousarray(inp["w_exp_down"][0]),
    }
    maps = []
    for b in batches:
        m = dict(shared)
        xb = np.ascontiguousarray(inp["x"][b])
        m["x"] = xb
        m["xT"] = np.ascontiguousarray(xb.T)
        maps.append(m)
    return maps


def kernel(**inputs):
    inp = {k: np.asarray(v) for k, v in inputs.items()}
    nc = build_program()
    maps = make_in_maps(inp, list(range(8)))
    res = run_bass_kernel_spmd(nc, maps, core_ids=list(range(8)))
    out = np.stack([np.asarray(r["out"]) for r in res.results], axis=0)
    return out.astype(np.float32)
```

```python
import os
import numpy as np
import concourse.bass as bass
import concourse.mybir as mybir
from concourse.bass_utils import run_bass_kernel_spmd

F32 = mybir.dt.float32
BF16 = mybir.dt.bfloat16
I32 = mybir.dt.int32
U32 = mybir.dt.uint32
AF = mybir.ActivationFunctionType
ALU = mybir.AluOpType
AX = mybir.AxisListType

SEQ = 8192
DM = 1024
NE = 256
CAP = 512
ALPHA = 2.0 ** 0.25
LAM_INIT = 0.2
EPS = 1e-5
DTSZ = {F32: 4, BF16: 2, I32: 4, U32: 4}


class Buf:
    __slots__ = ("name", "writers", "readers", "dsem", "dcnt", "dkey", "excl", "last_dma")

    def __init__(self, name, excl=False):
        self.name = name
        self.writers = []
        self.readers = []
        self.dsem = None
        self.dcnt = 0
        self.dkey = None
        self.excl = excl
        self.last_dma = None


class Ins:
    __slots__ = ("eng", "order", "fn", "waits", "needed", "sem", "val", "is_dma", "key")


class Sched:
    ENGS = ("pe", "act", "dve", "pool", "sp")

    def __init__(self, nc):
        self.nc = nc
        self.prog = {e: [] for e in self.ENGS}
        self.seen = {e: {} for e in self.ENGS}
        self.esem = {e: nc.alloc_semaphore("e_" + e) for e in self.ENGS}
        self.slots = []
        self.free_dsems = []
        self.nds = 0

    def _deps(self, eng, reads, writes, own_key=None):
        deps = {}

        def add(d):
            if d.eng == eng and not d.is_dma and eng == "pe":
                return
            if own_key is not None and d.is_dma and d.key == own_key:
                return
            cur = deps.get(d.key)
            if cur is None or cur.order < d.order:
                deps[d.key] = d

        for b in reads:
            for d in b.writers:
                add(d)
            if b.excl:
                for d in b.readers:
                    add(d)
        for b in writes:
            for d in b.writers:
                add(d)
            for d in b.readers:
                add(d)
        waits = []
        seen = self.seen[eng]
        for key, d in deps.items():
            if seen.get(key, -1) >= d.order:
                continue
            seen[key] = d.order
            d.needed = True
            waits.append(d)
        return waits

    def _commit(self, ins, reads, writes, acc):
        for b in writes:
            if acc:
                b.writers.append(ins)
                if len(b.writers) > 48:
                    b.writers = self._compress(b.writers)
            else:
                b.writers = [ins]
                b.readers = []
        for b in reads:
            b.readers.append(ins)
            if len(b.readers) > 48:
                b.readers = self._compress(b.readers)

    @staticmethod
    def _compress(lst):
        last = {}
        for r in lst:
            c = last.get(r.key)
            if c is None or c.order < r.order:
                last[r.key] = r
        return list(last.values())

    def op(self, eng, fn, reads=(), writes=(), acc=False):
        ins = Ins()
        ins.eng = eng
        ins.is_dma = False
        ins.key = eng
        ins.order = len(self.prog[eng])
        ins.fn = fn
        ins.needed = False
        ins.sem = self.esem[eng]
        ins.val = None
        ins.waits = self._deps(eng, reads, writes)
        self.prog[eng].append(ins)
        self._commit(ins, reads, writes, acc)
        return ins

    def dma(self, eng, fn, slot, reads=(), writes=(), acc=False):
        if slot.dsem is None:
            if self.free_dsems:
                slot.dsem, slot.dcnt, slot.dkey = self.free_dsems.pop()
            else:
                self.nds += 1
                slot.dkey = "ds%d" % self.nds
                slot.dsem = self.nc.alloc_semaphore(slot.dkey)
                slot.dcnt = 0
            self.slots.append(slot)
        ins = Ins()
        ins.eng = eng
        ins.is_dma = True
        ins.key = slot.dkey
        slot.dcnt += 1
        ins.order = slot.dcnt
        ins.fn = fn
        ins.needed = True
        ins.sem = slot.dsem
        ins.val = 16 * slot.dcnt
        ins.waits = self._deps(eng, reads, writes, own_key=(slot.dkey if acc else None))
        self.prog[eng].append(ins)
        self._commit(ins, reads, writes, acc)
        slot.last_dma = ins
        return ins

    def barrier(self, release=True):
        lasts = []
        for e in self.ENGS:
            for ins in reversed(self.prog[e]):
                if ins.fn is not None and not ins.is_dma:
                    lasts.append(ins)
                    break
        for s in self.slots:
            if s.last_dma is not None:
                lasts.append(s.last_dma)
        for e in self.ENGS:
            waits = []
            seen = self.seen[e]
            for d in lasts:
                if d.eng == e and not d.is_dma:
                    continue
                if seen.get(d.key, -1) >= d.order:
                    continue
                seen[d.key] = d.order
                d.needed = True
                waits.append(d)
            ins = Ins()
            ins.eng = e
            ins.is_dma = False
            ins.key = e
            ins.order = len(self.prog[e])
            ins.fn = None
            ins.needed = False
            ins.sem = self.esem[e]
            ins.val = None
            ins.waits = waits
            self.prog[e].append(ins)
        if release:
            for s in self.slots:
                self.free_dsems.append((s.dsem, s.dcnt, s.dkey))
                s.dsem = None
            self.slots = []

    def emit(self):
        nc = self.nc
        for e in self.ENGS:
            c = 0
            for ins in self.prog[e]:
                if not ins.is_dma and ins.needed:
                    c += 1
                    ins.val = c
        engobj = {"pe": nc.tensor, "act": nc.scalar, "dve": nc.vector, "pool": nc.gpsimd, "sp": nc.sync}

        def run(e):
            eo = engobj[e]
            for ins in self.prog[e]:
                for w in ins.waits:
                    eo.wait_ge(w.sem, w.val)
                if ins.fn is None:
                    continue
                r = ins.fn()
                if ins.is_dma:
                    r.then_inc(ins.sem, 16)
                elif ins.needed:
                    r.then_inc(ins.sem, 1)

        with nc.Block() as block:
            @block.tensor
            def _(eng):
                run("pe")

            @block.scalar
            def _(eng):
                run("act")

            @block.vector
            def _(eng):
                run("dve")

            @block.gpsimd
            def _(eng):
                run("pool")

            @block.sync
            def _(eng):
                run("sp")


CST_COLS = dict(ident=(0, 128), triu=(128, 256), sl=(256, 384), slt=(384, 512), ones=(512, 640), iota=(640, 896))
PRM_LAYOUT = [("lam4", 256), ("subln", 1), ("convw", 96), ("convb", 24), ("dtb", 32), ("alog", 32), ("dsk", 32),
              ("rbias", 256), ("ssdw", 2048), ("ln1g", 1024), ("ln1b", 1024), ("ln2g", 1024), ("ln2b", 1024)]
PRM_OFF = {}
_o = 0
for _n, _w in PRM_LAYOUT:
    PRM_OFF[_n] = (_o, _o + _w)
    _o += _w
PRM_W = _o


def build_program(last_phase=9, debug=False):
    nc = bass.Bass("TRN2", target_bir_lowering=False)
    S = Sched(nc)
    eng = {"pe": nc.tensor, "act": nc.scalar, "dve": nc.vector, "pool": nc.gpsimd, "sp": nc.sync}

    def DIN(name, shape, dt):
        return nc.dram_tensor(name, list(shape), dt, kind="ExternalInput").ap()

    def DSCR(name, shape, dt, dbg=False):
        kind = "ExternalOutput" if (dbg and debug) else "Internal"
        return nc.dram_tensor(name, list(shape), dt, kind=kind).ap()

    xT_d = DIN("xT", [DM, SEQ], F32)
    x_d = DIN("x", [SEQ, DM], F32)
    w_in_d = DIN("w_in", [DM, 10272], F32)
    cst_d = DIN("cst", [128, 896], F32)
    prm_d = DIN("prm", [128, PRM_W], F32)
    tokid_d = DIN("tokid", [128, 64], I32)
    w_bra_d = DIN("w_br_attn", [1024, 1024], F32)
    w_brs_d = DIN("w_br_ssd", [2048, 1024], F32)
    w_out_d = DIN("w_out", [1024, 1024], F32)
    w_rt_d = DIN("w_router", [1024, 256], F32)
    w_sg_d = DIN("w_sh_gate", [1024, 256], F32)
    w_su_d = DIN("w_sh_up", [1024, 256], F32)
    w_sd_d = DIN("w_sh_down", [256, 1024], F32)
    w_eg_d = DIN("w_exp_gate", [NE, 1024, 256], F32)
    w_eu_d = DIN("w_exp_up", [NE, 1024, 256], F32)
    w_ed_d = DIN("w_exp_down", [NE, 256, 1024], F32)
    out_d = nc.dram_tensor("out", [SEQ, DM], F32, kind="ExternalOutput").ap()

    QT_d = DSCR("QT", [1024, SEQ], BF16, dbg=True)
    KT_d = DSCR("KT", [1024, SEQ], BF16)
    V_d = DSCR("V", [SEQ, 1024], BF16, dbg=True)
    ZS_d = DSCR("ZS", [SEQ, 2048], BF16, dbg=True)
    XBCT_d = DSCR("XBCT", [3072, SEQ], BF16)
    DT_d = DSCR("DT", [SEQ, 32], F32, dbg=True)
    GT_d = DSCR("GT", [2048, SEQ], BF16, dbg=True)
    XS_d = DSCR("XS", [SEQ, 2048], BF16, dbg=True)
    BTM_d = DSCR("BTM", [SEQ, 512], BF16, dbg=True)
    BCT_d = DSCR("BCT", [1024, SEQ], BF16, dbg=True)
    AT_d = DSCR("AT", [1024, SEQ], BF16, dbg=True)
    YT_d = DSCR("YT", [2048, SEQ], BF16, dbg=True)
    R2_d = DSCR("R2", [SEQ, DM], F32, dbg=True)
    H1_d = DSCR("H1", [SEQ, DM], F32, dbg=True)
    XB_d = DSCR("XB", [NE * CAP, DM], BF16)
    Y_d = DSCR("Y", [NE * CAP, DM], BF16)

    def A(name, shape, dt):
        return nc.alloc_sbuf_tensor("s_" + name, shape, dt)
    cst = A("cst", [128, 896], F32)
    prm = A("prm", [128, PRM_W], F32)
    cstb = A("cstb", [128, 640], BF16)
    tokid = A("tokid", [128, 64], I32)
    slot_all = A("slot_all", [128, 64, 8], I32)
    tw_all = A("tw_all", [128, 64, 8], F32)
    derived = A("derived", [128, 40], F32)
    B_cst, B_prm, B_cstb, B_tokid = Buf("cst"), Buf("prm"), Buf("cstb"), Buf("tokid")
    B_slot, B_tw, B_der = Buf("slot_all"), Buf("tw_all"), Buf("derived")
    bst = A("bst", [128, 12], F32)
    mv = A("mv", [128, 4], F32)
    B_bst, B_mv = Buf("bst"), Buf("mv")

    def C(name):
        a, b = CST_COLS[name]
        return cst[:, a:b]

    def CB(name):
        a, b = CST_COLS[name]
        return cstb[:, a:b]

    def P(name):
        a, b = PRM_OFF[name]
        return prm[:, a:b]

    PS = [nc.alloc_psum_tensor("psb%d" % i, [128, 512], F32) for i in range(8)]
    BPS = [Buf("psb%d" % i, excl=True) for i in range(8)]

    arena_base = (nc.sbuf_base + 63) // 64 * 64
    arena_top = nc.sbuf_top
    st = {"ptr": arena_base, "n": 0}

    def phase_begin():
        st["ptr"] = arena_base

    def SB(name, shape, dt, excl=False):
        sz = DTSZ[dt]
        for s_ in shape[1:]:
            sz *= s_
        sz = (sz + 63) // 64 * 64
        off = st["ptr"]
        assert off + sz <= arena_top, ("SBUF arena overflow", name, off + sz - arena_top)
        st["ptr"] = off + sz
        st["n"] += 1
        t = nc.alloc_sbuf_tensor_at("%s_%d" % (name, st["n"]), list(shape), dt, offset=off)
        return t, Buf(name)

    def MM(out, lhsT, rhs, start, stop, R, W, acc=False):
        S.op("pe", lambda: nc.tensor.matmul(out, lhsT=lhsT, rhs=rhs, start=start, stop=stop), R, W, acc)

    def TR(out, in_, ident, R, W, acc=False):
        S.op("pe", lambda: nc.tensor.transpose(out=out, in_=in_, identity=ident), R, W, acc)

    def ACT(out, in_, func, R, W, bias=None, scale=None, accum_out=None, acc=False):
        kw = {}
        if bias is not None:
            kw["bias"] = bias
        if scale is not None:
            kw["scale"] = scale
        if accum_out is not None:
            kw["accum_out"] = accum_out
        S.op("act", lambda: nc.scalar.activation(out=out, in_=in_, func=func, **kw), R, W, acc)

    def CP(e, out, in_, R, W, acc=False):
        if e == "act":
            S.op("act", lambda: nc.scalar.copy(out=out, in_=in_), R, W, acc)
        else:
            S.op(e, lambda: eng[e].tensor_copy(out=out, in_=in_), R, W, acc)

    def TT(e, out, in0, in1, op, R, W, acc=False):
        S.op(e, lambda: eng[e].tensor_tensor(out=out, in0=in0, in1=in1, op=op), R, W, acc)

    def TS(e, out, in0, s1, s2, op0, op1, R, W, acc=False):
        if s2 is None:
            S.op(e, lambda: eng[e].tensor_scalar(out=out, in0=in0, scalar1=s1, scalar2=None, op0=op0), R, W, acc)
        else:
            S.op(e, lambda: eng[e].tensor_scalar(out=out, in0=in0, scalar1=s1, scalar2=s2, op0=op0, op1=op1), R, W, acc)

    def STT(out, in0, scalar, in1, op0, op1, R, W, acc=False):
        S.op("dve", lambda: nc.vector.scalar_tensor_tensor(out=out, in0=in0, scalar=scalar, in1=in1, op0=op0, op1=op1), R, W, acc)

    def RED(out, in_, op, R, W, acc=False):
        S.op("dve", lambda: nc.vector.tensor_reduce(out=out, in_=in_, axis=AX.X, op=op), R, W, acc)

    def MSET(e, ap, val, W, acc=False):
        S.op(e, lambda: eng[e].memset(ap, val), (), W, acc)

    def DMA(q, out, in_, slot, R, W, acc=False):
        S.dma(q, lambda: eng[q].dma_start(out=out, in_=in_), slot, R, W, acc)

    NOBUF = ()
    regs = {}

    def _init_pool_regs():
        regs["bc"] = nc.gpsimd.alloc_register("bc_reg")
        return nc.gpsimd.reg_mov(regs["bc"], NE * CAP - 1)

    S.op("pool", _init_pool_regs)

    DMA("sp", cst[:], cst_d, B_cst, NOBUF, [B_cst])
    DMA("sp", prm[:], prm_d, B_prm, NOBUF, [B_prm])
    DMA("sp", tokid[:], tokid_d, B_tokid, NOBUF, [B_tokid])
    CP("act", cstb[:], cst[:, 0:640], [B_cst], [B_cstb])
    l4 = P("lam4")
    phase_begin()
    tmpA, B_tmpA = SB("tmpA", [128, 64], F32)
    tmpS, B_tmpS = SB("tmpS", [128, 4], F32)
    TT("dve", tmpA[:], l4[:, 0:64], l4[:, 64:128], ALU.mult, [B_prm], [B_tmpA])
    RED(tmpS[:, 0:1], tmpA[:], ALU.add, [B_tmpA], [B_tmpS])
    TT("dve", tmpA[:], l4[:, 128:192], l4[:, 192:256], ALU.mult, [B_prm, B_tmpS], [B_tmpA])
    RED(tmpS[:, 1:2], tmpA[:], ALU.add, [B_tmpA], [B_tmpS], acc=True)
    ACT(tmpS[:, 2:4], tmpS[:, 0:2], AF.Exp, [B_tmpS], [B_tmpS])
    TS("dve", derived[:, 0:1], tmpS[:, 3:4], tmpS[:, 2:3], -LAM_INIT, ALU.subtract, ALU.add, [B_tmpS], [B_der])
    TS("dve", derived[:, 1:2], P("subln"), 1.0 - LAM_INIT, None, ALU.mult, None, [B_prm], [B_der], acc=True)
    ACT(derived[:, 8:40], P("alog"), AF.Exp, [B_prm], [B_der], acc=True)
    TS("dve", derived[:, 8:40], derived[:, 8:40], -1.0, None, ALU.mult, None, [B_der], [B_der])
    S.barrier()

    phase_begin()
    xTb, B_xTb = SB("xTb", [128, 8, SEQ], BF16)
    zero_t, B_zero = SB("zero", [128, 8192], BF16)
    Wb = [SB("Wb%d" % i, [128, 8, 512], BF16) for i in range(2)]
    stg = [SB("stg%d" % i, [128, 512], BF16) for i in range(4)]
    stgf = [SB("stgf%d" % i, [128, 32], F32) for i in range(2)]
    MSET("dve", zero_t[:], 0.0, [B_zero])
    for k in range(8):
        for hh in range(2):
            DMA("pool", xTb[:, k, hh * 4096:(hh + 1) * 4096], xT_d[k * 128:(k + 1) * 128, hh * 4096:(hh + 1) * 4096],
                B_xTb, NOBUF, [B_xTb], acc=True)
    XBz = XB_d.rearrange("(n p r) d -> n p (r d)", p=128, r=8)
    for n in range(NE * CAP // 1024):
        DMA("sp", XBz[n], zero_t[:], B_zero, [B_zero], NOBUF)

    w_in_v = w_in_d.rearrange("(k p) c -> p k c", p=128)
    blocks = []
    for i in range(2):
        blocks.append((i * 512, 512, "FM", QT_d, i * 512, None))
    for i in range(2):
        blocks.append((1024 + i * 512, 512, "FM", KT_d, i * 512, None))
    for i in range(2):
        blocks.append((2048 + i * 512, 512, "TM", V_d, i * 512, None))
    for i in range(4):
        blocks.append((3072 + i * 512, 512, "TM", ZS_d, i * 512, AF.Silu))
    for i in range(6):
        blocks.append((5120 + i * 512, 512, "FM", XBCT_d, i * 512, None))
    blocks.append((8192, 32, "TMF", DT_d, 0, None))
    for i in range(4):
        blocks.append((8224 + i * 512, 512, "FM", GT_d, i * 512, AF.Sigmoid))

    def load_w(bi):
        c0, ncol = blocks[bi][0], blocks[bi][1]
        wt, wbuf = Wb[bi % 2]
        DMA("pool", wt[:, :, 0:ncol], w_in_v[:, :, c0:c0 + ncol], wbuf, NOBUF, [wbuf])

    load_w(0)
    ev = 0
    for bi, (c0, ncol, kind, dst, dc0, func) in enumerate(blocks):
        if bi + 1 < len(blocks):
            load_w(bi + 1)
        wt, wbuf = Wb[bi % 2]
        if kind == "FM":
            for ct in range(ncol // 128):
                for tc in range(16):
                    pb = ev % 4
                    for k in range(8):
                        MM(PS[pb][:, :], wt[:, k, ct * 128:(ct + 1) * 128], xTb[:, k, tc * 512:(tc + 1) * 512],
                           k == 0, k == 7, [wbuf, B_xTb], [BPS[pb]], acc=(k > 0))
                    sg, bsg = stg[ev % 4]
                    if func is not None:
                        ACT(sg[:], PS[pb][:, :], func, [BPS[pb]], [bsg])
                    elif ev % 2 == 0:
                        CP("act", sg[:], PS[pb][:, :], [BPS[pb]], [bsg])
                    else:
                        CP("dve", sg[:], PS[pb][:, :], [BPS[pb]], [bsg])
                    DMA("sp", dst[dc0 + ct * 128: dc0 + (ct + 1) * 128, tc * 512:(tc + 1) * 512], sg[:], bsg, [bsg], NOBUF)
                    ev += 1
        else:
            for tt in range(64):
                pb = ev % 4
                for k in range(8):
                    MM(PS[pb][:, 0:ncol], xTb[:, k, tt * 128:(tt + 1) * 128], wt[:, k, 0:ncol],
                       k == 0, k == 7, [wbuf, B_xTb], [BPS[pb]], acc=(k > 0))
                if kind == "TMF":
                    sg, bsg = stgf[ev % 2]
                    CP("dve", sg[:], PS[pb][:, 0:ncol], [BPS[pb]], [bsg])
                    DMA("sp", dst[tt * 128:(tt + 1) * 128, :], sg[:], bsg, [bsg], NOBUF)
                else:
                    sg, bsg = stg[ev % 4]
                    if func is not None:
                        ACT(sg[:], PS[pb][:, :], func, [BPS[pb]], [bsg])
                    elif ev % 2 == 0:
                        CP("act", sg[:], PS[pb][:, :], [BPS[pb]], [bsg])
                    else:
                        CP("dve", sg[:], PS[pb][:, :], [BPS[pb]], [bsg])
                    DMA("sp", dst[tt * 128:(tt + 1) * 128, dc0:dc0 + 512], sg[:], bsg, [bsg], NOBUF)
                ev += 1
    S.barrier()

    phase_begin()
    xin = [SB("xin%d" % i, [128, 24, 515], BF16) for i in range(2)]
    cacc = [SB("cacc%d" % i, [128, 512], F32) for i in range(2)]
    xo = [SB("xo%d" % i, [128, 24, 512], BF16) for i in range(2)]
    xsst = [SB("xsst%d" % i, [128, 4, 2048], BF16) for i in range(2)]
    btst = [SB("btst%d" % i, [128, 4, 512], BF16) for i in range(2)]
    XBCT_v = XBCT_d.rearrange("(c p) t -> p c t", p=128)
    convw = P("convw")
    convb = P("convb")

    def load_xin(blk):
        t, b = xin[blk % 2]
        if blk == 0:
            MSET("dve", t[:, :, 0:3], 0.0, [b])
            DMA("sp", t[:, :, 3:515], XBCT_v[:, :, 0:512], b, NOBUF, [b], acc=True)
        else:
            DMA("sp", t[:, :, :], XBCT_v[:, :, blk * 512 - 3: blk * 512 + 512], b, NOBUF, [b])

    load_xin(0)
    ev = 0
    for blk in range(16):
        if blk + 1 < 16:
            load_xin(blk + 1)
        xi, bxi = xin[blk % 2]
        xot, bxo = xo[blk % 2]
        for ct in range(24):
            ca, bca = cacc[ct % 2]
            TS("dve", ca[:], xi[:, ct, 0:512], convw[:, ct * 4:ct * 4 + 1], None, ALU.mult, None, [bxi, B_prm], [bca])
            for j in range(1, 4):
                STT(ca[:], xi[:, ct, j:j + 512], convw[:, ct * 4 + j:ct * 4 + j + 1], ca[:], ALU.mult, ALU.add, [bxi, bca, B_prm], [bca])
            ACT(xot[:, ct, :], ca[:], AF.Silu, [bca, B_prm], [bxo], bias=convb[:, ct:ct + 1], scale=1.0, acc=(ct > 0))
        DMA("sp", BCT_d.rearrange("(c p) t -> p c t", p=128)[:, :, blk * 512:(blk + 1) * 512], xot[:, 16:24, :], bxo, [bxo], NOBUF)
        xs_t, bxs = xsst[blk % 2]
        bt_t, bbt = btst[blk % 2]
        for tt in range(4):
            for grp in range(3):
                nct = 8 if grp < 2 else 4
                pb = ev % 4
                psv = PS[pb][:].bitcast(BF16)
                for i in range(nct):
                    ct = grp * 8 + i
                    TR(psv[:, i * 128:(i + 1) * 128], xot[:, ct, tt * 128:(tt + 1) * 128], CB("ident"), [bxo, B_cstb], [BPS[pb]], acc=(i > 0))
                if grp < 2:
                    dst_ap, bdst = xs_t[:, tt, grp * 1024:(grp + 1) * 1024], bxs
                else:
                    dst_ap, bdst = bt_t[:, tt, :], bbt
                CP("act" if ev % 2 == 0 else "dve", dst_ap, psv[:, 0:nct * 128], [BPS[pb]], [bdst], acc=True)
                ev += 1
        DMA("sp", XS_d.rearrange("(t p) c -> p t c", p=128)[:, blk * 4:(blk + 1) * 4, :], xs_t[:], bxs, [bxs], NOBUF)
        DMA("sp", BTM_d.rearrange("(t p) c -> p t c", p=128)[:, blk * 4:(blk + 1) * 4, :], bt_t[:], bbt, [bbt], NOBUF)
    S.barrier()
    if last_phase < 2:
        return finish(nc, S, out_d)

    phase_begin()
    KTh = [SB("KTh%d" % i, [128, SEQ], BF16) for i in range(2)]
    Qz = [[SB("Qz%d_%d" % (i, m), [128, SEQ], BF16) for m in range(2)] for i in range(2)]
    Vh = [SB("Vh%d" % i, [128, 64, 128], BF16) for i in range(2)]
    pT = [SB("pT%d" % i, [128, 512], BF16) for i in range(6)]
    Osb = [SB("Osb%d" % i, [128, 512], F32) for i in range(4)]
    Lsb = [SB("Lsb%d" % i, [128, 512], F32) for i in range(4)]
    rL = [SB("rL%d" % i, [128, 512], F32) for i in range(2)]
    Aa = [SB("Aa%d" % i, [128, 512], F32) for i in range(2)]
    at32, B_at32 = SB("at32", [128, 512], F32)
    sq32, B_sq32 = SB("sq32", [128, 512], F32)
    rstd, B_rstd = SB("rstd", [128, 512], F32)
    ato = [SB("ato%d" % i, [128, 512], BF16) for i in range(2)]
    V_v = V_d.rearrange("(t p) c -> p t c", p=128)
    for i in range(2):
        MSET("dve", Qz[i][0][0][64:128, :], 0.0, [Qz[i][0][1]])
        MSET("pool", Qz[i][1][0][0:64, :], 0.0, [Qz[i][1][1]])

    def load_head(h):
        kt, bk = KTh[h % 2]
        vt, bv = Vh[h % 2]
        DMA("sp", kt[:], KT_d[h * 128:(h + 1) * 128, :], bk, NOBUF, [bk])
        q0, bq0 = Qz[h % 2][0]
        q1, bq1 = Qz[h % 2][1]
        DMA("sp", q0[0:64, :], QT_d[h * 128:h * 128 + 64, :], bq0, NOBUF, [bq0], acc=True)
        DMA("sp", q1[64:128, :], QT_d[h * 128 + 64:(h + 1) * 128, :], bq1, NOBUF, [bq1], acc=True)
        DMA("sp", vt[:], V_v[:, :, h * 128:(h + 1) * 128], bv, NOBUF, [bv])

    def attn_epilogue_a(h, j, par, epi):
        for m in range(2):
            ls_, bls = Lsb[par * 2 + m]
            os_, bos = Osb[par * 2 + m]
            rl, brl = rL[m]
            aa, baa = Aa[m]
            S.op("dve", (lambda o=rl, i_=ls_: nc.vector.reciprocal(out=o[:], in_=i_[:])), [bls], [brl])
            TT("dve", aa[:], os_[:], rl[:], ALU.mult, [bos, brl], [baa])
        STT(at32[:], Aa[1][0][:], derived[:, 0:1], Aa[0][0][:], ALU.mult, ALU.add, [Aa[1][1], Aa[0][1], B_der], [B_at32])
        TT("dve", sq32[:], at32[:], at32[:], ALU.mult, [B_at32], [B_sq32])

    def attn_epilogue_b(h, j, par, epi):
        MM(PS[7][:, :], C("ones"), sq32[:], True, True, [B_cst, B_sq32], [BPS[7]])
        TS("dve", rstd[:], PS[7][:, :], 1.0 / 128.0, EPS, ALU.mult, ALU.add, [BPS[7]], [B_rstd])
        ACT(rstd[:], rstd[:], AF.Ln, [B_rstd], [B_rstd])
        ACT(rstd[:], rstd[:], AF.Exp, [B_rstd], [B_rstd], scale=-0.5)
        ao, bao = ato[epi % 2]
        STT(ao[:], at32[:], derived[:, 1:2], rstd[:], ALU.mult, ALU.mult, [B_at32, B_rstd, B_der], [bao])
        DMA("sp", AT_d[h * 128:(h + 1) * 128, j * 512:(j + 1) * 512], ao[:], bao, [bao], NOBUF)

    load_head(0)
    pti = 0
    unit = 0
    pending = None
    LOOK = 2
    MASK_ENG = ("dve", "pool")
    for h in range(8):
        if h + 1 < 8:
            load_head(h + 1)
        kt, bk = KTh[h % 2]
        vt, bv = Vh[h % 2]
        for j in range(16):
            par = unit % 2
            tiles = []
            nk = 4 * j + 4
            for kti in range(nk):
                for m in range(2):
                    r = kti - 4 * j
                    tiles.append((m, kti, r if r > 0 else 0, r >= 0, kti == 0, kti == nk - 1))
            nt = len(tiles)

            def qk(i):
                m, kti, r, diag, first, last = tiles[i]
                c0 = 128 * r
                sb_ = i % 3
                qz, bqz = Qz[h % 2][m]
                MM(PS[sb_][:, c0:512], kt[:, kti * 128:(kti + 1) * 128], qz[:, j * 512 + c0:(j + 1) * 512], True, True, [bk, bqz], [BPS[sb_]])

            def pv(i, pslot):
                m, kti, r, diag, first, last = tiles[i]
                c0 = 128 * r
                sb_ = i % 3
                pt, bpt = pT[pslot]
                ACT(pt[:, c0:512], PS[sb_][:, c0:512], AF.Exp, [BPS[sb_]], [bpt], scale=0.125)
                if diag:
                    TT(MASK_ENG[m], pt[:, c0:c0 + 128], pt[:, c0:c0 + 128], CB("triu"), ALU.mult, [bpt, B_cstb], [bpt])
                MM(PS[3 + m][:, c0:512], vt[:, kti, :], pt[:, c0:512], first, last, [bv, bpt], [BPS[3 + m]], acc=(not first))
                MM(PS[5 + m][:, c0:512], CB("ones"), pt[:, c0:512], first, last, [B_cstb, bpt], [BPS[5 + m]], acc=(not first))

            for i in range(min(LOOK, nt)):
                qk(i)
            for i in range(nt):
                if i + LOOK < nt:
                    qk(i + LOOK)
                pv(i, pti % 6)
                pti += 1
                if i == 0 and pending is not None:
                    attn_epilogue_a(*pending)
                if i == nt - 1 and pending is not None:
                    attn_epilogue_b(*pending)
                    pending = None
            for m in range(2):
                os_, bos = Osb[par * 2 + m]
                ls_, bls = Lsb[par * 2 + m]
                CP("dve", os_[:], PS[3 + m][:, :], [BPS[3 + m]], [bos])
                CP("dve", ls_[:], PS[5 + m][:, :], [BPS[5 + m]], [bls])
            pending = (h, j, par, unit)
            unit += 1
    attn_epilogue_a(*pending)
    attn_epilogue_b(*pending)
    S.barrier()
    if last_phase < 3:
        return finish(nc, S, out_d)

    phase_begin()
    xs_b = [SB("xs%d" % i, [128, 2048], BF16) for i in range(2)]
    bt_b = [SB("bt%d" % i, [128, 512], BF16) for i in range(2)]
    bc_b = [SB("bc%d" % i, [128, 8, 128], BF16) for i in range(2)]
    dt_b = [SB("dtr%d" % i, [128, 32], F32) for i in range(2)]
    zs_b = [SB("zs%d" % i, [128, 2048], BF16) for i in range(2)]
    sm_b = [SB("sm%d" % i, [128, 8, 32], F32) for i in range(2)]
    xd_b = [SB("xd%d" % i, [128, 2048], BF16) for i in range(2)]
    xw_b = [SB("xw%d" % i, [128, 2048], BF16) for i in range(2)]
    xD_b = [SB("xD%d" % i, [128, 2048], F32) for i in range(2)]
    cbm_b = [[SB("cbm%d_%d" % (j, i), [128, 128], F32) for i in range(4)] for j in range(2)]
    Lh = [SB("Lh%d" % i, [128, 4, 128], F32) for i in range(3)]
    dec = [SB("dec%d" % i, [128, 4, 128], F32) for i in range(3)]
    MT = [SB("MT%d" % i, [128, 4, 128], BF16) for i in range(3)]
    stf, B_stf = SB("stf", [128, 2048], F32)
    stb, B_stb = SB("stb", [128, 2048], BF16)
    t1, B_t1 = SB("t1", [128, 512], F32)
    t2, B_t2 = SB("t2", [128, 512], F32)
    junk, B_junk = SB("junk", [128, 512], F32)
    ssq, B_ssq = SB("ssq", [128, 4], F32)
    yn, B_yn = SB("yn", [128, 512], BF16)
    yTs = [SB("yTs%d" % i, [128, 16, 512], BF16) for i in range(2)]
    MSET("dve", stf[:], 0.0, [B_stf])
    MSET("pool", stb[:], 0.0, [B_stb])
    BCT_v = BCT_d.rearrange("(c p) t -> p c t", p=128)
    YT_v = YT_d.rearrange("(c p) t -> p c t", p=128)

    def load_chunk(c):
        i = c % 2
        DMA("sp", xs_b[i][0][:], XS_d[c * 128:(c + 1) * 128, :], xs_b[i][1], NOBUF, [xs_b[i][1]])
        DMA("sp", bt_b[i][0][:], BTM_d[c * 128:(c + 1) * 128, :], bt_b[i][1], NOBUF, [bt_b[i][1]])
        DMA("sp", bc_b[i][0][:], BCT_v[:, :, c * 128:(c + 1) * 128], bc_b[i][1], NOBUF, [bc_b[i][1]])
        DMA("sp", dt_b[i][0][:], DT_d[c * 128:(c + 1) * 128, :], dt_b[i][1], NOBUF, [dt_b[i][1]])
        DMA("sp", zs_b[i][0][:], ZS_d[c * 128:(c + 1) * 128, :], zs_b[i][1], NOBUF, [zs_b[i][1]])

    def b3(ap32):
        return ap32.unsqueeze(2).to_broadcast([128, 32, 64])

    def prologue(c):
        i_ = c % 2
        xs_, bxs_ = xs_b[i_]
        bc_, bbc_ = bc_b[i_]
        dtr_, bdt_ = dt_b[i_]
        sm_, B_sm_ = sm_b[i_]
        xd_, B_xd_ = xd_b[i_]
        TT("dve", sm_[:, 6, :], dtr_[:], P("dtb"), ALU.add, [bdt_, B_prm], [B_sm_])
        ACT(sm_[:, 6, :], sm_[:, 6, :], AF.Exp, [B_sm_], [B_sm_])
        ACT(sm_[:, 0, :], sm_[:, 6, :], AF.Ln, [B_sm_], [B_sm_], bias=1.0, scale=1.0)
        TT("dve", sm_[:, 1, :], sm_[:, 0, :], derived[:, 8:40], ALU.mult, [B_sm_, B_der], [B_sm_])
        MM(PS[7][:, 0:32], C("triu"), sm_[:, 1, :], True, True, [B_cst, B_sm_], [BPS[7]])
        MM(PS[7][:, 32:64], C("ones"), sm_[:, 1, :], True, True, [B_cst, B_sm_], [BPS[7]], acc=True)
        CP("dve", sm_[:, 2, :], PS[7][:, 0:32], [BPS[7]], [B_sm_])
        ACT(sm_[:, 3, :], PS[7][:, 0:32], AF.Exp, [BPS[7]], [B_sm_])
        TT("dve", sm_[:, 6, :], PS[7][:, 32:64], sm_[:, 2, :], ALU.subtract, [BPS[7], B_sm_], [B_sm_])
        ACT(sm_[:, 4, :], sm_[:, 6, :], AF.Exp, [B_sm_], [B_sm_])
        ACT(sm_[:, 5, :], PS[7][:, 32:64], AF.Exp, [BPS[7]], [B_sm_])
        TT("dve", xd_[:].rearrange("p (h q) -> p h q", h=32), xs_[:].rearrange("p (h q) -> p h q", h=32), b3(sm_[:, 0, :]), ALU.mult, [bxs_, B_sm_], [B_xd_])
        TT("pool", xD_b[i_][0][:].rearrange("p (h q) -> p h q", h=32), xs_[:].rearrange("p (h q) -> p h q", h=32), b3(P("dsk")), ALU.mult, [bxs_, B_prm], [xD_b[i_][1]])
        TT("pool", xw_b[i_][0][:].rearrange("p (h q) -> p h q", h=32), xd_[:].rearrange("p (h q) -> p h q", h=32), b3(sm_[:, 4, :]), ALU.mult, [B_xd_, B_sm_], [xw_b[i_][1]])
        for g_ in range(4):
            MM(PS[6][:, g_ * 128:(g_ + 1) * 128], bc_[:, g_, :], bc_[:, 4 + g_, :], True, True, [bbc_], [BPS[6]], acc=(g_ > 0))
        for g_ in range(4):
            TT("dve", cbm_b[i_][g_][0][:], PS[6][:, g_ * 128:(g_ + 1) * 128], C("triu"), ALU.mult, [BPS[6], B_cst], [cbm_b[i_][g_][1]])

    load_chunk(0)
    prologue(0)
    hc = 0
    for c in range(64):
        if c + 1 < 64:
            load_chunk(c + 1)
        i = c % 2
        xs, bxs = xs_b[i]
        bt, bbt = bt_b[i]
        bc, bbc = bc_b[i]
        dtr, bdt = dt_b[i]
        zs, bzs = zs_b[i]
        sm, B_sm = sm_b[i]
        xd, B_xd = xd_b[i]
        cbm = cbm_b[i]
        xw, B_xw = xw_b[i]
        xD, B_xD = xD_b[i]
        xs3 = xs[:].rearrange("p (h q) -> p h q", h=32)
        if c % 4 == 0:
            ys, bys = yTs[(c // 4) % 2]
        def quads(g, hcbox):
            ypb = 4 + (g % 2)
            for qd in range(2):
                h0 = g * 8 + qd * 4
                k3 = hcbox[0] % 3
                lh, blh = Lh[k3]
                de, bde = dec[k3]
                mt, bmt = MT[k3]
                TT("pool", lh[:], C("sl").unsqueeze(1).to_broadcast([128, 4, 128]),
                   sm[:, 1, h0:h0 + 4].unsqueeze(2).to_broadcast([128, 4, 128]), ALU.mult, [B_cst, B_sm], [blh])
                sb_ = hcbox[0] % 2
                for q in range(4):
                    MM(PS[sb_][:, q * 128:(q + 1) * 128], lh[:, q, :], C("triu"), True, True, [blh, B_cst], [BPS[sb_]], acc=(q > 0))
                ACT(de[:], PS[sb_][:, :].rearrange("p (q l) -> p q l", q=4), AF.Exp, [BPS[sb_]], [bde])
                TT("dve", mt[:], de[:], cbm[g][0][:].unsqueeze(1).to_broadcast([128, 4, 128]), ALU.mult, [bde, cbm[g][1]], [bmt])
                for q in range(4):
                    hh = qd * 4 + q
                    hd = h0 + q
                    MM(PS[ypb][:, hh * 64:(hh + 1) * 64], mt[:, q, :], xd[:, hd * 64:(hd + 1) * 64], True, True, [bmt, B_xd], [BPS[ypb]], acc=(hh > 0))
                hcbox[0] += 1
        hcbox = [hc]
        quads(0, hcbox)
        for g in range(4):
            ypb = 4 + (g % 2)
            if g + 1 < 4:
                quads(g + 1, hcbox)
            if g == 1 and c + 1 < 64:
                prologue(c + 1)
            sb_ = 2 + hc % 2
            hc += 1
            MM(PS[sb_][:, :], bc[:, 4 + g, :], stb[:, g * 512:(g + 1) * 512], True, True, [bbc, B_stb], [BPS[sb_]])
            TT("dve", t1[:].rearrange("p (h q) -> p h q", h=8), PS[sb_][:].rearrange("p (h q) -> p h q", h=8),
               sm[:, 3, g * 8:(g + 1) * 8].unsqueeze(2).to_broadcast([128, 8, 64]), ALU.mult, [BPS[sb_], B_sm], [B_t1])
            TT("dve", t2[:], PS[ypb][:, :], t1[:], ALU.add, [BPS[ypb], B_t1], [B_t2])
            TT("dve", t2[:], t2[:], xD[:, g * 512:(g + 1) * 512], ALU.add, [B_t2, B_xD], [B_t2])
            TT("dve", t2[:], t2[:], zs[:, g * 512:(g + 1) * 512], ALU.mult, [B_t2, bzs], [B_t2])
            ACT(junk[:], t2[:], AF.Square, [B_t2], [B_junk, B_ssq], accum_out=ssq[:, 0:1])
            TS("dve", ssq[:, 1:2], ssq[:, 0:1], 1.0 / 512.0, EPS, ALU.mult, ALU.add, [B_ssq], [B_ssq])
            ACT(ssq[:, 2:3], ssq[:, 1:2], AF.Ln, [B_ssq], [B_ssq])
            ACT(ssq[:, 3:4], ssq[:, 2:3], AF.Exp, [B_ssq], [B_ssq], scale=-0.5)
            a0, a1 = PRM_OFF["ssdw"]
            STT(yn[:], t2[:], ssq[:, 3:4], prm[:, a0 + g * 512:a0 + (g + 1) * 512], ALU.mult, ALU.mult, [B_t2, B_ssq, B_prm], [B_yn])
            tb = 6 + (g % 2) if False else 7
            psv = PS[7][:].bitcast(BF16)
            for q in range(4):
                TR(psv[:, q * 128:(q + 1) * 128], yn[:, q * 128:(q + 1) * 128], CB("ident"), [B_yn, B_cstb], [BPS[7]], acc=(q > 0))
            CP("act", ys[:, g * 4:(g + 1) * 4, (c % 4) * 128:(c % 4 + 1) * 128], psv[:, 0:512].rearrange("p (q t) -> p q t", q=4),
               [BPS[7]], [bys], acc=True)
            sb_ = 2 + hc % 2
            hc += 1
            MM(PS[sb_][:, :], bt[:, g * 128:(g + 1) * 128], xw[:, g * 512:(g + 1) * 512], True, True, [bbt, B_xw], [BPS[sb_]])
            TT("pool", stf[:, g * 512:(g + 1) * 512].rearrange("p (h q) -> p h q", h=8), stf[:, g * 512:(g + 1) * 512].rearrange("p (h q) -> p h q", h=8),
               sm[:, 5, g * 8:(g + 1) * 8].unsqueeze(2).to_broadcast([128, 8, 64]), ALU.mult, [B_stf, B_sm], [B_stf])
            TT("dve", stf[:, g * 512:(g + 1) * 512], stf[:, g * 512:(g + 1) * 512], PS[sb_][:, :], ALU.add, [B_stf, BPS[sb_]], [B_stf])
            CP("act", stb[:, g * 512:(g + 1) * 512], stf[:, g * 512:(g + 1) * 512], [B_stf], [B_stb])
        if c % 4 == 3:
            DMA("sp", YT_v[:, :, (c // 4) * 512:(c // 4 + 1) * 512], ys[:], bys, [bys], NOBUF)
    S.barrier()
    if last_phase < 4:
        return finish(nc, S, out_d)

    phase_begin()
    wba, B_wba = SB("wba", [128, 8, 1024], BF16)
    wbs, B_wbs = SB("wbs", [128, 16, 1024], BF16)
    wo, B_wo = SB("wo", [128, 8, 1024], BF16)
    DMA("pool", wba[:], w_bra_d.rearrange("(k p) c -> p k c", p=128), B_wba, NOBUF, [B_wba])
    DMA("pool", wbs[:], w_brs_d.rearrange("(k p) c -> p k c", p=128), B_wbs, NOBUF, [B_wbs])
    DMA("pool", wo[:], w_out_d.rearrange("(k p) c -> p k c", p=128), B_wo, NOBUF, [B_wo])
    at_b = [SB("at%d" % i, [128, 8, 256], BF16) for i in range(2)]
    yt_b = [SB("yt%d" % i, [128, 16, 256], BF16) for i in range(2)]
    gt_b = [SB("gt%d" % i, [128, 16, 256], BF16) for i in range(2)]
    xr_b = [SB("xr%d" % i, [128, 2, 1024], F32) for i in range(2)]
    m1, B_m1 = SB("m1", [128, 256], F32)
    m2, B_m2 = SB("m2", [128, 256], F32)
    mT, B_mT = SB("mT", [128, 8, 256], BF16)
    h1s = [SB("h1s%d" % i, [128, 1024], F32) for i in range(2)]
    AT_v = AT_d.rearrange("(c p) t -> p c t", p=128)
    GT_v = GT_d.rearrange("(c p) t -> p c t", p=128)
    YT_v2 = YT_d.rearrange("(c p) t -> p c t", p=128)
    x_v = x_d.rearrange("(t p) c -> p t c", p=128)

    def load_p4(ch):
        i = ch % 2
        DMA("sp", at_b[i][0][:], AT_v[:, :, ch * 256:(ch + 1) * 256], at_b[i][1], NOBUF, [at_b[i][1]])
        DMA("sp", yt_b[i][0][:], YT_v2[:, :, ch * 256:(ch + 1) * 256], yt_b[i][1], NOBUF, [yt_b[i][1]])
        DMA("sp", gt_b[i][0][:], GT_v[:, :, ch * 256:(ch + 1) * 256], gt_b[i][1], NOBUF, [gt_b[i][1]])
        DMA("sp", xr_b[i][0][:], x_v[:, ch * 2:(ch + 1) * 2, :], xr_b[i][1], NOBUF, [xr_b[i][1]])

    def layer_norm(dst, src, bsrc, bdst, gname, bname):
        S.op("dve", lambda: nc.vector.bn_stats(out=bst[:, 0:6], in_=src[:, 0:512]), [bsrc], [B_bst])
        S.op("dve", lambda: nc.vector.bn_stats(out=bst[:, 6:12], in_=src[:, 512:1024]), [bsrc], [B_bst], acc=True)
        S.op("dve", lambda: nc.vector.bn_aggr(out=mv[:, 0:2], in_=bst[:]), [B_bst], [B_mv])
        TS("dve", mv[:, 2:3], mv[:, 1:2], EPS, None, ALU.add, None, [B_mv], [B_mv])
        ACT(mv[:, 2:3], mv[:, 2:3], AF.Ln, [B_mv], [B_mv])
        ACT(mv[:, 3:4], mv[:, 2:3], AF.Exp, [B_mv], [B_mv], scale=-0.5)
        TS("dve", src, src, mv[:, 0:1], mv[:, 3:4], ALU.subtract, ALU.mult, [bsrc, B_mv], [bsrc])
        TT("dve", src, src, P(gname), ALU.mult, [bsrc, B_prm], [bsrc])
        TT("dve", dst, src, P(bname), ALU.add, [bsrc, B_prm], [bdst])

    load_p4(0)
    ev = 0
    for ch in range(32):
        if ch + 1 < 32:
            load_p4(ch + 1)
        i = ch % 2
        at, bat = at_b[i]
        yt, byt = yt_b[i]
        gt, bgt = gt_b[i]
        xr, bxr = xr_b[i]
        for dmi in range(8):
            pa = ev % 2
            pbk = 2 + ev % 2
            ev += 1
            for k in range(8):
                MM(PS[pa][:, 0:256], wba[:, k, dmi * 128:(dmi + 1) * 128], at[:, k, :], k == 0, k == 7, [B_wba, bat], [BPS[pa]], acc=(k > 0))
            for k in range(16):
                MM(PS[pbk][:, 0:256], wbs[:, k, dmi * 128:(dmi + 1) * 128], yt[:, k, :], k == 0, k == 15, [B_wbs, byt], [BPS[pbk]], acc=(k > 0))
            TT("dve", m1[:], PS[pa][:, 0:256], gt[:, dmi, :], ALU.mult, [BPS[pa], bgt], [B_m1])
            TT("dve", m2[:], PS[pbk][:, 0:256], gt[:, 8 + dmi, :], ALU.mult, [BPS[pbk], bgt], [B_m2])
            TT("pool", mT[:, dmi, :], m1[:], m2[:], ALU.add, [B_m1, B_m2], [B_mT], acc=(dmi > 0))
        for tt in range(2):
            T = ch * 2 + tt
            for half in range(2):
                pb = 4 + half
                for k in range(8):
                    MM(PS[pb][:, :], mT[:, k, tt * 128:(tt + 1) * 128], wo[:, k, half * 512:(half + 1) * 512], k == 0, k == 7,
                       [B_mT, B_wo], [BPS[pb]], acc=(k > 0))
                STT(xr[:, tt, half * 512:(half + 1) * 512], xr[:, tt, half * 512:(half + 1) * 512], ALPHA, PS[pb][:, :], ALU.mult, ALU.add,
                    [bxr, BPS[pb]], [bxr])
            hs, bhs = h1s[T % 2]
            layer_norm(hs[:], xr[:, tt, :], bxr, bhs, "ln1g", "ln1b")
            DMA("sp", H1_d[T * 128:(T + 1) * 128, :], hs[:], bhs, [bhs], NOBUF)
    S.barrier()
    if last_phase < 5:
        return finish(nc, S, out_d)

    phase_begin()
    wr, B_wr = SB("wr", [128, 8, 256], F32)
    wsg, B_wsg = SB("wsg", [128, 8, 256], BF16)
    wsu, B_wsu = SB("wsu", [128, 8, 256], BF16)
    wsd, B_wsd = SB("wsd", [128, 2, 1024], BF16)
    DMA("sp", wr[:], w_rt_d.rearrange("(k p) c -> p k c", p=128), B_wr, NOBUF, [B_wr])
    DMA("pool", wsg[:], w_sg_d.rearrange("(k p) c -> p k c", p=128), B_wsg, NOBUF, [B_wsg])
    DMA("pool", wsu[:], w_su_d.rearrange("(k p) c -> p k c", p=128), B_wsu, NOBUF, [B_wsu])
    DMA("pool", wsd[:], w_sd_d.rearrange("(k p) c -> p k c", p=128), B_wsd, NOBUF, [B_wsd])
    h1c_b = [SB("h1c%d" % i, [128, 4, 1024], F32) for i in range(2)]
    h1b = [SB("h1b%d" % i, [128, 1024], BF16) for i in range(2)]
    h1T, B_h1T = SB("h1T", [128, 8, 128], F32)
    h1Tb, B_h1Tb = SB("h1Tb", [128, 8, 512], BF16)
    rt, B_rt = SB("rt", [128, 6, 256], F32)
    selcum, B_selcum = SB("selcum", [128, 256], F32)
    rs_, B_rs = SB("rs", [128, 8, 8], F32)
    i8u, B_i8u = SB("i8u", [128, 8], U32)
    rsc, B_rsc = SB("rsc", [128, 4], F32)
    tmpr, B_tmpr = SB("tmpr", [128, 256], F32)
    sgs, B_sgs = SB("sgs", [128, 512], F32)
    hsT, B_hsT = SB("hsT", [128, 2, 512], BF16)
    r2s = [SB("r2s%d" % i, [128, 1024], F32) for i in range(2)]
    MSET("dve", selcum[:], 0.0, [B_selcum])
    H1_v = H1_d.rearrange("(t p) c -> p t c", p=128)

    def load_h1(ch):
        t_, b_ = h1c_b[ch % 2]
        DMA("sp", t_[:], H1_v[:, ch * 4:(ch + 1) * 4, :], b_, NOBUF, [b_])

    load_h1(0)
    for ch in range(16):
        if ch + 1 < 16:
            load_h1(ch + 1)
        h1c, B_h1c = h1c_b[ch % 2]
        for tt in range(4):
            T = ch * 4 + tt
            hb, bhb = h1b[T % 2]
            CP("act", hb[:], h1c[:, tt, :], [B_h1c], [bhb])
            for half in range(2):
                pb = 4 + half
                for q in range(4):
                    k = half * 4 + q
                    TR(PS[pb][:, q * 128:(q + 1) * 128], h1c[:, tt, k * 128:(k + 1) * 128], C("ident"), [B_h1c, B_cst], [BPS[pb]], acc=(q > 0))
                CP("act", h1T[:, half * 4:(half + 1) * 4, :], PS[pb][:, :].rearrange("p (q t) -> p q t", q=4), [BPS[pb]], [B_h1T], acc=(half > 0))
            CP("pool", h1Tb[:, :, tt * 128:(tt + 1) * 128], h1T[:], [B_h1T], [B_h1Tb], acc=True)
            for k in range(8):
                MM(PS[6][:, 0:256], h1T[:, k, :], wr[:, k, :], k == 0, k == 7, [B_h1T, B_wr], [BPS[6]], acc=(k > 0))
            sc = rt[:, 0, :]
            chh = rt[:, 1, :]
            wk2 = rt[:, 2, :]
            sel = rt[:, 3, :]
            wsel = rt[:, 4, :]
            pos = rt[:, 5, :]
            ACT(sc, PS[6][:, 0:256], AF.Sigmoid, [BPS[6]], [B_rt])
            TT("dve", chh, sc, P("rbias"), ALU.add, [B_rt, B_prm], [B_rt])
            ch3 = chh.rearrange("p (g e) -> p g e", g=8)
            wk3 = wk2.rearrange("p (g e) -> p g e", g=8)
            RED(rs_[:, 0, :], ch3, ALU.max, [B_rt], [B_rs])
            TT("dve", wk3, ch3, rs_[:, 0, :].unsqueeze(2).to_broadcast([128, 8, 32]), ALU.is_equal, [B_rt, B_rs], [B_rt])
            STT(wk2, wk2, -1e9, chh, ALU.mult, ALU.add, [B_rt], [B_rt])
            RED(rs_[:, 1, :], wk3, ALU.max, [B_rt], [B_rs])
            TT("dve", rs_[:, 1, :], rs_[:, 1, :], rs_[:, 0, :], ALU.add, [B_rs], [B_rs])
            S.op("dve", lambda: nc.vector.max(out=rs_[:, 2, :], in_=rs_[:, 1, :]), [B_rs], [B_rs])
            TS("dve", rs_[:, 3, :], rs_[:, 1, :], rs_[:, 2, 3:4], None, ALU.is_ge, None, [B_rs], [B_rs])
            TS("dve", rs_[:, 3, :], rs_[:, 3, :], 1e9, -1e9, ALU.mult, ALU.add, [B_rs], [B_rs])
            TT("dve", wk3, ch3, rs_[:, 3, :].unsqueeze(2).to_broadcast([128, 8, 32]), ALU.add, [B_rt, B_rs], [B_rt])
            S.op("dve", lambda: nc.vector.max(out=rs_[:, 7, :], in_=rt[:, 2, :]), [B_rt, B_rs], [B_rs])
            TS("dve", sel, wk2, rs_[:, 7, 7:8], None, ALU.is_ge, None, [B_rt, B_rs], [B_rt])
            TT("dve", wsel, sc, sel, ALU.mult, [B_rt], [B_rt])
            S.op("dve", lambda: nc.vector.max(out=rs_[:, 4, :], in_=rt[:, 4, :]), [B_rt, B_rs], [B_rs])
            S.op("dve", lambda: nc.vector.max_index(out=i8u[:], in_max=rs_[:, 4, :], in_values=rt[:, 4, :]), [B_rt, B_rs], [B_i8u])
            CP("dve", rs_[:, 5, :], i8u[:], [B_i8u], [B_rs])
            RED(rsc[:, 0:1], rs_[:, 4, :], ALU.add, [B_rs], [B_rsc])
            S.op("dve", lambda: nc.vector.reciprocal(out=rsc[:, 1:2], in_=rsc[:, 0:1]), [B_rsc], [B_rsc])
            MM(PS[7][:, 0:256], C("slt"), sel, True, False, [B_cst, B_rt], [BPS[7]])
            MM(PS[7][:, 0:256], C("ones"), selcum[:], False, True, [B_cst, B_selcum], [BPS[7]], acc=True)
            CP("act", pos, PS[7][:, 0:256], [BPS[7]], [B_rt])
            TT("pool", selcum[:], selcum[:], sel, ALU.add, [B_selcum, B_rt], [B_selcum])
            for k in range(8):
                STT(tmpr[:], cst[:, 640:896], rs_[:, 5, k:k + 1], pos, ALU.is_equal, ALU.mult, [B_cst, B_rs, B_rt, B_tmpr], [B_tmpr])
                RED(rs_[:, 6, k:k + 1], tmpr[:], ALU.add, [B_tmpr], [B_rs])
            STT(rs_[:, 7, :], rs_[:, 5, :], float(CAP), rs_[:, 6, :], ALU.mult, ALU.add, [B_rs], [B_rs])
            TS("dve", rs_[:, 3, :], rs_[:, 6, :], CAP - 0.5, 1e6, ALU.is_gt, ALU.mult, [B_rs], [B_rs])
            TT("dve", rs_[:, 7, :], rs_[:, 7, :], rs_[:, 3, :], ALU.max, [B_rs], [B_rs])
            CP("dve", slot_all[:, T, :], rs_[:, 7, :], [B_rs], [B_slot], acc=True)
            TS("dve", rs_[:, 3, :], rs_[:, 6, :], CAP - 0.5, None, ALU.is_lt, None, [B_rs], [B_rs])
            TS("dve", rs_[:, 4, :], rs_[:, 4, :], rsc[:, 1:2], 2.5, ALU.mult, ALU.mult, [B_rs, B_rsc], [B_rs])
            TT("dve", tw_all[:, T, :], rs_[:, 4, :], rs_[:, 3, :], ALU.mult, [B_rs], [B_tw], acc=True)
            for k in range(8):
                S.dma("pool", (lambda T=T, k=k, hb=hb: nc.gpsimd.indirect_dma_start(
                    out=XB_d[:, :], out_offset=bass.IndirectOffsetOnAxis(ap=slot_all[:, T, k:k + 1], axis=0),
                    in_=hb[:, :], in_offset=None, bounds_check=regs["bc"], oob_is_err=False)),
                    bhb, [bhb, B_slot], NOBUF)
        for fh in range(2):
            for k in range(8):
                MM(PS[0][:, :], wsg[:, k, fh * 128:(fh + 1) * 128], h1Tb[:, k, :], k == 0, k == 7, [B_wsg, B_h1Tb], [BPS[0]], acc=(k > 0))
            for k in range(8):
                MM(PS[1][:, :], wsu[:, k, fh * 128:(fh + 1) * 128], h1Tb[:, k, :], k == 0, k == 7, [B_wsu, B_h1Tb], [BPS[1]], acc=(k > 0))
            ACT(sgs[:], PS[0][:, :], AF.Silu, [BPS[0]], [B_sgs])
            TT("dve", hsT[:, fh, :], PS[1][:, :], sgs[:], ALU.mult, [BPS[1], B_sgs], [B_hsT], acc=(fh > 0))
        for tt in range(4):
            T = ch * 4 + tt
            r2, br2 = r2s[T % 2]
            for half in range(2):
                pb = 2 + half
                for fk in range(2):
                    MM(PS[pb][:, :], hsT[:, fk, tt * 128:(tt + 1) * 128], wsd[:, fk, half * 512:(half + 1) * 512], fk == 0, fk == 1,
                       [B_hsT, B_wsd], [BPS[pb]], acc=(fk > 0))
                STT(r2[:, half * 512:(half + 1) * 512], h1c[:, tt, half * 512:(half + 1) * 512], ALPHA, PS[pb][:, :], ALU.mult, ALU.add,
                    [B_h1c, BPS[pb]], [br2], acc=(half > 0))
            DMA("sp", R2_d[T * 128:(T + 1) * 128, :], r2[:], br2, [br2], NOBUF)
    S.barrier()
    if last_phase < 6:
        return finish(nc, S, out_d)

    phase_begin()
    xb_b = [SB("xb%d" % i, [128, 4, 1024], BF16) for i in range(3)]
    wf_b = [SB("wf%d" % i, [128, 6144], F32) for i in range(3)]
    wbf_b = [SB("wbf%d" % i, [128, 6144], BF16) for i in range(3)]
    xbT_b = [SB("xbT%d" % i, [128, 8, 512], BF16) for i in range(2)]
    sg_b = [SB("sg%d" % i, [128, 512], BF16) for i in range(2)]
    hT_b = [SB("hT%d" % i, [128, 2, 512], BF16) for i in range(2)]
    yb_b = [SB("yb%d" % i, [128, 4, 1024], BF16) for i in range(2)]
    XB_v = XB_d.rearrange("(e p t) d -> e p t d", p=128, t=4)
    Y_v = Y_d.rearrange("(e p t) d -> e p t d", p=128, t=4)

    def load_e(e):
        DMA("sp", xb_b[e % 3][0][:], XB_v[e], xb_b[e % 3][1], NOBUF, [xb_b[e % 3][1]])
        wf, bwf = wf_b[e % 3]
        DMA("sp", wf[:, 0:2048], w_eg_d[e].rearrange("(p k) f -> p (k f)", p=128), bwf, NOBUF, [bwf])
        DMA("sp", wf[:, 2048:4096], w_eu_d[e].rearrange("(p k) f -> p (k f)", p=128), bwf, NOBUF, [bwf], acc=True)
        DMA("sp", wf[:, 4096:6144].rearrange("p (k f) -> p k f", k=2), w_ed_d[e].rearrange("(k p) f -> p k f", p=128), bwf, NOBUF, [bwf], acc=True)

    def cast_e(e):
        wf, bwf = wf_b[e % 3]
        wb, bwb = wbf_b[e % 3]
        CP("act", wb[:, 0:2048], wf[:, 0:2048], [bwf], [bwb])
        CP("dve", wb[:, 2048:4096], wf[:, 2048:4096], [bwf], [bwb], acc=True)
        CP("pool", wb[:, 4096:6144], wf[:, 4096:6144], [bwf], [bwb], acc=True)

    def stage_T(e):
        xb, bxb = xb_b[e % 3]
        xbT, bxbT = xbT_b[e % 2]
        for t in range(4):
            pb = t % 2
            psv = PS[pb][:].bitcast(BF16)
            for k in range(8):
                TR(psv[:, k * 128:(k + 1) * 128], xb[:, t, :].rearrange("p (d k) -> p k d", k=8)[:, k, :], CB("ident"), [bxb, B_cstb], [BPS[pb]], acc=(k > 0))
            CP("act" if t % 2 == 0 else "dve", xbT[:, :, t * 128:(t + 1) * 128], psv[:, :].rearrange("p (k s) -> p k s", k=8), [BPS[pb]], [bxbT], acc=(t > 0))

    def stage_GU(e):
        wb, bwb = wbf_b[e % 3]
        xbT, bxbT = xbT_b[e % 2]
        sg, bsg = sg_b[e % 2]
        hT, bhT = hT_b[e % 2]
        weg = wb[:, 0:2048].rearrange("p (k f) -> p k f", k=8)
        weu = wb[:, 2048:4096].rearrange("p (k f) -> p k f", k=8)
        for fh in range(2):
            for k in range(8):
                MM(PS[2 + fh][:, :], weg[:, k, fh * 128:(fh + 1) * 128], xbT[:, k, :], k == 0, k == 7, [bwb, bxbT], [BPS[2 + fh]], acc=(k > 0))
            for k in range(8):
                MM(PS[4 + fh][:, :], weu[:, k, fh * 128:(fh + 1) * 128], xbT[:, k, :], k == 0, k == 7, [bwb, bxbT], [BPS[4 + fh]], acc=(k > 0))
            ACT(sg[:], PS[2 + fh][:, :], AF.Silu, [BPS[2 + fh]], [bsg])
            TT("dve", hT[:, fh, :], PS[4 + fh][:, :], sg[:], ALU.mult, [BPS[4 + fh], bsg], [bhT], acc=(fh > 0))

    def stage_D(e):
        wb, bwb = wbf_b[e % 3]
        hT, bhT = hT_b[e % 2]
        yb, byb = yb_b[e % 2]
        wed = wb[:, 4096:6144].rearrange("p (k f) -> p k f", k=2)
        n6 = 0
        for t in range(4):
            for half in range(2):
                pb = 6 + n6 % 2
                for fk in range(2):
                    MM(PS[pb][:, :], hT[:, fk, t * 128:(t + 1) * 128], wed[:, fk, half * 512:(half + 1) * 512], fk == 0, fk == 1,
                       [bhT, bwb], [BPS[pb]], acc=(fk > 0))
                CP("act" if n6 % 2 == 0 else "dve", yb[:, t, half * 512:(half + 1) * 512], PS[pb][:, :], [BPS[pb]], [byb], acc=(n6 > 0))
                n6 += 1
        DMA("act", Y_v[e], yb[:], byb, [byb], NOBUF)

    for e in range(3):
        load_e(e)
    for i in range(-2, NE):
        if 3 <= i + 4 < NE:
            load_e(i + 4)
        if 0 <= i + 2 < NE:
            cast_e(i + 2)
            stage_T(i + 2)
        if 0 <= i + 1 < NE:
            stage_GU(i + 1)
        if i >= 0:
            stage_D(i)
    S.barrier()
    if last_phase < 7:
        return finish(nc, S, out_d)

    phase_begin()
    r2_b = [SB("r2l%d" % i, [128, 1024], F32) for i in range(2)]
    gk_b = [SB("gk%d" % i, [128, 8, 1024], BF16) for i in range(2)]
    ot_b = [SB("ot%d" % i, [128, 1024], F32) for i in range(2)]
    B_out = Buf("out")

    def load_t(T):
        i = T % 2
        DMA("sp", r2_b[i][0][:], R2_d[T * 128:(T + 1) * 128, :], r2_b[i][1], NOBUF, [r2_b[i][1]])
        gk, bgk = gk_b[i]
        for k in range(8):
            S.dma("pool", (lambda T=T, k=k, gk=gk: nc.gpsimd.indirect_dma_start(
                out=gk[:, k, :], out_offset=None, in_=Y_d[:, :],
                in_offset=bass.IndirectOffsetOnAxis(ap=slot_all[:, T, k:k + 1], axis=0),
                bounds_check=regs["bc"], oob_is_err=False)),
                bgk, [B_slot], [bgk], acc=(k > 0))

    for i in range(2):
        MSET("dve", gk_b[i][0][:], 0.0, [gk_b[i][1]])
    load_t(0)
    for T in range(64):
        if T + 1 < 64:
            load_t(T + 1)
        i = T % 2
        r2, br2 = r2_b[i]
        gk, bgk = gk_b[i]
        ot, bot = ot_b[i]
        for k in range(8):
            STT(r2[:], gk[:, k, :], tw_all[:, T, k:k + 1], r2[:], ALU.mult, ALU.add, [bgk, B_tw, br2], [br2])
        layer_norm(ot[:], r2[:], br2, bot, "ln2g", "ln2b")
        DMA("sp", out_d[T * 128:(T + 1) * 128, :], ot[:], bot, [bot], [B_out], acc=True)
    S.barrier(release=False)
    return finish(nc, S, out_d)


def finish(nc, S, out_d):
    S.barrier(release=False)
    S.emit()
    return nc


def make_consts():
    p = np.arange(128)[:, None]
    f = np.arange(128)[None, :]
    cst = np.zeros((128, 896), np.float32)
    cst[:, 0:128] = (p == f)
    cst[:, 128:256] = (p <= f)
    cst[:, 256:384] = (p > f)
    cst[:, 384:512] = (p < f)
    cst[:, 512:640] = 1.0
    cst[:, 640:896] = np.arange(256, dtype=np.float32)[None, :]
    tokid = (np.arange(64)[None, :] * 128 + np.arange(128)[:, None]).astype(np.int32)
    return cst, tokid


def make_prm(inp):
    prm = np.zeros((128, PRM_W), np.float32)

    def put(name, arr):
        a, b = PRM_OFF[name]
        prm[:, a:b] = arr

    put("lam4", np.concatenate([inp["lambda_q1"][0], inp["lambda_k1"][0], inp["lambda_q2"][0], inp["lambda_k2"][0]])[None, :])
    put("subln", inp["attn_subln_w"][0][:, None])
    cw = inp["conv_w"][0]
    put("convw", cw.reshape(4, 24, 128).transpose(2, 1, 0).reshape(128, 96))
    put("convb", inp["conv_b"][0].reshape(24, 128).T)
    put("dtb", inp["dt_bias"][0][None, :])
    put("alog", inp["a_log"][0][None, :])
    put("dsk", inp["d_skip"][0][None, :])
    put("rbias", inp["router_bias"][0][None, :])
    put("ssdw", inp["ssd_norm_w"][0][None, :])
    put("ln1g", inp["ln1_g"][0][None, :])
    put("ln1b", inp["ln1_b"][0][None, :])
    put("ln2g", inp["ln2_g"][0][None, :])
    put("ln2b", inp["ln2_b"][0][None, :])
    return prm


def make_in_maps(inp, batches):
    cst, tokid = make_consts()
    prm = make_prm(inp)
    shared = {
        "w_in": np.ascontiguousarray(inp["w_in"][0]), "cst": cst, "prm": prm, "tokid": tokid,
        "w_br_attn": np.ascontiguousarray(inp["w_br_attn"][0]), "w_br_ssd": np.ascontiguousarray(inp["w_br_ssd"][0]),
        "w_out": np.ascontiguousarray(inp["w_out"][0]), "w_router": np.ascontiguousarray(inp["w_router"][0]),
        "w_sh_gate": np.ascontiguousarray(inp["w_sh_gate"][0]), "w_sh_up": np.ascontiguousarray(inp["w_sh_up"][0]),
        "w_sh_down": np.ascontiguousarray(inp["w_sh_down"][0]),
        "w_exp_gate": np.ascontiguousarray(inp["w_exp_gate"][0]), "w_exp_up": np.ascontiguousarray(inp["w_exp_up"][0]),
        "w_exp_down": np.ascontiguousarray(inp["w_exp_down"][0]),
    }
    maps = []
    for b in batches:
        m = dict(shared)
        xb = np.ascontiguousarray(inp["x"][b])
        m["x"] = xb
        m["xT"] = np.ascontiguousarray(xb.T)
        maps.append(m)
    return maps


def kernel(**inputs):
    inp = {k: np.asarray(v) for k, v in inputs.items()}
    nc = build_program()
    maps = make_in_maps(inp, list(range(8)))
    res = run_bass_kernel_spmd(nc, maps, core_ids=list(range(8)))
    out = np.stack([np.asarray(r["out"]) for r in res.results], axis=0)
    return out.astype(np.float32)
```
